# Optimizing a Trainium2 kernel written in Bass

```python
import jax, jax.numpy as jnp
from jax import lax
import numpy as np

D_MODEL = 1024
BATCH = 8
SEQ = 4096
DEPTH = 4

MEM_LEN = 256
LRU_WIDTH = D_MODEL // 2
LRU_BLOCKS = 8
LRU_BLOCK_DIM = LRU_WIDTH // LRU_BLOCKS
LRU_CONV = 4
LRU_C = 8.0
NSA_HEADS = 8
NSA_KV_HEADS = 2
HEAD_DIM = 64
CMP_LEN = 32
CMP_STRIDE = 16
CMP_HIDDEN = 128
SEL_BLOCK = 64
SEL_TOP = 16
WINDOW = 512
NSA_QBLOCK = 64
ROPE_THETA = 500000.0
ROT_DIM = HEAD_DIM // 4
SC_WIDTH = D_MODEL
SC_CONV = 3
XA_HEADS = 4
XA_HEAD_DIM = D_MODEL // XA_HEADS
N_GROUPS = 4
EXPERTS_PER_GROUP = 4
N_EXPERTS = N_GROUPS * EXPERTS_PER_GROUP
TOP_K = 2
D_EXPERT = D_MODEL // 2

EPS = 1e-6
NEG = -1e30
FORCE = 1e9
N_EVEN = (DEPTH + 1) // 2
N_ODD = DEPTH // 2
MIX_WIDTH = LRU_WIDTH + NSA_HEADS * HEAD_DIM
EVEN_SPLITS = (LRU_WIDTH, LRU_WIDTH, NSA_HEADS * HEAD_DIM) + (NSA_KV_HEADS * HEAD_DIM,) * 6 + (NSA_HEADS * 3,)
EVEN_PROJ = int(sum(EVEN_SPLITS))
EVEN_OFFSETS = tuple(int(o) for o in np.cumsum(EVEN_SPLITS)[:-1])

kernel_name = 'hybrid_rglru_nsa_shortconv_hmoe'


def rms_norm(x, g):
    x32 = x.astype(jnp.float32)
    y = x32 * lax.rsqrt(jnp.mean(x32 * x32, axis=-1, keepdims=True) + EPS)
    return (y * g.astype(jnp.float32)).astype(x.dtype)


def rope_tables(positions):
    inv = ROPE_THETA ** (-jnp.arange(0, ROT_DIM, 2, dtype=jnp.float32) / ROT_DIM)
    ang = positions.astype(jnp.float32)[..., None] * inv
    return jnp.cos(ang), jnp.sin(ang)


def apply_partial_rope(x, cos, sin):
    half = ROT_DIM // 2
    c = cos[:, :, None, :].astype(x.dtype)
    s = sin[:, :, None, :].astype(x.dtype)
    x1 = x[..., :half]
    x2 = x[..., half:ROT_DIM]
    return jnp.concatenate([x1 * c - x2 * s, x2 * c + x1 * s, x[..., ROT_DIM:]], axis=-1)


def causal_dwconv(x, w, b=None):
    width, ch = w.shape
    y = lax.conv_general_dilated(x, w[:, None, :].astype(x.dtype), window_strides=(1,),
                                 padding=[(width - 1, 0)],
                                 dimension_numbers=('NWC', 'WIO', 'NWC'),
                                 feature_group_count=ch)
    if b is not None:
        y = y + b.astype(x.dtype)
    return y


def masked_softmax(s, mask):
    p = jax.nn.softmax(jnp.where(mask, s, NEG), axis=-1)
    return jnp.where(mask, p, 0.0)


def rg_lru(x, gate_branch, conv_w, conv_b, w_r, b_r, w_i, b_i, lam):
    xc = causal_dwconv(x, conv_w, conv_b)
    bb, t, w = xc.shape
    xb = xc.reshape(bb, t, LRU_BLOCKS, LRU_BLOCK_DIM)
    r = jax.nn.sigmoid(jnp.einsum('btki,kij->btkj', xb, w_r).reshape(bb, t, w) + b_r)
    i = jax.nn.sigmoid(jnp.einsum('btki,kij->btkj', xb, w_i).reshape(bb, t, w) + b_i)
    log_a = -LRU_C * r.astype(jnp.float32) * jax.nn.softplus(-lam.astype(jnp.float32))
    a = jnp.exp(log_a)
    u = jnp.sqrt(-jnp.expm1(2.0 * log_a)) * (i * xc).astype(jnp.float32)

    def combine(left, right):
        a1, b1 = left
        a2, b2 = right
        return a1 * a2, a2 * b1 + b2

    _, h = lax.associative_scan(combine, (a, u), axis=1)
    return h.astype(x.dtype) * jax.nn.gelu(gate_branch)


def compress_kv(kv, pos_emb, w1, w2):
    bb, t, g, dh = kv.shape
    ch = kv.reshape(bb, t // CMP_STRIDE, CMP_STRIDE, g, dh)
    blocks = jnp.concatenate([ch[:, :-1], ch[:, 1:]], axis=2)
    blocks = blocks + pos_emb[None, None, :, None, :]
    ncmp = blocks.shape[1]
    flat = blocks.transpose(0, 3, 1, 2, 4).reshape(bb, g, ncmp, CMP_LEN * dh)
    return jax.nn.gelu(flat @ w1) @ w2


def nsa(q, k_cmp, v_cmp, k_sel, v_sel, k_win, v_win, gate_logits, cos, sin,
        pos_k, w1_k, w2_k, pos_v, w1_v, w2_v):
    bb, t, nh, dh = q.shape
    g = k_cmp.shape[2]
    hg = nh // g
    scale = dh ** -0.5
    kc = compress_kv(k_cmp, pos_k, w1_k, w2_k)
    vc = compress_kv(v_cmp, pos_v, w1_v, w2_v)
    n_cmp = kc.shape[2]
    n_sel = t // SEL_BLOCK
    n_top = min(SEL_TOP, n_sel)
    cmp_start = jnp.arange(n_cmp) * CMP_STRIDE
    cmp_end = cmp_start + CMP_LEN - 1
    sel_start = jnp.arange(n_sel) * SEL_BLOCK
    cover = ((cmp_start[:, None] <= sel_start[None, :] + SEL_BLOCK - 1)
             & (cmp_end[:, None] >= sel_start[None, :])).astype(jnp.float32)

    def kv_major(z):
        return z.transpose(0, 2, 1, 3)

    ks = kv_major(apply_partial_rope(k_sel, cos, sin)).reshape(bb, g, n_sel, SEL_BLOCK, dh)
    vs = kv_major(v_sel).reshape(bb, g, n_sel, SEL_BLOCK, dh)
    pad = ((0, 0), (0, 0), (WINDOW, 0), (0, 0))
    kw = jnp.pad(kv_major(apply_partial_rope(k_win, cos, sin)), pad)
    vw = jnp.pad(kv_major(v_win), pad)

    def by_query_block(z):
        c = z.shape[-1]
        z = z.reshape(bb, t // NSA_QBLOCK, NSA_QBLOCK, g, hg, c)
        return z.transpose(1, 0, 3, 4, 2, 5)

    qn = by_query_block(q)
    qr = by_query_block(apply_partial_rope(q, cos, sin))
    gt = by_query_block(jax.nn.sigmoid(gate_logits.astype(jnp.float32)).astype(q.dtype))
    b_ix = jnp.arange(bb)[:, None, None, None]
    g_ix = jnp.arange(g)[None, :, None, None]
    blk = jnp.arange(n_sel)
    ar_sel = jnp.arange(SEL_BLOCK)
    ar_win = jnp.arange(NSA_QBLOCK + WINDOW)

    def block(args):
        c, qn_c, qr_c, g_c = args
        tq = c * NSA_QBLOCK + jnp.arange(NSA_QBLOCK)
        s = jnp.einsum('bghqd,bgnd->bghqn', qn_c, kc).astype(jnp.float32) * scale
        p_cmp = masked_softmax(s, cmp_end[None, :] <= tq[:, None])
        o_cmp = jnp.einsum('bghqn,bgnd->bghqd', p_cmp.astype(vc.dtype), vc)
        imp = jnp.einsum('bghqn,ns->bgqs', p_cmp, cover)
        cur = tq // SEL_BLOCK
        forced = (blk[None, :] == 0) | (blk[None, :] == cur[:, None]) | (blk[None, :] == cur[:, None] - 1)
        causal = sel_start[None, :] <= tq[:, None]
        score = jnp.where(causal, jnp.where(forced, FORCE, imp), NEG)
        _, idx = lax.top_k(score, n_top)
        ksg = ks[b_ix, g_ix, idx]
        vsg = vs[b_ix, g_ix, idx]
        key_pos = idx[..., None] * SEL_BLOCK + ar_sel
        m_sel = (key_pos <= tq[:, None, None]).reshape(bb, g, 1, NSA_QBLOCK, n_top * SEL_BLOCK)
        s_sel = jnp.einsum('bghqd,bgqnkd->bghqnk', qr_c, ksg).astype(jnp.float32) * scale
        shp = s_sel.shape
        p_sel = masked_softmax(s_sel.reshape(bb, g, hg, NSA_QBLOCK, n_top * SEL_BLOCK), m_sel).reshape(shp)
        o_sel = jnp.einsum('bghqnk,bgqnkd->bghqd', p_sel.astype(vsg.dtype), vsg)
        start = c * NSA_QBLOCK
        kwc = lax.dynamic_slice_in_dim(kw, start, NSA_QBLOCK + WINDOW, axis=2)
        vwc = lax.dynamic_slice_in_dim(vw, start, NSA_QBLOCK + WINDOW, axis=2)
        kpos = start - WINDOW + ar_win
        m_w = ((kpos[None, :] >= 0) & (kpos[None, :] <= tq[:, None])
               & (kpos[None, :] > tq[:, None] - WINDOW))
        s_w = jnp.einsum('bghqd,bgkd->bghqk', qr_c, kwc).astype(jnp.float32) * scale
        p_w = masked_softmax(s_w, m_w)
        o_w = jnp.einsum('bghqk,bgkd->bghqd', p_w.astype(vwc.dtype), vwc)
        return g_c[..., 0:1] * o_cmp + g_c[..., 1:2] * o_sel + g_c[..., 2:3] * o_w

    out = lax.map(block, (jnp.arange(t // NSA_QBLOCK), qn, qr, gt))
    return out.transpose(1, 0, 4, 2, 3, 5).reshape(bb, t, nh * dh)


def even_mixer(xn, w_in, w_out, conv_w, conv_b, w_r, b_r, w_i, b_i, lam,
               pos_k, w1_k, w2_k, pos_v, w1_v, w2_v, cos, sin):
    bb, t, _ = xn.shape
    parts = jnp.split(xn @ w_in, EVEN_OFFSETS, axis=-1)
    x_lru, g_lru, q, kc, vc, ks, vs, kw, vw, gl = parts
    y_lru = rg_lru(x_lru, g_lru, conv_w, conv_b, w_r, b_r, w_i, b_i, lam)

    def kvh(z):
        return z.reshape(bb, t, NSA_KV_HEADS, HEAD_DIM)

    y_nsa = nsa(q.reshape(bb, t, NSA_HEADS, HEAD_DIM), kvh(kc), kvh(vc), kvh(ks), kvh(vs),
                kvh(kw), kvh(vw), gl.reshape(bb, t, NSA_HEADS, 3), cos, sin,
                pos_k, w1_k, w2_k, pos_v, w1_v, w2_v)
    return jnp.concatenate([y_lru, y_nsa], axis=-1) @ w_out


def short_conv_mixer(xn, w_in, conv_w, w_out):
    b_g, c_g, v = jnp.split(xn @ w_in, 3, axis=-1)
    return (b_g * causal_dwconv(c_g * v, conv_w)) @ w_out


def memory_cross_attn(xn, mem, g_mem, wq, wk, wv, wo):
    bb, t, _ = xn.shape
    mn = rms_norm(mem, g_mem)
    q = (xn @ wq).reshape(bb, t, XA_HEADS, XA_HEAD_DIM)
    k = (mn @ wk).reshape(bb, -1, XA_HEADS, XA_HEAD_DIM)
    v = (mn @ wv).reshape(bb, -1, XA_HEADS, XA_HEAD_DIM)
    s = jnp.einsum('bthd,bmhd->bhtm', q, k).astype(jnp.float32) * (XA_HEAD_DIM ** -0.5)
    p = jax.nn.softmax(s, axis=-1).astype(v.dtype)
    o = jnp.einsum('bhtm,bmhd->bthd', p, v).reshape(bb, t, XA_HEADS * XA_HEAD_DIM)
    return o @ wo


def hier_moe(xn, w_group, b_group, w_expert, b_expert, w_gate, w_up, w_down):
    bb, t, d = xn.shape
    xt = xn.reshape(-1, d)
    n = xt.shape[0]
    g_prob = jax.nn.softmax((xt @ w_group).astype(jnp.float32) + b_group.astype(jnp.float32), axis=-1)
    g_top, g_idx = lax.top_k(g_prob, 1)
    e_logits = ((xt @ w_expert).astype(jnp.float32) + b_expert.astype(jnp.float32)).reshape(n, N_GROUPS, EXPERTS_PER_GROUP)
    e_in = e_logits[jnp.arange(n), g_idx[:, 0]]
    e_top, e_idx = lax.top_k(e_in, TOP_K)
    w = jax.nn.softmax(e_top, axis=-1) * g_top
    eid = g_idx * EXPERTS_PER_GROUP + e_idx
    gate = jnp.sum(jax.nn.one_hot(eid, N_EXPERTS, dtype=jnp.float32) * w[..., None], axis=1).astype(xn.dtype)
    y = jnp.zeros_like(xt)
    for e in range(N_EXPERTS):
        h = jax.nn.silu(xt @ w_gate[e]) * (xt @ w_up[e])
        y = y + gate[:, e:e + 1] * (h @ w_down[e])
    return y.reshape(bb, t, d)


def setup_inputs(seed: int = 0) -> dict:
    key = jax.random.key(seed)
    keys = iter(jax.random.split(key, 64))
    res = (3 * DEPTH) ** -0.5

    def nrm(shape, scale):
        return jax.random.normal(next(keys), shape, jnp.float32) * scale

    def gain(shape):
        return 1.0 + nrm(shape, 0.02)

    def lru_lambda():
        u = jax.random.uniform(next(keys), (N_EVEN, LRU_WIDTH), jnp.float32, 0.9, 0.999)
        a = u ** (1.0 / LRU_C)
        return jnp.log(a) - jnp.log1p(-a)

    offset = jax.random.randint(next(keys), (BATCH, 1), 0, 1024)
    positions = (offset + jnp.arange(SEQ)[None, :]).astype(jnp.int32)
    return {
        'x': nrm((BATCH, SEQ, D_MODEL), 1.0),
        'mem': nrm((BATCH, MEM_LEN, D_MODEL), 1.0),
        'positions': positions,
        'norm_mix': gain((DEPTH, D_MODEL)),
        'norm_xattn': gain((DEPTH, D_MODEL)),
        'norm_mem': gain((DEPTH, D_MODEL)),
        'norm_ffn': gain((DEPTH, D_MODEL)),
        'norm_final': gain((D_MODEL,)),
        'even_w_in': nrm((N_EVEN, D_MODEL, EVEN_PROJ), D_MODEL ** -0.5),
        'even_w_out': nrm((N_EVEN, MIX_WIDTH, D_MODEL), MIX_WIDTH ** -0.5 * res),
        'lru_conv_w': nrm((N_EVEN, LRU_CONV, LRU_WIDTH), LRU_CONV ** -0.5),
        'lru_conv_b': nrm((N_EVEN, LRU_WIDTH), 0.01),
        'lru_w_r': nrm((N_EVEN, LRU_BLOCKS, LRU_BLOCK_DIM, LRU_BLOCK_DIM), LRU_BLOCK_DIM ** -0.5),
        'lru_b_r': nrm((N_EVEN, LRU_WIDTH), 0.01),
        'lru_w_i': nrm((N_EVEN, LRU_BLOCKS, LRU_BLOCK_DIM, LRU_BLOCK_DIM), LRU_BLOCK_DIM ** -0.5),
        'lru_b_i': nrm((N_EVEN, LRU_WIDTH), 0.01),
        'lru_lambda': lru_lambda(),
        'nsa_cmp_pos_k': nrm((N_EVEN, CMP_LEN, HEAD_DIM), 0.02),
        'nsa_cmp_w1_k': nrm((N_EVEN, CMP_LEN * HEAD_DIM, CMP_HIDDEN), (CMP_LEN * HEAD_DIM) ** -0.5),
        'nsa_cmp_w2_k': nrm((N_EVEN, CMP_HIDDEN, HEAD_DIM), CMP_HIDDEN ** -0.5),
        'nsa_cmp_pos_v': nrm((N_EVEN, CMP_LEN, HEAD_DIM), 0.02),
        'nsa_cmp_w1_v': nrm((N_EVEN, CMP_LEN * HEAD_DIM, CMP_HIDDEN), (CMP_LEN * HEAD_DIM) ** -0.5),
        'nsa_cmp_w2_v': nrm((N_EVEN, CMP_HIDDEN, HEAD_DIM), CMP_HIDDEN ** -0.5),
        'odd_w_in': nrm((N_ODD, D_MODEL, 3 * SC_WIDTH), D_MODEL ** -0.5),
        'odd_conv_w': nrm((N_ODD, SC_CONV, SC_WIDTH), SC_CONV ** -0.5),
        'odd_w_out': nrm((N_ODD, SC_WIDTH, D_MODEL), SC_WIDTH ** -0.5 * res),
        'xa_wq': nrm((DEPTH, D_MODEL, XA_HEADS * XA_HEAD_DIM), D_MODEL ** -0.5),
        'xa_wk': nrm((DEPTH, D_MODEL, XA_HEADS * XA_HEAD_DIM), D_MODEL ** -0.5),
        'xa_wv': nrm((DEPTH, D_MODEL, XA_HEADS * XA_HEAD_DIM), D_MODEL ** -0.5),
        'xa_wo': nrm((DEPTH, XA_HEADS * XA_HEAD_DIM, D_MODEL), (XA_HEADS * XA_HEAD_DIM) ** -0.5 * res),
        'moe_w_group': nrm((DEPTH, D_MODEL, N_GROUPS), D_MODEL ** -0.5),
        'moe_b_group': nrm((DEPTH, N_GROUPS), 0.01),
        'moe_w_expert': nrm((DEPTH, D_MODEL, N_EXPERTS), D_MODEL ** -0.5),
        'moe_b_expert': nrm((DEPTH, N_EXPERTS), 0.01),
        'moe_w_gate': nrm((DEPTH, N_EXPERTS, D_MODEL, D_EXPERT), D_MODEL ** -0.5),
        'moe_w_up': nrm((DEPTH, N_EXPERTS, D_MODEL, D_EXPERT), D_MODEL ** -0.5),
        'moe_w_down': nrm((DEPTH, N_EXPERTS, D_EXPERT, D_MODEL), D_EXPERT ** -0.5 * res),
    }


def reference(x, mem, positions, norm_mix, norm_xattn, norm_mem, norm_ffn, norm_final,
              even_w_in, even_w_out, lru_conv_w, lru_conv_b, lru_w_r, lru_b_r, lru_w_i, lru_b_i,
              lru_lambda, nsa_cmp_pos_k, nsa_cmp_w1_k, nsa_cmp_w2_k, nsa_cmp_pos_v, nsa_cmp_w1_v,
              nsa_cmp_w2_v, odd_w_in, odd_conv_w, odd_w_out, xa_wq, xa_wk, xa_wv, xa_wo,
              moe_w_group, moe_b_group, moe_w_expert, moe_b_expert, moe_w_gate, moe_w_up, moe_w_down):
    cos, sin = rope_tables(positions)
    h = x
    for layer in range(DEPTH):
        xn = rms_norm(h, norm_mix[layer])
        if layer % 2 == 0:
            e = layer // 2
            h = h + even_mixer(xn, even_w_in[e], even_w_out[e], lru_conv_w[e], lru_conv_b[e],
                               lru_w_r[e], lru_b_r[e], lru_w_i[e], lru_b_i[e], lru_lambda[e],
                               nsa_cmp_pos_k[e], nsa_cmp_w1_k[e], nsa_cmp_w2_k[e],
                               nsa_cmp_pos_v[e], nsa_cmp_w1_v[e], nsa_cmp_w2_v[e], cos, sin)
        else:
            o = layer // 2
            h = h + short_conv_mixer(xn, odd_w_in[o], odd_conv_w[o], odd_w_out[o])
        h = h + memory_cross_attn(rms_norm(h, norm_xattn[layer]), mem, norm_mem[layer],
                                  xa_wq[layer], xa_wk[layer], xa_wv[layer], xa_wo[layer])
        h = h + hier_moe(rms_norm(h, norm_ffn[layer]), moe_w_group[layer], moe_b_group[layer],
                         moe_w_expert[layer], moe_b_expert[layer], moe_w_gate[layer],
                         moe_w_up[layer], moe_w_down[layer])
    return rms_norm(h, norm_final)
```

```python
import contextlib
import math
import numpy as np
import concourse.bass as bass
import concourse.mybir as mybir
from concourse.bass_utils import run_bass_kernel_spmd

F32 = mybir.dt.float32
BF16 = mybir.dt.bfloat16
I32 = mybir.dt.int32
ALU = mybir.AluOpType
AF = mybir.ActivationFunctionType
AX = mybir.AxisListType

D = 1024
T = 4096
NCORES = 8
DEPTH = 4
EPS = 1e-6
ENGS = ("sync", "gpsimd", "scalar", "vector", "tensor")
NS_DMA = 8
NSLOT_T = 11
NSLOT = NSLOT_T * 512
XSW = 1024 + 8


class Op:
    __slots__ = ("eng", "fn", "deps", "marked", "is_dma", "dma_idx", "semval", "gid")


class Builder:
    def __init__(self, nc):
        self.nc = nc
        self.q = {e: [] for e in ENGS}
        self.lastw = {}
        self.readers = {}
        self.ndma = {e: 0 for e in ENGS}
        self.stack = contextlib.ExitStack()
        self.stacks = [self.stack]
        self.uid = 0
        self.gid = 0

    def sb(self, shape, dtype, name=None):
        self.uid += 1
        return self.stacks[-1].enter_context(self.nc.sbuf_tensor(f"{name or 'sb'}_{self.uid}", list(shape), dtype))

    def ps(self, shape, dtype=F32, name=None):
        self.uid += 1
        return self.stacks[-1].enter_context(self.nc.psum_tensor(f"{name or 'ps'}_{self.uid}", list(shape), dtype))

    @contextlib.contextmanager
    def scope(self):
        st = contextlib.ExitStack()
        self.stacks.append(st)
        try:
            yield
        finally:
            self.barrier()
            self.stacks.pop()
            st.close()

    def dram(self, name, shape, dtype, kind="Internal"):
        return self.nc.dram_tensor(name, list(shape), dtype, kind=kind).ap()

    def op(self, eng, fn, reads=(), writes=(), dma=False):
        o = Op()
        o.eng = eng
        o.fn = fn
        o.marked = False
        o.is_dma = dma
        o.dma_idx = -1
        o.semval = 0
        self.gid += 1
        o.gid = self.gid
        psr = [r for r in reads if isinstance(r, tuple) and len(r) == 2 and r[0] == "ps"]
        if psr:
            reads = [r for r in reads if r not in psr]
            writes = list(writes) + psr
        deps = {}
        for r in reads:
            w = self.lastw.get(r)
            if w is not None:
                deps[id(w)] = w
        for k in writes:
            w = self.lastw.get(k)
            if w is not None:
                deps[id(w)] = w
            rd = self.readers.get(k)
            if rd:
                for x in rd[0].values():
                    deps[id(x)] = x
                for x in rd[1]:
                    deps[id(x)] = x
        o.deps = []
        for d in deps.values():
            if d is o:
                continue
            if eng == "tensor" and d.eng == "tensor" and not d.is_dma and not dma:
                continue
            d.marked = True
            o.deps.append(d)
        for r in reads:
            rd = self.readers.get(r)
            if rd is None:
                rd = ({}, [])
                self.readers[r] = rd
            if dma:
                rd[1].append(o)
            else:
                rd[0][eng] = o
        for k in writes:
            self.lastw[k] = o
            self.readers[k] = ({}, [])
        if dma:
            o.dma_idx = self.ndma[eng]
            self.ndma[eng] += 1
        self.q[eng].append(o)
        return o

    def dma(self, eng, out, in_, reads=(), writes=()):
        return self.op(eng, lambda e: e.dma_start(out=out, in_=in_), reads, writes, dma=True)

    def barrier(self):
        key = ("__bar__", self.gid)
        allk = list(self.lastw.keys())
        rk = [k for k, v in self.readers.items() if v[0] or v[1]]
        keys = list(set(allk) | set(rk))
        first = self.op("vector", lambda e: None, reads=(), writes=keys + [key])
        for e in ENGS:
            if e != "vector":
                self.op(e, lambda e_: None, reads=[key], writes=())
        self.lastw = {key: first}
        self.readers = {}

    def emit(self):
        nc = self.nc
        st = self.stack
        esem = {e: st.enter_context(nc.semaphore(f"es_{e}")) for e in ENGS}
        dsem = {e: [st.enter_context(nc.semaphore(f"ds_{e}{i}")) for i in range(NS_DMA)]
                for e in ENGS if self.ndma[e] > 0}
        for e in ENGS:
            c = 0
            for o in self.q[e]:
                if o.is_dma:
                    continue
                if o.marked:
                    c += 1
                    o.semval = c
                else:
                    o.semval = c

        def semref(d):
            if d.is_dma:
                k = d.dma_idx % NS_DMA
                return ("d", d.eng, k), dsem[d.eng][k], 16 * (d.dma_idx // NS_DMA + 1)
            return ("e", d.eng), esem[d.eng], d.semval

        def run(ename, eng):
            waited = {}
            pending_inc = 0
            for o in self.q[ename]:
                for d in o.deps:
                    key, sem, val = semref(d)
                    if waited.get(key, 0) >= val:
                        continue
                    eng.wait_ge(sem, val)
                    waited[key] = val
                if o.is_dma:
                    k = o.dma_idx % NS_DMA
                    prev = 16 * (o.dma_idx // NS_DMA)
                    key = ("d", ename, k)
                    if prev > 0 and waited.get(key, 0) < prev:
                        eng.wait_ge(dsem[ename][k], prev)
                        waited[key] = prev
                    ins = o.fn(eng)
                    ins.then_inc(dsem[ename][k], 16)
                else:
                    ins = o.fn(eng)
                    if o.marked:
                        if ins is None:
                            ins = eng.nop()
                        ins.then_inc(esem[ename], 1)

        with nc.Block() as block:
            @block.sync
            def _(e):
                run("sync", e)

            @block.gpsimd
            def _(e):
                run("gpsimd", e)

            @block.scalar
            def _(e):
                run("scalar", e)

            @block.vector
            def _(e):
                run("vector", e)

            @block.tensor
            def _(e):
                run("tensor", e)
        st.close()


class Ctx:
    pass


def mm(b, out, lhsT, rhs, start, stop, reads, writes):
    return b.op("tensor", lambda e: e.matmul(out, lhsT, rhs, start=start, stop=stop), reads, writes)


def load_w(b, dst, src, key, eng="gpsimd"):
    return b.op(eng, lambda e: e.dma_start(out=dst, in_=src, max_dma_last_dim=4096), writes=[key], dma=True)


def load_w_kc(b, dst, src, key, nk):
    sv = src.rearrange("(c p) m -> p c m", p=128)
    for c in range(nk):
        load_w(b, dst[:, c, :], sv[:, c, :], (key, c))


def wkeys(key, nk):
    return [(key, c) for c in range(nk)]


def setup_consts(b, cx):
    cx.eps_col = b.sb([128, 1], F32, "eps_col")
    b.op("vector", lambda e: e.memset(cx.eps_col[:], EPS), writes=["eps"])
    cx.ones_f = b.sb([128, 128], F32, "ones_f")
    b.op("vector", lambda e: e.memset(cx.ones_f[:], 1.0), writes=["ones_f"])
    cx.ones_b = b.sb([128, 128], BF16, "ones_b")
    b.op("vector", lambda e: e.memset(cx.ones_b[:], 1.0), writes=["ones_b"])
    cx.ident_i = b.sb([128, 128], I32, "ident_i")
    cx.ident_f = b.sb([128, 128], F32, "ident_f")
    cx.ident_b = b.sb([128, 128], BF16, "ident_b")
    b.op("gpsimd", lambda e: e.iota(cx.ident_i[:], pattern=[[1, 128]], base=0, channel_multiplier=-1),
         writes=["ident_i"])
    b.op("vector", lambda e: e.tensor_single_scalar(out=cx.ident_f[:], in_=cx.ident_i[:], scalar=0.0, op=ALU.is_equal),
         reads=["ident_i"], writes=["ident_f"])
    b.op("vector", lambda e: e.tensor_copy(out=cx.ident_b[:], in_=cx.ident_f[:]), reads=["ident_f"], writes=["ident_b"])
    cx.sel_i = b.sb([16, 16, 128], I32, "sel_i")
    cx.sel_f = b.sb([16, 16, 128], F32, "sel_f")
    b.op("gpsimd", lambda e: e.iota(cx.sel_i[:], pattern=[[1, 16], [0, 128]], base=0, channel_multiplier=-1),
         writes=["sel_i"])
    b.op("vector", lambda e: e.tensor_single_scalar(out=cx.sel_f[:], in_=cx.sel_i[:], scalar=0.0, op=ALU.is_equal),
         reads=["sel_i"], writes=["sel_f"])
    cx.gains = b.sb([128, 4 * DEPTH + 1, 8], F32, "gains")
    for i, nm in enumerate(("norm_mix", "norm_xattn", "norm_mem", "norm_ffn")):
        src = cx.inp[nm].rearrange("l (c p) -> p l c", p=128)
        b.op("sync", lambda e, i=i, src=src: e.dma_start(
            out=cx.gains[:, i * DEPTH:(i + 1) * DEPTH, :], in_=src, allow_slow_non_contiguous=True),
            writes=[("gains", i)], dma=True)
    srcf = cx.inp["norm_final"].rearrange("(c p) -> p c", p=128)
    b.op("sync", lambda e: e.dma_start(out=cx.gains[:, 4 * DEPTH, :], in_=srcf, allow_slow_non_contiguous=True),
         writes=[("gains", 4)], dma=True)
    cx.psum = [b.ps([128, 512], F32, f"bank{i}") for i in range(8)]
    cx.psum_b = [p.bitcast(BF16) for p in cx.psum]
    cx.tri_f = b.sb([128, 128], F32, "tri_f")
    b.op("vector", lambda e: e.tensor_single_scalar(out=cx.tri_f[:], in_=cx.ident_i[:], scalar=0.0, op=ALU.is_ge),
         reads=["ident_i"], writes=["tri_f"])
    cx.thr_i = b.sb([128, 4, 8], I32, "thr_i")
    cx.thr512 = b.sb([128, 4, 8], F32, "thr512")
    b.op("gpsimd", lambda e: e.iota(cx.thr_i[:], pattern=[[0, 4], [512, 8]], base=0, channel_multiplier=0), writes=["thr_i"])
    b.op("vector", lambda e: e.tensor_copy(out=cx.thr512[:], in_=cx.thr_i[:]), reads=["thr_i"], writes=["thr512"])
    cx.k32_i = b.sb([128, 32], I32, "k32_i")
    cx.k32 = b.sb([128, 32], F32, "k32")
    b.op("gpsimd", lambda e: e.iota(cx.k32_i[:], pattern=[[128, 32]], base=0, channel_multiplier=1), writes=["k32_i"])
    b.op("vector", lambda e: e.tensor_copy(out=cx.k32[:], in_=cx.k32_i[:]), reads=["k32_i"], writes=["k32"])
    cx.tst_i = b.sb([128, NSLOT_T, 3], I32, "tst_i")
    cx.tstart = b.sb([128, NSLOT_T, 3], F32, "tstart")
    b.op("gpsimd", lambda e: e.iota(cx.tst_i[:], pattern=[[512, NSLOT_T], [0, 3]], base=0, channel_multiplier=0), writes=["tst_i"])
    b.op("vector", lambda e: e.tensor_copy(out=cx.tstart[:], in_=cx.tst_i[:]), reads=["tst_i"], writes=["tstart"])


def PS(cx, i):
    return cx.psum[i][:], ("ps", i)


def gain_col(cx, kind, layer, c):
    idx = 4 * DEPTH if kind == 4 else kind * DEPTH + layer
    return cx.gains[:, idx, c:c + 1]


def rmsnorm_tile(b, cx, ht, htk, N, kind, layer, outs, sq, sqk, bank, rstd, rstdk):
    ps, psk = PS(cx, bank)
    ps = ps[:, 0:N]
    b.op("scalar", lambda e: e.activation(out=sq, in_=ht, func=AF.Square), reads=[htk], writes=[sqk])
    for c in range(8):
        mm(b, ps, cx.ones_f[:], sq[:, c, :], c == 0, c == 7, reads=[sqk, "ones_f"], writes=[psk])
    b.op("scalar", lambda e: e.activation(out=rstd, in_=ps, func=AF.Sqrt, scale=1.0 / D, bias=cx.eps_col[:, 0:1]),
         reads=[psk, "eps"], writes=[rstdk])
    b.op("vector", lambda e: e.reciprocal(out=rstd, in_=rstd), reads=[rstdk], writes=[rstdk])
    for (o, ok) in outs:
        for c in range(8):
            b.op("vector", lambda e, o=o, c=c: e.scalar_tensor_tensor(
                out=o[:, c, :], in0=ht[:, c, :], scalar=gain_col(cx, kind, layer, c), in1=rstd,
                op0=ALU.mult, op1=ALU.mult),
                reads=[htk, rstdk, ("gains", kind)], writes=[ok])


def hview(ap):
    return ap.rearrange("(c p) t -> p c t", p=128)


def phase_final_norm(b, cx, h_in, hk, out_dram):
    NT = 512
    with b.scope():
        ht = [b.sb([128, 8, NT], F32, "fn_ht") for i in range(2)]
        sq = [b.sb([128, 8, NT], F32, "fn_sq") for i in range(2)]
        ot = [b.sb([128, 8, NT], F32, "fn_ot") for i in range(2)]
        rs = [b.sb([128, NT], F32, "fn_rs") for i in range(2)]
        hv = hview(h_in)
        ov = hview(out_dram)
        def ld(t):
            b.dma("sync", ht[t % 2][:], hv[:, :, t * NT:(t + 1) * NT], reads=[(hk, t)], writes=[("fn_ht", t % 2)])
        ld(0)
        for t in range(T // NT):
            s = t % 2
            if t + 1 < T // NT:
                ld(t + 1)
            rmsnorm_tile(b, cx, ht[s][:], ("fn_ht", s), NT, 4, 0, [(ot[s][:], ("fn_ot", s))],
                         sq[s][:], ("fn_sq", s), s, rs[s][:], ("fn_rs", s))
            b.dma("sync", ov[:, :, t * NT:(t + 1) * NT], ot[s][:], reads=[("fn_ot", s)], writes=[("out", t)])


def phase_odd_mixer(b, cx, layer, h_in, hk_in, h_out, hk_out):
    o = layer // 2
    NT = 512
    with b.scope():
        w_in = b.sb([128, 8, 3072], BF16, "od_win")
        w_out = b.sb([128, 8, 1024], BF16, "od_wout")
        cw = b.sb([128, 8, 3], F32, "od_cw")
        load_w_kc(b, w_in, cx.inp["odd_w_in"][o], "od_win", 8)
        load_w_kc(b, w_out, cx.inp["odd_w_out"][o], "od_wout", 8)
        for k in range(3):
            b.op("sync", lambda e, k=k: e.dma_start(out=cw[:, :, k], in_=cx.inp["odd_conv_w"][o, k].rearrange("(c p) -> p c", p=128),
                                                    allow_slow_non_contiguous=True), writes=["od_cw"], dma=True)
        ht = [b.sb([128, 8, NT], F32, "od_ht") for i in range(2)]
        sq = b.sb([128, 8, NT], F32, "od_sq")
        rs = b.sb([128, NT], F32, "od_rs")
        xn = [b.sb([128, 8, NT], BF16, "od_xn") for i in range(2)]
        z = [b.sb([128, 8, NT + 2], F32, "od_z") for i in range(2)]
        csb = [b.sb([128, NT], F32, "od_c") for i in range(2)]
        acc = [b.sb([128, NT], F32, "od_acc") for i in range(2)]
        uT = b.sb([128, 8, NT], BF16, "od_uT")
        b.op("gpsimd", lambda e: e.memset(z[0][:, :, 0:2], 0.0), writes=[("od_zh", 0)])
        hv_in = hview(h_in)
        hv_out = hview(h_out)
        WK = wkeys("od_win", 8)
        WOK = wkeys("od_wout", 8)
        def ld(t):
            b.dma("sync", ht[t % 2][:], hv_in[:, :, t * NT:(t + 1) * NT], reads=[(hk_in, t)], writes=[("od_ht", t % 2)])
        ld(0)
        for t in range(T // NT):
            s = t % 2
            if t + 1 < T // NT:
                ld(t + 1)
            if t == 0:
                rmsnorm_tile(b, cx, ht[0][:], ("od_ht", 0), NT, 0, layer, [(xn[0][:], ("od_xn", 0))],
                             sq[:], "od_sq", 0, rs[:], "od_rs")
            for c in range(8):
                a = c % 2
                banks = [1 + 3 * a, 2 + 3 * a, 3 + 3 * a]
                pss = []
                for j in range(3):
                    ps, psk = PS(cx, banks[j])
                    col = j * 1024 + c * 128
                    for kc in range(8):
                        mm(b, ps, w_in[:, kc, col:col + 128], xn[s][:, kc, :], kc == 0, kc == 7,
                           reads=[("od_xn", s), ("od_win", kc)], writes=[psk])
                    pss.append((ps, psk))
                (pb, pbk), (pc, pck), (pv, pvk) = pss
                b.op("scalar", lambda e, a=a, pc=pc: e.copy(out=csb[a][:], in_=pc), reads=[pck], writes=[("od_c", a)])
                b.op("vector", lambda e, a=a, pv=pv, c=c, s=s: e.tensor_tensor(
                    out=z[s][:, c, 2:2 + NT], in0=pv, in1=csb[a][:], op=ALU.mult),
                    reads=[pvk, ("od_c", a)], writes=[("od_z", s, c)])
                b.op("vector", lambda e, a=a, c=c, s=s: e.tensor_scalar(
                    out=acc[a][:], in0=z[s][:, c, 2:2 + NT], scalar1=cw[:, c, 2:3], scalar2=None, op0=ALU.mult),
                    reads=[("od_z", s, c), "od_cw"], writes=[("od_acc", a)])
                for k in (1, 0):
                    b.op("vector", lambda e, a=a, c=c, s=s, k=k: e.scalar_tensor_tensor(
                        out=acc[a][:], in0=z[s][:, c, k:k + NT], scalar=cw[:, c, k:k + 1], in1=acc[a][:],
                        op0=ALU.mult, op1=ALU.add),
                        reads=[("od_z", s, c), ("od_zh", s), "od_cw", ("od_acc", a)], writes=[("od_acc", a)])
                b.op("vector", lambda e, a=a, c=c, pb=pb: e.tensor_tensor(
                    out=uT[:, c, :], in0=pb, in1=acc[a][:], op=ALU.mult),
                    reads=[pbk, ("od_acc", a)], writes=[("od_uT", c)])
            if t + 1 < T // NT:
                rmsnorm_tile(b, cx, ht[1 - s][:], ("od_ht", 1 - s), NT, 0, layer, [(xn[1 - s][:], ("od_xn", 1 - s))],
                             sq[:], "od_sq", 0, rs[:], "od_rs")
            b.op("gpsimd", lambda e, s=s: e.tensor_copy(out=z[1 - s][:, :, 0:2], in_=z[s][:, :, NT:NT + 2]),
                 reads=[("od_z", s, c) for c in range(8)], writes=[("od_zh", 1 - s)])
            for mc in range(8):
                ps, psk = PS(cx, 1 + (mc % 2) * 3)
                for kc in range(8):
                    mm(b, ps, w_out[:, kc, mc * 128:(mc + 1) * 128], uT[:, kc, :], kc == 0, kc == 7,
                       reads=[("od_uT", kc), ("od_wout", kc)], writes=[psk])
                b.op("vector", lambda e, mc=mc, ps=ps, s=s: e.tensor_tensor(
                    out=ht[s][:, mc, :], in0=ps, in1=ht[s][:, mc, :], op=ALU.add),
                    reads=[psk, ("od_ht", s)], writes=[("od_ht", s)])
            b.dma("sync", hv_out[:, :, t * NT:(t + 1) * NT], ht[s][:], reads=[("od_ht", s)], writes=[(hk_out, t)])


def phase_xattn(b, cx, layer, h_in, hk_in, h_out, hk_out):
    NT = 512
    with b.scope():
        wq = b.sb([128, 8, 1024], BF16, "xa_wq")
        wo = b.sb([128, 8, 1024], BF16, "xa_wo")
        KT = b.sb([128, 8, 256], BF16, "xa_KT")
        V = b.sb([128, 2, 1024], BF16, "xa_V")
        ht = [b.sb([128, 8, NT], F32, "xa_ht") for i in range(3)]
        hv_in = hview(h_in)
        hv_out = hview(h_out)

        def ld(t):
            b.dma("sync", ht[t % 3][:], hv_in[:, :, t * NT:(t + 1) * NT], reads=[(hk_in, t)], writes=[("xa_ht", t % 3)])
        with b.scope():
            wk = b.sb([128, 8, 1024], BF16, "xa_wk")
            wv = b.sb([128, 8, 1024], BF16, "xa_wv")
            memT = b.sb([128, 8, 256], F32, "xa_memT")
            mn = b.sb([128, 8, 256], BF16, "xa_mn")
            sqm = b.sb([128, 8, 256], F32, "xa_sqm")
            rsm = b.sb([128, 256], F32, "xa_rsm")
            for w, nm in ((wk, "xa_wk"), (wv, "xa_wv"), (wq, "xa_wq"), (wo, "xa_wo")):
                load_w_kc(b, w, cx.inp[nm][layer], nm, 8)
            b.dma("sync", memT[:], hview(cx.inp["memT"]), writes=["xa_memT"])
            ld(0)
            ld(1)
            rmsnorm_tile(b, cx, memT[:], "xa_memT", 256, 2, layer, [(mn[:], "xa_mn")],
                         sqm[:], "xa_sqm", 0, rsm[:], "xa_rsm")
            for mc in range(8):
                ps, psk = PS(cx, 1 + mc % 2)
                ps = ps[:, 0:256]
                for kc in range(8):
                    mm(b, ps, wk[:, kc, mc * 128:(mc + 1) * 128], mn[:, kc, :], kc == 0, kc == 7,
                       reads=["xa_mn", ("xa_wk", kc)], writes=[psk])
                b.op("scalar", lambda e, mc=mc, ps=ps: e.copy(out=KT[:, mc, :], in_=ps), reads=[psk], writes=["xa_KT"])
            for mch in range(2):
                for half in range(2):
                    ps, psk = PS(cx, 1 + half)
                    for kc in range(8):
                        mm(b, ps, mn[:, kc, mch * 128:(mch + 1) * 128], wv[:, kc, half * 512:(half + 1) * 512],
                           kc == 0, kc == 7, reads=["xa_mn", ("xa_wv", kc)], writes=[psk])
                    b.op("scalar", lambda e, mch=mch, half=half, ps=ps: e.copy(
                        out=V[:, mch, half * 512:(half + 1) * 512], in_=ps), reads=[psk], writes=["xa_V"])
        sq = b.sb([128, 8, NT], F32, "xa_sq")
        rs = b.sb([128, NT], F32, "xa_rs")
        xn = [b.sb([128, 8, NT], BF16, "xa_xn") for i in range(2)]
        qT = b.sb([128, 8, NT], BF16, "xa_qT")
        PT = [b.sb([128, 2, NT], BF16, "xa_PT") for i in range(2)]
        rden = [b.sb([128, NT], F32, "xa_rden") for i in range(2)]
        oT = b.sb([128, 8, NT], BF16, "xa_oT")
        scale = 1.0 / 16.0
        def nrm(t):
            s_ = t % 2
            rmsnorm_tile(b, cx, ht[t % 3][:], ("xa_ht", t % 3), NT, 1, layer, [(xn[s_][:], ("xa_xn", s_))],
                         sq[:], "xa_sq", 0, rs[:], "xa_rs")
        nrm(0)
        for t in range(T // NT):
            s = t % 2
            s3 = t % 3
            if t + 2 < T // NT:
                ld(t + 2)
            for mc in range(8):
                ps, psk = PS(cx, 1 + mc % 2)
                for kc in range(8):
                    mm(b, ps, wq[:, kc, mc * 128:(mc + 1) * 128], xn[s][:, kc, :], kc == 0, kc == 7,
                       reads=[("xa_xn", s), ("xa_wq", kc)], writes=[psk])
                b.op("scalar", lambda e, mc=mc, ps=ps: e.copy(out=qT[:, mc, :], in_=ps), reads=[psk], writes=[("xa_qT", mc)])
            if t + 1 < T // NT:
                nrm(t + 1)
            for h in range(4):
                a = h % 2
                for mch in range(2):
                    ps, psk = PS(cx, 3 + mch)
                    for dc in range(2):
                        mm(b, ps, KT[:, 2 * h + dc, mch * 128:(mch + 1) * 128], qT[:, 2 * h + dc, :], dc == 0, dc == 1,
                           reads=["xa_KT", ("xa_qT", 2 * h + dc)], writes=[psk])
                    b.op("scalar", lambda e, a=a, mch=mch, ps=ps: e.activation(
                        out=PT[a][:, mch, :], in_=ps, func=AF.Exp, scale=scale), reads=[psk], writes=[("xa_PT", a, mch)])
                ps, psk = PS(cx, 5)
                for mch in range(2):
                    mm(b, ps, cx.ones_b[:], PT[a][:, mch, :], mch == 0, mch == 1,
                       reads=[("xa_PT", a, mch), "ones_b"], writes=[psk])
                b.op("vector", lambda e, a=a, ps=ps: e.reciprocal(out=rden[a][:], in_=ps), reads=[psk], writes=[("xa_rden", a)])
                for dc in range(2):
                    ps, psk = PS(cx, 6 + dc)
                    for mch in range(2):
                        col = h * 256 + dc * 128
                        mm(b, ps, V[:, mch, col:col + 128], PT[a][:, mch, :], mch == 0, mch == 1,
                           reads=[("xa_PT", a, mch), "xa_V"], writes=[psk])
                    b.op("vector", lambda e, a=a, h=h, dc=dc, ps=ps: e.tensor_tensor(
                        out=oT[:, 2 * h + dc, :], in0=ps, in1=rden[a][:], op=ALU.mult),
                        reads=[psk, ("xa_rden", a)], writes=[("xa_oT", 2 * h + dc)])
            for mc in range(8):
                ps, psk = PS(cx, 1 + mc % 2)
                for kc in range(8):
                    mm(b, ps, wo[:, kc, mc * 128:(mc + 1) * 128], oT[:, kc, :], kc == 0, kc == 7,
                       reads=[("xa_oT", kc), ("xa_wo", kc)], writes=[psk])
                b.op("vector", lambda e, mc=mc, ps=ps, s3=s3: e.tensor_tensor(
                    out=ht[s3][:, mc, :], in0=ps, in1=ht[s3][:, mc, :], op=ALU.add),
                    reads=[psk, ("xa_ht", s3)], writes=[("xa_ht", s3)])
            b.dma("sync", hv_out[:, :, t * NT:(t + 1) * NT], ht[s3][:], reads=[("xa_ht", s3)], writes=[(hk_out, t)])


def bc(ap, shape, axis):
    return ap.unsqueeze(axis).to_broadcast(list(shape))


INPUT_NAMES = ["x", "mem", "positions", "norm_mix", "norm_xattn", "norm_mem", "norm_ffn", "norm_final",
               "even_w_in", "even_w_out", "lru_conv_w", "lru_conv_b", "lru_w_r", "lru_b_r", "lru_w_i", "lru_b_i",
               "lru_lambda", "nsa_cmp_pos_k", "nsa_cmp_w1_k", "nsa_cmp_w2_k", "nsa_cmp_pos_v", "nsa_cmp_w1_v",
               "nsa_cmp_w2_v", "odd_w_in", "odd_conv_w", "odd_w_out", "xa_wq", "xa_wk", "xa_wv", "xa_wo",
               "moe_w_group", "moe_b_group", "moe_w_expert", "moe_b_expert", "moe_w_gate", "moe_w_up", "moe_w_down"]


def build_program(shapes, phases):
    nc = bass.Bass("TRN2", target_bir_lowering=False)
    b = Builder(nc)
    cx = Ctx()
    cx.nc = nc
    cx.inp = {}
    for nm, (shape, dt) in shapes.items():
        cx.inp[nm] = nc.dram_tensor(nm, list(shape), dt, kind="ExternalInput").ap()
    cx.out = nc.dram_tensor("out", [D, T], F32, kind="ExternalOutput").ap()
    setup_consts(b, cx)
    phases(b, cx)
    b.barrier()
    b.emit()
    return nc


NEG = -1e30
FORCE = 1e9
BIGM = 30000.0
GELU_K = 2.0 * math.sqrt(2.0 / math.pi)


_FILL_REGS = {}


def asel(e, **kw):
    f = float(kw["fill"])
    key = (id(e), f)
    if key not in _FILL_REGS:
        _FILL_REGS[key] = e.to_reg(f)
    kw["fill"] = _FILL_REGS[key]
    return e.affine_select(**kw)


def scratch(cx, name, shape, dtype):
    kind = "ExternalOutput" if getattr(cx, "debug", False) else "Internal"
    t = cx.nc.dram_tensor(name, list(shape), dtype, kind=kind).ap()
    return t


def gelu_ops(b, src, srck, out, outk, tA, tAk, tB, tBk):
    b.op("scalar", lambda e: e.activation(out=tA, in_=src, func=AF.Square), reads=[srck], writes=[tAk])
    b.op("vector", lambda e: e.tensor_scalar(out=tA, in0=tA, scalar1=0.044715, scalar2=1.0, op0=ALU.mult, op1=ALU.add),
         reads=[tAk], writes=[tAk])
    b.op("vector", lambda e: e.tensor_tensor(out=tA, in0=src, in1=tA, op=ALU.mult), reads=[srck, tAk], writes=[tAk])
    b.op("scalar", lambda e: e.activation(out=tB, in_=tA, func=AF.Sigmoid, scale=GELU_K), reads=[tAk], writes=[tBk])
    b.op("vector", lambda e: e.tensor_tensor(out=out, in0=src, in1=tB, op=ALU.mult), reads=[srck, tBk], writes=[outk])


def phase_rope_tables(b, cx):
    cx.ropeC = scratch(cx, "ropeC", [128, T], F32)
    cx.ropeS = scratch(cx, "ropeS", [128, T], F32)
    TWO_PI = 2.0 * math.pi
    C1 = 6.28125
    C2 = TWO_PI - C1
    with b.scope():
        pi_ = b.sb([128, T], I32, "rp_pi")
        ang = b.sb([128, T], F32, "rp_ang")
        tmp = b.sb([128, T], F32, "rp_tmp")
        ki = b.sb([128, T], I32, "rp_ki")
        kf = b.sb([128, T], F32, "rp_kf")
        r = b.sb([128, T], F32, "rp_r")
        m = b.sb([128, T], F32, "rp_m")
        inv = b.sb([128, 1], F32, "rp_inv")
        b.dma("sync", inv[:], cx.inp["rope_inv"], writes=["rp_inv"])
        b.op("sync", lambda e: e.dma_start(out=pi_[:], in_=cx.inp["pos"].partition_broadcast(128)), writes=["rp_pi"], dma=True)
        b.op("vector", lambda e: e.tensor_copy(out=ang[:], in_=pi_[:]), reads=["rp_pi"], writes=["rp_ang"])
        b.op("vector", lambda e: e.tensor_scalar(out=ang[:], in0=ang[:], scalar1=inv[:, 0:1], scalar2=None, op0=ALU.mult),
             reads=["rp_ang", "rp_inv"], writes=["rp_ang"])
        for which, off, dst in (("s", 0.0, cx.ropeS), ("c", math.pi / 2.0, cx.ropeC)):
            b.op("vector", lambda e, off=off: e.tensor_scalar(out=r[:], in0=ang[:], scalar1=off, scalar2=None, op0=ALU.add),
                 reads=["rp_ang", "rp_r"], writes=["rp_r"])
            b.op("vector", lambda e: e.tensor_scalar(out=tmp[:], in0=r[:], scalar1=1.0 / TWO_PI, scalar2=None, op0=ALU.mult),
                 reads=["rp_r"], writes=["rp_tmp"])
            b.op("vector", lambda e: e.tensor_copy(out=ki[:], in_=tmp[:]), reads=["rp_tmp"], writes=["rp_ki"])
            b.op("vector", lambda e: e.tensor_copy(out=kf[:], in_=ki[:]), reads=["rp_ki"], writes=["rp_kf"])
            for cc in (C1, C2):
                b.op("vector", lambda e, cc=cc: e.scalar_tensor_tensor(out=r[:], in0=kf[:], scalar=-cc, in1=r[:],
                                                                       op0=ALU.mult, op1=ALU.add),
                     reads=["rp_kf", "rp_r"], writes=["rp_r"])
            b.op("vector", lambda e: e.tensor_single_scalar(out=m[:], in_=r[:], scalar=math.pi, op=ALU.is_gt),
                 reads=["rp_r"], writes=["rp_m"])
            b.op("vector", lambda e: e.scalar_tensor_tensor(out=r[:], in0=m[:], scalar=-TWO_PI, in1=r[:], op0=ALU.mult, op1=ALU.add),
                 reads=["rp_m", "rp_r"], writes=["rp_r"])
            b.op("vector", lambda e: e.tensor_single_scalar(out=m[:], in_=r[:], scalar=-math.pi, op=ALU.is_lt),
                 reads=["rp_r"], writes=["rp_m"])
            b.op("vector", lambda e: e.scalar_tensor_tensor(out=r[:], in0=m[:], scalar=TWO_PI, in1=r[:], op0=ALU.mult, op1=ALU.add),
                 reads=["rp_m", "rp_r"], writes=["rp_r"])
            b.op("vector", lambda e: e.tensor_scalar(out=r[:], in0=r[:], scalar1=math.pi, scalar2=-math.pi, op0=ALU.min, op1=ALU.max),
                 reads=["rp_r"], writes=["rp_r"])
            b.op("scalar", lambda e: e.activation(out=tmp[:], in_=r[:], func=AF.Sin), reads=["rp_r"], writes=["rp_tmp"])
            b.dma("sync", dst, tmp[:], reads=["rp_tmp"], writes=["rope_" + which])


def even_scratch(cx):
    if hasattr(cx, "ev"):
        return cx.ev
    ev = Ctx()
    ev.xl = scratch(cx, "ev_xl", [512, T], F32)
    ev.gg = scratch(cx, "ev_gg", [512, T], F32)
    ev.qn = scratch(cx, "ev_qn", [512, T], BF16)
    ev.qr = scratch(cx, "ev_qr", [512, T], BF16)
    ev.kc = scratch(cx, "ev_kc", [128, T], BF16)
    ev.vc = scratch(cx, "ev_vc", [128, T], BF16)
    ev.ks = scratch(cx, "ev_ks", [128, T], BF16)
    ev.kw = scratch(cx, "ev_kw", [128, T], BF16)
    ev.vs = scratch(cx, "ev_vs", [T, 128], BF16)
    ev.vw = scratch(cx, "ev_vw", [T, 128], BF16)
    ev.gt = scratch(cx, "ev_gt", [T, 24], F32)
    ev.ylru = scratch(cx, "ev_ylru", [512, T], BF16)
    ev.ynsa = scratch(cx, "ev_ynsa", [512, T], BF16)
    cx.ev = ev
    return ev


def phase_even_proj(b, cx, layer, h_in, hk_in, parts=('lru', 'gelu', 'q', 'k', 'kcvc', 'tok')):
    e_ = layer // 2
    ev = even_scratch(cx)
    NT = 512
    NP = 2328
    with b.scope():
        w_in = b.sb([128, 8, NP], BF16, "ev_win")
        w_rot = b.sb([128, 8, 768], BF16, "ev_wrot")
        load_w_kc(b, w_in, cx.inp["even_w_in"][e_], "ev_win", 8)
        b.op("gpsimd", lambda e: e.memset(w_rot[:], 0.0), writes=["ev_wrot"])
        for kc in range(8):
            for (dst0, src0, nh) in ((0, 1024, 8), (512, 1792, 2), (640, 2048, 2)):
                dv = w_rot[:, kc, dst0:dst0 + 64 * nh].rearrange("p (h d) -> p h d", d=64)
                sv = w_in[:, kc, src0:src0 + 64 * nh].rearrange("p (h d) -> p h d", d=64)
                b.op("scalar", lambda e, dv=dv, sv=sv: e.mul(out=dv[:, :, 0:8], in_=sv[:, :, 8:16], mul=-1.0),
                     reads=[("ev_win", kc), "ev_wrot"], writes=["ev_wrot"])
                b.op("scalar", lambda e, dv=dv, sv=sv: e.copy(out=dv[:, :, 8:16], in_=sv[:, :, 0:8]),
                     reads=[("ev_win", kc), "ev_wrot"], writes=["ev_wrot"])
        ht = [b.sb([128, 8, NT], F32, "ev_ht") for i in range(2)]
        sq = b.sb([128, 8, NT], F32, "ev_sq")
        rs = b.sb([128, NT], F32, "ev_rs")
        xn2 = [b.sb([128, 8, NT], BF16, "ev_xn") for i in range(2)]
        Ct = [b.sb([128, NT], F32, "ev_C") for i in range(2)]
        St = [b.sb([128, NT], F32, "ev_S") for i in range(2)]
        stf = [b.sb([128, NT], F32, "ev_stf") for i in range(2)]
        stb = [b.sb([128, NT], BF16, "ev_stb") for i in range(2)]
        tA = [b.sb([128, NT], F32, "ev_tA") for i in range(2)]
        tB = [b.sb([128, NT], F32, "ev_tB") for i in range(2)]
        vst = [b.sb([128, 2, 128], BF16, "ev_vst") for i in range(2)]
        gst = [b.sb([128, 24], F32, "ev_gst") for i in range(2)]
        hv_in = hview(h_in)
        WK = None
        cnt = {"f": 0, "b": 0}

        cur = {}

        def proj(ps, psk, wt, wkey, col, ncols=128):
            for kc in range(8):
                mm(b, ps[0:ncols, :] if ncols != 128 else ps, wt[:, kc, col:col + ncols], cur["xn"][:, kc, :], kc == 0, kc == 7,
                   reads=[cur["k"], wkey if wkey == "ev_wrot" else (wkey, kc)], writes=[psk])

        def nrm(t_):
            s_ = t_ % 2
            rmsnorm_tile(b, cx, ht[s_][:], ("ev_ht", s_), NT, 0, layer, [(xn2[s_][:], ("ev_xn", s_))], sq[:], "ev_sq", 0, rs[:], "ev_rs")

        def ld(t):
            s = t % 2
            tsl = slice(t * NT, (t + 1) * NT)
            b.dma("sync", ht[s][:], hv_in[:, :, tsl], reads=[(hk_in, t)], writes=[("ev_ht", s)])
            b.dma("sync", Ct[s][:], cx.ropeC[:, tsl], reads=["rope_c"], writes=[("ev_C", s)])
            b.dma("sync", St[s][:], cx.ropeS[:, tsl], reads=["rope_s"], writes=[("ev_S", s)])
        ld(0)
        for t in range(T // NT):
            s = t % 2
            tsl = slice(t * NT, (t + 1) * NT)
            if t + 1 < T // NT:
                ld(t + 1)
            if t == 0:
                nrm(0)
            cur["xn"], cur["k"] = xn2[s], ("ev_xn", s)
            xn = xn2[s]
            for c in (range(4) if 'lru' in parts else ()):
                a = cnt["f"] % 2; cnt["f"] += 1
                ps, psk = PS(cx, 1 + a)
                proj(ps, psk, w_in, "ev_win", c * 128)
                b.op("scalar", lambda e, a=a, ps=ps: e.copy(out=stf[a][:], in_=ps), reads=[psk], writes=[("ev_stf", a)])
                b.dma("sync", ev.xl[c * 128:(c + 1) * 128, tsl], stf[a][:], reads=[("ev_stf", a)], writes=[("ev_xl", c, t)])
            for c in (range(4) if 'gelu' in parts else ()):
                a = cnt["f"] % 2; cnt["f"] += 1
                ps, psk = PS(cx, 1 + a)
                proj(ps, psk, w_in, "ev_win", 512 + c * 128)
                gelu_ops(b, ps, psk, stf[a][:], ("ev_stf", a), tA[a][:], ("ev_tA", a), tB[a][:], ("ev_tB", a))
                b.dma("sync", ev.gg[c * 128:(c + 1) * 128, tsl], stf[a][:], reads=[("ev_stf", a)], writes=[("ev_gg", c, t)])
            if t + 1 < T // NT:
                nrm(t + 1)
            def roped(col, rcol, plain_dst, rope_dst, key):
                a = cnt["f"] % 2; cnt["f"] += 1
                ps, psk = PS(cx, 1 + a)
                pr, prk = PS(cx, 3 + a)
                proj(ps, psk, w_in, "ev_win", col)
                proj(pr, prk, w_rot, "ev_wrot", rcol)
                if plain_dst is not None:
                    a2 = cnt["b"] % 2; cnt["b"] += 1
                    b.op("scalar", lambda e, a2=a2, ps=ps: e.copy(out=stb[a2][:], in_=ps), reads=[psk], writes=[("ev_stb", a2)])
                    b.dma("sync", plain_dst, stb[a2][:], reads=[("ev_stb", a2)], writes=[(key, "p", t)])
                b.op("vector", lambda e, a=a, pr=pr, s=s: e.tensor_tensor(out=tA[a][:], in0=pr, in1=St[s][:], op=ALU.mult),
                     reads=[prk, ("ev_S", s)], writes=[("ev_tA", a)])
                b.op("vector", lambda e, a=a, ps=ps, s=s: e.tensor_tensor(out=tB[a][:], in0=ps, in1=Ct[s][:], op=ALU.mult),
                     reads=[psk, ("ev_C", s)], writes=[("ev_tB", a)])
                a2 = cnt["b"] % 2; cnt["b"] += 1
                b.op("vector", lambda e, a=a, a2=a2: e.tensor_tensor(out=stb[a2][:], in0=tA[a][:], in1=tB[a][:], op=ALU.add),
                     reads=[("ev_tA", a), ("ev_tB", a)], writes=[("ev_stb", a2)])
                b.dma("sync", rope_dst, stb[a2][:], reads=[("ev_stb", a2)], writes=[(key, "r", t)])

            for c in (range(4) if 'q' in parts else ()):
                roped(1024 + c * 128, c * 128, ev.qn[c * 128:(c + 1) * 128, tsl], ev.qr[c * 128:(c + 1) * 128, tsl], ("ev_q", c))
            if 'k' in parts:
                roped(1792, 512, None, ev.ks[:, tsl], "ev_ks")
                roped(2048, 640, None, ev.kw[:, tsl], "ev_kw")
            for (col, dst, key) in (((1536, ev.kc, "ev_kc"), (1664, ev.vc, "ev_vc")) if 'kcvc' in parts else ()):
                a = cnt["f"] % 2; cnt["f"] += 1
                ps, psk = PS(cx, 1 + a)
                proj(ps, psk, w_in, "ev_win", col)
                a2 = cnt["b"] % 2; cnt["b"] += 1
                b.op("scalar", lambda e, a2=a2, ps=ps: e.copy(out=stb[a2][:], in_=ps), reads=[psk], writes=[("ev_stb", a2)])
                b.dma("sync", dst[:, tsl], stb[a2][:], reads=[("ev_stb", a2)], writes=[(key, t)])
            for su in (range(4) if 'tok' in parts else ()):
                a = su % 2
                ps, psk = PS(cx, 5 + a)
                tok0 = t * NT + su * 128
                for kc in range(8):
                    mm(b, ps[:, 0:408], xn[:, kc, su * 128:(su + 1) * 128], w_in[:, kc, 1920:2328], kc == 0, kc == 7,
                       reads=[("ev_xn", s), ("ev_win", kc)], writes=[psk])
                b.op("scalar", lambda e, a=a, ps=ps: e.copy(out=vst[a][:, 0, :], in_=ps[:, 0:128]), reads=[psk], writes=[("ev_vst", a, 0)])
                b.op("scalar", lambda e, a=a, ps=ps: e.copy(out=vst[a][:, 1, :], in_=ps[:, 256:384]), reads=[psk], writes=[("ev_vst", a, 1)])
                b.op("scalar", lambda e, a=a, ps=ps: e.activation(out=gst[a][:], in_=ps[:, 384:408], func=AF.Sigmoid),
                     reads=[psk], writes=[("ev_gst", a)])
                b.dma("sync", ev.vs[tok0:tok0 + 128, :], vst[a][:, 0, :], reads=[("ev_vst", a, 0)], writes=[("ev_vs", t, su)])
                b.dma("sync", ev.vw[tok0:tok0 + 128, :], vst[a][:, 1, :], reads=[("ev_vst", a, 1)], writes=[("ev_vw", t, su)])
                b.dma("sync", ev.gt[tok0:tok0 + 128, :], gst[a][:], reads=[("ev_gst", a)], writes=[("ev_gt", t, su)])


def phase_even_lru(b, cx, layer, stop=9):
    e_ = layer // 2
    ev = even_scratch(cx)
    NT = 512
    with b.scope():
        cwv = b.sb([128, 4, 4], F32, "lr_cw")
        cb = b.sb([128, 4], F32, "lr_cb")
        br = b.sb([128, 4], F32, "lr_br")
        bi = b.sb([128, 4], F32, "lr_bi")
        lam = b.sb([128, 4], F32, "lr_lam")
        sp = b.sb([128, 4], F32, "lr_sp")
        n8 = b.sb([128, 4], F32, "lr_n8")
        n16 = b.sb([128, 4], F32, "lr_n16")
        Wr = b.sb([128, 4, 128], BF16, "lr_Wr")
        Wi = b.sb([128, 4, 128], BF16, "lr_Wi")
        for k in range(4):
            b.op("sync", lambda e, k=k: e.dma_start(out=cwv[:, :, k], in_=cx.inp["lru_conv_w"][e_, k].rearrange("(c p) -> p c", p=128),
                                                    allow_slow_non_contiguous=True), writes=["lr_cw"], dma=True)
        for (dst, nm, key) in ((cb, "lru_conv_b", "lr_cb"), (br, "lru_b_r", "lr_br"), (bi, "lru_b_i", "lr_bi"), (lam, "lru_lambda", "lr_lam")):
            b.op("sync", lambda e, dst=dst, nm=nm: e.dma_start(out=dst[:], in_=cx.inp[nm][e_].rearrange("(c p) -> p c", p=128),
                                                               allow_slow_non_contiguous=True), writes=[key], dma=True)
        Wst = b.sb([128, 2, 4, 128], F32, "lr_Wst")
        b.op("vector", lambda e: e.memset(Wst[:], 0.0), writes=["lr_Wst"])
        for wi_, nm in enumerate(("lru_w_r", "lru_w_i")):
            for blk in range(8):
                c, hb = blk // 2, blk % 2
                b.op("sync", lambda e, wi_=wi_, nm=nm, blk=blk, c=c, hb=hb: e.dma_start(
                    out=Wst[hb * 64:(hb + 1) * 64, wi_, c, hb * 64:(hb + 1) * 64], in_=cx.inp[nm][e_, blk]),
                    reads=[], writes=["lr_Wst"], dma=True)
        b.op("vector", lambda e: e.tensor_copy(out=Wr[:], in_=Wst[:, 0, :, :]), reads=["lr_Wst"], writes=["lr_Wr"])
        b.op("vector", lambda e: e.tensor_copy(out=Wi[:], in_=Wst[:, 1, :, :]), reads=["lr_Wst"], writes=["lr_Wi"])
        b.op("scalar", lambda e: e.activation(out=sp[:], in_=lam[:], func=AF.Exp, scale=-1.0), reads=["lr_lam"], writes=["lr_sp"])
        b.op("scalar", lambda e: e.activation(out=sp[:], in_=sp[:], func=AF.Ln, bias=cx.ones_f[:, 0:1]), reads=["lr_sp", "ones_f"], writes=["lr_sp"])
        b.op("vector", lambda e: e.tensor_scalar(out=n8[:], in0=sp[:], scalar1=-8.0, scalar2=None, op0=ALU.mult), reads=["lr_sp"], writes=["lr_n8"])
        b.op("vector", lambda e: e.tensor_scalar(out=n16[:], in0=sp[:], scalar1=-16.0, scalar2=None, op0=ALU.mult), reads=["lr_sp"], writes=["lr_n16"])
        xc = [b.sb([128, NT], F32, "lr_xc") for i in range(2)]
        xcb = [b.sb([128, NT], BF16, "lr_xcb") for i in range(2)]
        rr = [b.sb([128, NT], F32, "lr_r") for i in range(2)]
        ii = [b.sb([128, NT], F32, "lr_i") for i in range(2)]
        aa = [b.sb([128, NT], F32, "lr_a") for i in range(2)]
        a2 = [b.sb([128, NT], F32, "lr_a2") for i in range(2)]
        uu = [b.sb([128, NT], F32, "lr_u") for i in range(2)]
        hh = [b.sb([128, NT], F32, "lr_h") for i in range(2)]
        gg = [b.sb([128, NT], F32, "lr_gg") for i in range(3)]
        xp3 = [b.sb([128, NT + 3], F32, "lr_xp3") for i in range(3)]
        yb = [b.sb([128, NT], BF16, "lr_yb") for i in range(2)]
        nt = T // NT
        pairs = [(c, t) for c in range(4) for t in range(nt)] if stop >= 1 else []
        NPR = len(pairs)

        def ld(i):
            c, t = pairs[i]
            s3 = i % 3
            rows = slice(c * 128, (c + 1) * 128)
            tsl = slice(t * NT, (t + 1) * NT)
            b.dma("sync", xp3[s3][:, 3:3 + NT], ev.xl[rows, tsl], reads=[("ev_xl", c, t)], writes=[("lr_xp", s3)])
            b.dma("sync", gg[s3][:], ev.gg[rows, tsl], reads=[("ev_gg", c, t)], writes=[("lr_gg", s3)])

        def front(i):
            c, t = pairs[i]
            s = i % 2
            s3 = i % 3
            xq = xp3[s3]
            if t == 0:
                b.op("gpsimd", lambda e: e.memset(xq[:, 0:3], 0.0), writes=[("lr_xph", s3)])
            b.op("vector", lambda e: e.tensor_scalar(out=xc[s][:], in0=xq[:, 3:3 + NT], scalar1=cwv[:, c, 3:4],
                                                     scalar2=cb[:, c:c + 1], op0=ALU.mult, op1=ALU.add),
                 reads=[("lr_xp", s3), "lr_cw", "lr_cb"], writes=[("lr_xc", s)])
            for k in range(3):
                b.op("vector", lambda e, k=k: e.scalar_tensor_tensor(
                    out=xc[s][:], in0=xq[:, k:k + NT], scalar=cwv[:, c, k:k + 1], in1=xc[s][:], op0=ALU.mult, op1=ALU.add),
                    reads=[("lr_xp", s3), ("lr_xph", s3), "lr_cw", ("lr_xc", s)], writes=[("lr_xc", s)])
            if t + 1 < nt:
                xnx = xp3[(i + 1) % 3]
                b.op("gpsimd", lambda e: e.tensor_copy(out=xnx[:, 0:3], in_=xq[:, NT:NT + 3]),
                     reads=[("lr_xp", s3)], writes=[("lr_xph", (i + 1) % 3)])
            if stop < 2:
                return
            b.op("scalar", lambda e: e.copy(out=xcb[s][:], in_=xc[s][:]), reads=[("lr_xc", s)], writes=[("lr_xcb", s)])
            pr, prk = PS(cx, 1 + s)
            pi, pik = PS(cx, 3 + s)
            mm(b, pr, Wr[:, c, :], xcb[s][:], True, True, reads=[("lr_xcb", s), "lr_Wr"], writes=[prk])
            mm(b, pi, Wi[:, c, :], xcb[s][:], True, True, reads=[("lr_xcb", s), "lr_Wi"], writes=[pik])
            b.op("scalar", lambda e: e.activation(out=rr[s][:], in_=pr, func=AF.Sigmoid, bias=br[:, c:c + 1]),
                 reads=[prk, "lr_br"], writes=[("lr_r", s)])
            b.op("scalar", lambda e: e.activation(out=ii[s][:], in_=pi, func=AF.Sigmoid, bias=bi[:, c:c + 1]),
                 reads=[pik, "lr_bi"], writes=[("lr_i", s)])
            if stop < 3:
                return
            b.op("scalar", lambda e: e.activation(out=aa[s][:], in_=rr[s][:], func=AF.Exp, scale=n8[:, c:c + 1]),
                 reads=[("lr_r", s), "lr_n8"], writes=[("lr_a", s)])
            b.op("scalar", lambda e: e.activation(out=a2[s][:], in_=rr[s][:], func=AF.Exp, scale=n16[:, c:c + 1]),
                 reads=[("lr_r", s), "lr_n16"], writes=[("lr_a2", s)])
            b.op("scalar", lambda e: e.activation(out=a2[s][:], in_=a2[s][:], func=AF.Sqrt, scale=-1.0, bias=cx.ones_f[:, 0:1]),
                 reads=[("lr_a2", s), "ones_f"], writes=[("lr_a2", s)])

        def back(i):
            c, t = pairs[i]
            s = i % 2
            s3 = i % 3
            if stop < 3:
                return
            b.op("vector", lambda e: e.tensor_tensor(out=uu[s][:], in0=ii[s][:], in1=xc[s][:], op=ALU.mult),
                 reads=[("lr_i", s), ("lr_xc", s)], writes=[("lr_u", s)])
            b.op("vector", lambda e: e.tensor_tensor(out=uu[s][:], in0=uu[s][:], in1=a2[s][:], op=ALU.mult),
                 reads=[("lr_u", s), ("lr_a2", s)], writes=[("lr_u", s)])
            if stop < 4:
                return
            init = 0.0 if t == 0 else hh[1 - s][:, NT - 1:NT]
            b.op("vector", lambda e: e.tensor_tensor_scan(out=hh[s][:], data0=aa[s][:], data1=uu[s][:], initial=init,
                                                          op0=ALU.mult, op1=ALU.add),
                 reads=[("lr_a", s), ("lr_u", s), ("lr_h", 1 - s)], writes=[("lr_h", s)])
            b.op("vector", lambda e: e.tensor_tensor(out=yb[s][:], in0=hh[s][:], in1=gg[s3][:], op=ALU.mult),
                 reads=[("lr_h", s), ("lr_gg", s3)], writes=[("lr_yb", s)])
            b.dma("sync", ev.ylru[c * 128:(c + 1) * 128, t * NT:(t + 1) * NT], yb[s][:], reads=[("lr_yb", s)], writes=[("ev_ylru", c, t)])

        for i in range(min(2, NPR)):
            ld(i)
        if NPR:
            front(0)
        for i in range(NPR):
            if i + 2 < NPR:
                ld(i + 2)
            if i + 1 < NPR:
                front(i + 1)
            back(i)


def phase_even_attn(b, cx, layer, stop=9, nqt=None):
    e_ = layer // 2
    ev = even_scratch(cx)
    NT = 512
    NQT = T // NT
    NKT = T // 128
    NC_ = 255
    scale = 1.0 / 8.0
    with b.scope():
        KS2 = [b.sb([128, T], BF16, "at_KS2") for g in range(2)]
        KW2 = [b.sb([128, T], BF16, "at_KW2") for g in range(2)]
        KC2 = [b.sb([128, 256], BF16, "at_KC2") for g in range(2)]
        VS = [b.sb([128, NKT, 65], BF16, "at_VS") for g in range(2)]
        VW = [b.sb([128, NKT, 65], BF16, "at_VW") for g in range(2)]
        VC = [b.sb([128, 2, 129], BF16, "at_VC") for g in range(2)]
        GT = b.sb([128, T // 128, 24], F32, "at_GT")
        Eall = b.sb([128, NKT, 128], BF16, "at_E")
        for g in range(2):
            for hb in range(2):
                b.dma("sync", KS2[g][hb * 64:(hb + 1) * 64, :], ev.ks[g * 64:(g + 1) * 64, :],
                      reads=[("ev_ks", "r", t) for t in range(NQT)], writes=[("at_KS2", g)])
                b.dma("sync", KW2[g][hb * 64:(hb + 1) * 64, :], ev.kw[g * 64:(g + 1) * 64, :],
                      reads=[("ev_kw", "r", t) for t in range(NQT)], writes=[("at_KW2", g)])
            b.op("vector", lambda e, g=g: e.memset(VS[g][:, :, 64:65], 1.0), writes=[("at_VS", g)])
            b.op("vector", lambda e, g=g: e.memset(VW[g][:, :, 64:65], 1.0), writes=[("at_VW", g)])
            b.op("vector", lambda e, g=g: e.memset(VC[g][:], 0.0), writes=[("at_VC", g)])
            for kq in range(8):
                ksl = slice(kq * 4, (kq + 1) * 4)
                tok = slice(kq * 512, (kq + 1) * 512)
                b.op("sync", lambda e, g=g, ksl=ksl, tok=tok: e.dma_start(
                    out=VS[g][:, ksl, 0:64], in_=ev.vs[tok, g * 64:(g + 1) * 64].rearrange("(k p) d -> p k d", p=128)),
                    reads=[("ev_vs", kq, su) for su in range(4)] + [("at_VS", g)], writes=[("at_VS", g)], dma=True)
                b.op("sync", lambda e, g=g, ksl=ksl, tok=tok: e.dma_start(
                    out=VW[g][:, ksl, 0:64], in_=ev.vw[tok, g * 64:(g + 1) * 64].rearrange("(k p) d -> p k d", p=128)),
                    reads=[("ev_vw", kq, su) for su in range(4)] + [("at_VW", g)], writes=[("at_VW", g)], dma=True)
        for kq in range(8):
            b.op("sync", lambda e, kq=kq: e.dma_start(out=GT[:, kq * 4:(kq + 1) * 4, :],
                                                     in_=ev.gt[kq * 512:(kq + 1) * 512, :].rearrange("(k p) j -> p k j", p=128)),
                 reads=[("ev_gt", kq, su) for su in range(4)], writes=["at_GT"], dma=True)
        b.op("vector", lambda e: e.memset(Eall[:], BIGM), writes=["at_E"])
        for hb in range(2):
            rows = slice(hb * 64, (hb + 1) * 64)
            b.op("gpsimd", lambda e, rows=rows: asel(e, out=Eall[rows], in_=Eall[rows], pattern=[[128, NKT], [1, 128]], compare_op=ALU.is_ge,
                                                     fill=0.0, base=0, channel_multiplier=-64), reads=["at_E"], writes=["at_E"])
            b.op("gpsimd", lambda e, rows=rows: asel(e, out=Eall[rows], in_=Eall[rows], pattern=[[-128, NKT], [-1, 128]], compare_op=ALU.is_ge,
                                                     fill=0.0, base=63, channel_multiplier=64), reads=["at_E"], writes=["at_E"])
        with (b.scope() if stop >= 1 else contextlib.nullcontext()):
          if stop >= 1:
              w1 = b.sb([128, 32, 128], BF16, "cp_w1")
              w2k = b.sb([128, 128], BF16, "cp_w2k")
              w2v = b.sb([128, 64], BF16, "cp_w2v")
              posT = b.sb([128, 32], F32, "cp_posT")
              src = b.sb([128, T], BF16, "cp_src")
              sA = b.sb([128, 256, 16], BF16, "cp_sA")
              sB = b.sb([128, 256, 16], BF16, "cp_sB")
              hg = b.sb([128, 256], BF16, "cp_hg")
              tA = b.sb([128, 256], F32, "cp_tA")
              tB = b.sb([128, 256], F32, "cp_tB")
              cov = b.sb([128, 2, 64], BF16, "cp_cov")
              load_w(b, cov[:], cx.inp["cover"].rearrange("(k p) j -> p k j", p=128), "cp_cov")
              for kv in range(2):
                  sfx = "k" if kv == 0 else "v"
                  srcd = ev.kc if kv == 0 else ev.vc
                  srck = "ev_kc" if kv == 0 else "ev_vc"
                  for hb in range(2):
                      load_w(b, w1[hb * 64:(hb + 1) * 64, :, :], cx.inp["nsa_cmp_w1_" + sfx][e_].rearrange("(l d) j -> d l j", d=64), "cp_w1")
                      b.op("sync", lambda e, hb=hb, sfx=sfx: e.dma_start(out=posT[hb * 64:(hb + 1) * 64, :],
                                                                        in_=cx.inp["nsa_cmp_pos_" + sfx][e_].rearrange("l d -> d l"),
                                                                        allow_slow_non_contiguous=True), writes=["cp_posT"], dma=True)
                  if kv == 0:
                      load_w(b, w2k[:, 0:64], cx.inp["nsa_cmp_w2_k"][e_], "cp_w2k")
                      load_w(b, w2k[:, 64:128], cx.inp["nsa_cmp_w2_k"][e_], "cp_w2k")
                  else:
                      load_w(b, w2v[:], cx.inp["nsa_cmp_w2_v"][e_], "cp_w2v")
                  b.dma("sync", src[:], srcd, reads=[(srck, t) for t in range(NQT)], writes=["cp_src"])
                  sv = src[:].rearrange("p (n l) -> p n l", l=16)
                  b.op("vector", lambda e, sv=sv: e.tensor_tensor(out=sA[:], in0=sv, in1=bc(posT[:, 0:16], [128, 256, 16], 1), op=ALU.add),
                       reads=["cp_src", "cp_posT"], writes=["cp_sA"])
                  b.op("vector", lambda e, sv=sv: e.tensor_tensor(out=sB[:], in0=sv, in1=bc(posT[:, 16:32], [128, 256, 16], 1), op=ALU.add),
                       reads=["cp_src", "cp_posT"], writes=["cp_sB"])
                  for g in range(2):
                      rows = slice(g * 64, (g + 1) * 64)
                      ps, psk = PS(cx, 1 + g)
                      for l in range(32):
                          if l < 16:
                              rhs = sA[rows, 0:NC_, l]
                          else:
                              rhs = sB[rows, 1:NC_ + 1, l - 16]
                          mm(b, ps[:, 0:NC_], w1[rows, l, :], rhs, l == 0, l == 31, reads=["cp_w1", "cp_sA", "cp_sB"], writes=[psk])
                      gelu_ops(b, ps[:, 0:NC_], psk, hg[:, 0:NC_], "cp_hg", tA[:, 0:NC_], "cp_tA", tB[:, 0:NC_], "cp_tB")
                      if kv == 0:
                          p2, p2k = PS(cx, 3 + g)
                          mm(b, p2[:, 0:NC_], w2k[:], hg[:, 0:NC_], True, True, reads=["cp_hg", "cp_w2k"], writes=[p2k])
                          b.op("scalar", lambda e, g=g, p2=p2: e.copy(out=KC2[g][:, 0:NC_], in_=p2[:, 0:NC_]), reads=[p2k], writes=[("at_KC2", g)])
                      else:
                          for nt_ in range(2):
                              nn = 128 if nt_ == 0 else NC_ - 128
                              p2, p2k = PS(cx, 3 + nt_)
                              mm(b, p2[0:nn, 0:64], hg[:, nt_ * 128:nt_ * 128 + nn], w2v[:], True, True, reads=["cp_hg", "cp_w2v"], writes=[p2k])
                              b.op("scalar", lambda e, g=g, nt_=nt_, nn=nn, p2=p2: e.copy(out=VC[g][0:nn, nt_, 0:64], in_=p2[0:nn, 0:64]),
                                   reads=[p2k, ("at_VC", g)], writes=[("at_VC", g)])
                              b.op("vector", lambda e, g=g, nt_=nt_, nn=nn: e.tensor_copy(out=VC[g][0:nn, nt_, 64:128], in_=cov[0:nn, nt_, :]),
                                   reads=["cp_cov", ("at_VC", g)], writes=[("at_VC", g)])
                              b.op("vector", lambda e, g=g, nt_=nt_, nn=nn: e.memset(VC[g][0:nn, nt_, 128:129], 1.0),
                                   reads=[("at_VC", g)], writes=[("at_VC", g)])
        qn = [b.sb([128, 4, NT], BF16, "at_qn") for i in range(2)]
        qr = [b.sb([128, 4, NT], BF16, "at_qr") for i in range(2)]
        qrs = [b.sb([128, 4, NT], BF16, "at_qrs") for i in range(2)]
        PT = [b.sb([128, NKT, NT], BF16, "at_PT") for i in range(2)]
        yacc = b.sb([128, 4, 512], F32, "at_yacc")
        imp = b.sb([128, 4, 64], F32, "at_imp")
        rden = [b.sb([128, 1], F32, "at_rden") for i in range(4)]
        sc1 = b.sb([128, 4, 64], F32, "at_sc1")
        sc2 = b.sb([128, 4, 64], F32, "at_sc2")
        sc3 = b.sb([128, 4, 64], F32, "at_sc3")
        m8a = b.sb([128, 4, 8], F32, "at_m8a")
        m8b = b.sb([128, 4, 8], F32, "at_m8b")
        selm = b.sb([128, 4, 128], F32, "at_selm")
        selT = b.sb([128, NT], BF16, "at_selT")
        yT = [b.sb([128, 4, NT], BF16, "at_yT") for i in range(2)]
        pt_i = 0
        rd_i = 0
        pv_i = 0
        for qt in (range(NQT if nqt is None else nqt) if stop >= 2 else ()):
            s = qt % 2
            q0 = qt * NT
            qsl = slice(q0, q0 + NT)
            def ldq(qt_):
                s_ = qt_ % 2
                qsl_ = slice(qt_ * NT, (qt_ + 1) * NT)
                b.dma("sync", qn[s_][:], ev.qn.rearrange("(c p) t -> p c t", p=128)[:, :, qsl_],
                      reads=[(("ev_q", c), "p", qt_) for c in range(4)], writes=[("at_qn", s_)])
                b.dma("sync", qr[s_][:], ev.qr.rearrange("(c p) t -> p c t", p=128)[:, :, qsl_],
                      reads=[(("ev_q", c), "r", qt_) for c in range(4)], writes=[("at_qr", s_)])
                qsw = ev.qr.rearrange("(c h p) t -> h p c t", h=2, p=64)
                for hb_ in range(2):
                    b.dma("sync", qrs[s_][hb_ * 64:(hb_ + 1) * 64, :, :], qsw[1 - hb_][:, :, qsl_],
                          reads=[(("ev_q", c), "r", qt_) for c in range(4)], writes=[("at_qrs", s_, hb_)])
            nq_ = NQT if nqt is None else nqt
            if qt == 0:
                ldq(0)
            if qt + 1 < nq_:
                ldq(qt + 1)
            for g in range(2):
                for hh in range(4):
                    h = 4 * g + hh
                    c_, hb = h // 2, h % 2
                    prow = slice(hb * 64, (hb + 1) * 64)
                    P = PT[pt_i % 2]; pk = ("at_PT", pt_i % 2); pt_i += 1
                    for nt_ in range(2):
                        nn = 128 if nt_ == 0 else NC_ - 128
                        ps, psk = PS(cx, 1 + nt_)
                        mm(b, ps[0:nn, :], KC2[g][prow, nt_ * 128:nt_ * 128 + nn], qn[s][prow, c_, :], True, True,
                           reads=[("at_KC2", g), ("at_qn", s)], writes=[psk])
                        b.op("scalar", lambda e, P=P, nt_=nt_, nn=nn, ps=ps: e.activation(out=P[0:nn, nt_, :], in_=ps[0:nn, :], func=AF.Exp, scale=scale),
                             reads=[psk], writes=[(pk, nt_)])
                        b.op("gpsimd", lambda e, P=P, nt_=nt_, nn=nn, q0=q0: asel(e,
                            out=P[0:nn, nt_, :], in_=P[0:nn, nt_, :], pattern=[[1, NT]], compare_op=ALU.is_ge, fill=0.0,
                            base=q0 - 31 - 16 * 128 * nt_, channel_multiplier=-16), reads=[(pk, nt_)], writes=[(pk, nt_)])
                    for qs in range(4):
                        po, pok = PS(cx, 3 + pv_i % 2); pv_i += 1
                        for nt_ in range(2):
                            nn = 128 if nt_ == 0 else NC_ - 128
                            mm(b, po[:, 0:129], P[0:nn, nt_, qs * 128:(qs + 1) * 128], VC[g][0:nn, nt_, :], nt_ == 0, nt_ == 1,
                               reads=[(pk, 0), (pk, 1), ("at_VC", g)], writes=[pok])
                        rd = rden[rd_i % 4]; rdk = ("at_rden", rd_i % 4); rd_i += 1
                        b.op("vector", lambda e, rd=rd, po=po: e.tensor_scalar(out=rd[:], in0=po[:, 128:129], scalar1=1e-30, scalar2=None, op0=ALU.max),
                             reads=[pok], writes=[rdk])
                        b.op("vector", lambda e, rd=rd: e.reciprocal(out=rd[:], in_=rd[:]), reads=[rdk], writes=[rdk])
                        gi = qt * 4 + qs
                        b.op("vector", lambda e, rd=rd, po=po, qs=qs, h=h, gi=gi: e.tensor_scalar(
                            out=yacc[:, qs, h * 64:(h + 1) * 64], in0=po[:, 0:64], scalar1=rd[:, 0:1], scalar2=GT[:, gi, 3 * h:3 * h + 1],
                            op0=ALU.mult, op1=ALU.mult), reads=[pok, rdk, "at_GT"], writes=[("at_yacc", qs, h)])
                        if hh == 0:
                            b.op("vector", lambda e, rd=rd, po=po, qs=qs: e.tensor_scalar(
                                out=imp[:, qs, :], in0=po[:, 64:128], scalar1=rd[:, 0:1], scalar2=None, op0=ALU.mult),
                                reads=[pok, rdk], writes=[("at_imp", qs)])
                        else:
                            b.op("vector", lambda e, rd=rd, po=po, qs=qs: e.scalar_tensor_tensor(
                                out=imp[:, qs, :], in0=po[:, 64:128], scalar=rd[:, 0:1], in1=imp[:, qs, :], op0=ALU.mult, op1=ALU.add),
                                reads=[pok, rdk, ("at_imp", qs)], writes=[("at_imp", qs)])
                for qs in (range(4) if stop >= 3 else ()):
                    qq0 = q0 + qs * 128
                    b.op("gpsimd", lambda e, qs=qs, qq0=qq0: asel(e,
                        out=sc1[:, qs, :], in_=imp[:, qs, :], pattern=[[-64, 64]], compare_op=ALU.is_ge, fill=FORCE,
                        base=qq0 - 128, channel_multiplier=1), reads=[("at_imp", qs)], writes=[("at_sc1", qs)])
                    b.op("gpsimd", lambda e, qs=qs, qq0=qq0: asel(e,
                        out=sc2[:, qs, :], in_=sc1[:, qs, :], pattern=[[-64, 64]], compare_op=ALU.is_ge, fill=NEG,
                        base=qq0, channel_multiplier=1), reads=[("at_sc1", qs)], writes=[("at_sc2", qs)])
                    b.op("vector", lambda e, qs=qs: e.memset(sc2[:, qs, 0:1], FORCE), reads=[("at_sc2", qs)], writes=[("at_sc2", qs)])
                    b.op("vector", lambda e, qs=qs: e.max(out=m8a[:, qs, :], in_=sc2[:, qs, :]), reads=[("at_sc2", qs)], writes=[("at_m8a", qs)])
                    b.op("vector", lambda e, qs=qs: e.match_replace(out=sc3[:, qs, :], in_to_replace=m8a[:, qs, :], in_values=sc2[:, qs, :],
                                                                    imm_value=-3.0e38),
                         reads=[("at_sc2", qs), ("at_m8a", qs)], writes=[("at_sc3", qs)])
                    b.op("vector", lambda e, qs=qs: e.max(out=m8b[:, qs, :], in_=sc3[:, qs, :]), reads=[("at_sc3", qs)], writes=[("at_m8b", qs)])
                    for hb in range(2):
                        b.op("vector", lambda e, qs=qs, hb=hb: e.tensor_scalar(out=selm[:, qs, hb * 64:(hb + 1) * 64], in0=sc2[:, qs, :],
                                                                           scalar1=m8b[:, qs, 7:8], scalar2=-1.0, op0=ALU.is_ge, op1=ALU.add),
                             reads=[("at_sc2", qs), ("at_m8b", qs)], writes=[("at_selm", qs)])

                def emit_selT():
                    pst, pstk = PS(cx, 5)
                    for qs in range(4):
                        b.op("tensor", lambda e, qs=qs, pst=pst: e.transpose(pst[:, qs * 128:(qs + 1) * 128], selm[:, qs, :], cx.ident_f[:]),
                             reads=[("at_selm", qs), "ident_f"], writes=[pstk])
                    b.op("scalar", lambda e, pst=pst: e.copy(out=selT[:], in_=pst), reads=[pstk], writes=["at_selT"])

                def emit_qk(hh, br_):
                    nonlocal pt_i
                    h = 4 * g + hh
                    c_, hb = h // 2, h % 2
                    prow = slice(hb * 64, (hb + 1) * 64)
                    P = PT[pt_i % 2]; pk = ("at_PT", pt_i % 2); pt_i += 1
                    if br_ == 1:
                        kts = list(range(0, 4 * qt + 4))
                        Ksrc, Kkey = KS2[g], ("at_KS2", g)
                    else:
                        kts = list(range(max(0, 4 * qt - 4), 4 * qt + 4))
                        Ksrc, Kkey = KW2[g], ("at_KW2", g)
                    for kt in kts:
                        k0 = kt * 128
                        ps, psk = PS(cx, (1, 2, 6, 7)[kt % 4])
                        if kt % 2 == 0:
                            pr, qsrc, qkey = prow, qr[s], ("at_qr", s)
                        else:
                            pr, qsrc, qkey = slice((1 - hb) * 64, (2 - hb) * 64), qrs[s], ("at_qrs", s, 1 - hb)
                        mm(b, ps, Ksrc[pr, k0:k0 + 128], qsrc[pr, c_, :], True, br_ == 2,
                           reads=[Kkey, qkey], writes=[psk])
                        if br_ == 1:
                            mm(b, ps, Eall[pr, kt, :], selT[pr, :], False, True, reads=["at_E", "at_selT"], writes=[psk])
                        b.op("scalar", lambda e, P=P, kt=kt, ps=ps: e.activation(out=P[:, kt, :], in_=ps, func=AF.Exp, scale=scale),
                             reads=[psk], writes=[(pk, kt)])
                        if kt >= 4 * qt:
                            b.op("gpsimd", lambda e, P=P, kt=kt, k0=k0, q0=q0: asel(e,
                                out=P[:, kt, :], in_=P[:, kt, :], pattern=[[1, NT]], compare_op=ALU.is_ge, fill=0.0,
                                base=q0 - k0, channel_multiplier=-1), reads=[(pk, kt)], writes=[(pk, kt)])
                        elif br_ == 2:
                            b.op("gpsimd", lambda e, P=P, kt=kt, k0=k0, q0=q0: asel(e,
                                out=P[:, kt, :], in_=P[:, kt, :], pattern=[[-1, NT]], compare_op=ALU.is_ge, fill=0.0,
                                base=k0 - q0 + 511, channel_multiplier=1), reads=[(pk, kt)], writes=[(pk, kt)])
                    return (hh, br_, P, pk)

                def emit_pv(hh, br_, P, pk):
                    nonlocal pv_i, rd_i
                    h = 4 * g + hh
                    if br_ == 1:
                        Vsrc, Vkey = VS[g], ("at_VS", g)
                    else:
                        Vsrc, Vkey = VW[g], ("at_VW", g)
                    for qs in range(4):
                        hi = 4 * qt + qs
                        lo = 0 if br_ == 1 else max(0, hi - 4)
                        klist = list(range(lo, hi + 1))
                        po, pok = PS(cx, 3 + pv_i % 2); pv_i += 1
                        for i_, kt in enumerate(klist):
                            mm(b, po[:, 0:65], P[:, kt, qs * 128:(qs + 1) * 128], Vsrc[:, kt, :], i_ == 0, i_ == len(klist) - 1,
                               reads=[(pk, kt), Vkey], writes=[pok])
                        rd = rden[rd_i % 4]; rdk = ("at_rden", rd_i % 4); rd_i += 1
                        b.op("vector", lambda e, rd=rd, po=po: e.reciprocal(out=rd[:], in_=po[:, 64:65]), reads=[pok], writes=[rdk])
                        gi = qt * 4 + qs
                        b.op("vector", lambda e, rd=rd, po=po, qs=qs, h=h, gi=gi, br_=br_: e.tensor_scalar(
                            out=sc1[:, qs, :], in0=po[:, 0:64], scalar1=rd[:, 0:1], scalar2=GT[:, gi, 3 * h + br_:3 * h + br_ + 1],
                            op0=ALU.mult, op1=ALU.mult), reads=[pok, rdk, "at_GT"], writes=[("at_sc1", qs)])
                        b.op("vector", lambda e, qs=qs, h=h: e.tensor_tensor(
                            out=yacc[:, qs, h * 64:(h + 1) * 64], in0=yacc[:, qs, h * 64:(h + 1) * 64], in1=sc1[:, qs, :], op=ALU.add),
                            reads=[("at_sc1", qs), ("at_yacc", qs, h)], writes=[("at_yacc", qs, h)])

                if stop >= 4:
                    units = [(hh, 2) for hh in range(4)] + [(hh, 1) for hh in range(4)]
                    pend = None
                    for (hh, br_) in units:
                        if br_ == 1 and hh == 0:
                            emit_selT()
                        cur = emit_qk(hh, br_)
                        if pend is not None:
                            emit_pv(*pend)
                        pend = cur
                    emit_pv(*pend)
                elif stop >= 3:
                    emit_selT()
            if stop < 5:
                continue
            for fc in range(4):
                pt, ptk = PS(cx, 6 + fc % 2)
                for qs in range(4):
                    b.op("tensor", lambda e, fc=fc, qs=qs, pt=pt: e.transpose(pt[:, qs * 128:(qs + 1) * 128], yacc[:, qs, fc * 128:(fc + 1) * 128], cx.ident_f[:]),
                         reads=[("at_yacc", qs, 2 * fc), ("at_yacc", qs, 2 * fc + 1), "ident_f"], writes=[ptk])
                b.op("scalar", lambda e, fc=fc, pt=pt, s=s: e.copy(out=yT[s][:, fc, :], in_=pt), reads=[ptk], writes=[("at_yT", s, fc)])
            b.dma("sync", ev.ynsa.rearrange("(c p) t -> p c t", p=128)[:, :, qsl], yT[s][:],
                  reads=[("at_yT", s, fc) for fc in range(4)], writes=[("ev_ynsa", qt)])


def phase_even_out(b, cx, layer, h_in, hk_in, h_out, hk_out):
    e_ = layer // 2
    ev = even_scratch(cx)
    NT = 512
    with b.scope():
        w_out = b.sb([128, 8, 1024], BF16, "eo_w")
        load_w_kc(b, w_out, cx.inp["even_w_out"][e_], "eo_w", 8)
        ht = [b.sb([128, 8, NT], F32, "eo_ht") for i in range(2)]
        yy = [b.sb([128, 8, NT], BF16, "eo_y") for i in range(2)]
        hv_in = hview(h_in)
        hv_out = hview(h_out)
        def ld(t):
            s = t % 2
            tsl = slice(t * NT, (t + 1) * NT)
            b.dma("sync", ht[s][:], hv_in[:, :, tsl], reads=[(hk_in, t)], writes=[("eo_ht", s)])
            b.dma("sync", yy[s][:, 0:4, :], ev.ylru.rearrange("(c p) t -> p c t", p=128)[:, :, tsl],
                  reads=[("ev_ylru", c, t) for c in range(4)], writes=[("eo_y", s, 0)])
            b.dma("sync", yy[s][:, 4:8, :], ev.ynsa.rearrange("(c p) t -> p c t", p=128)[:, :, tsl],
                  reads=[("ev_ynsa", t)], writes=[("eo_y", s, 1)])
        ld(0)
        for t in range(T // NT):
            s = t % 2
            tsl = slice(t * NT, (t + 1) * NT)
            if t + 1 < T // NT:
                ld(t + 1)
            for mc in range(8):
                ps, psk = PS(cx, 1 + mc % 2)
                for kc in range(8):
                    mm(b, ps, w_out[:, kc, mc * 128:(mc + 1) * 128], yy[s][:, kc, :], kc == 0, kc == 7,
                       reads=[("eo_y", s, 0), ("eo_y", s, 1), ("eo_w", kc)], writes=[psk])
                b.op("vector", lambda e, mc=mc, ps=ps, s=s: e.tensor_tensor(out=ht[s][:, mc, :], in0=ps, in1=ht[s][:, mc, :], op=ALU.add),
                     reads=[psk, ("eo_ht", s)], writes=[("eo_ht", s)])
            b.dma("sync", hv_out[:, :, tsl], ht[s][:], reads=[("eo_ht", s)], writes=[(hk_out, t)])


def phase_even_mixer(b, cx, layer, h_in, hk_in, h_out, hk_out):
    phase_even_proj(b, cx, layer, h_in, hk_in)
    phase_even_lru(b, cx, layer)
    phase_even_attn(b, cx, layer)
    phase_even_out(b, cx, layer, h_in, hk_in, h_out, hk_out)


_BOUND_REGS = {}


def bound_reg(e):
    if id(e) not in _BOUND_REGS:
        _BOUND_REGS[id(e)] = e.to_reg(NSLOT - 1)
    return _BOUND_REGS[id(e)]


def wbound_reg(e, val):
    key = (id(e), "w", int(val))
    if key not in _BOUND_REGS:
        _BOUND_REGS[key] = e.to_reg(int(val))
    return _BOUND_REGS[key]


def moe_scratch(cx):
    if hasattr(cx, "mo"):
        return cx.mo
    mo = Ctx()
    mo.xs = scratch(cx, "mo_xs", [NSLOT, XSW], BF16)
    mo.ys = scratch(cx, "mo_ys", [NSLOT, 1024], F32)
    cx.mo = mo
    return mo


def phase_moe_init(b, cx):
    mo = moe_scratch(cx)
    with b.scope():
        zb = b.sb([128, 4, XSW], BF16, "mo_zb")
        b.op("vector", lambda e: e.memset(zb[:], 0.0), writes=["mo_zb"])
        for i in range(NSLOT_T):
            b.dma("sync", mo.xs[i * 512:(i + 1) * 512, :].rearrange("(k p) d -> p k d", p=128), zb[:],
                  reads=["mo_zb"], writes=[("mo_xs0", i)])


def phase_moe2(b, cx, layer, h_in, hk_in, h_out, hk_out):
    mo = moe_scratch(cx)
    NT = 512
    NSUB = 16
    V = "vector"
    hv_in = hview(h_in)
    hv_out = hview(h_out)
    with b.scope():
        dest_i = b.sb([128, 32], I32, "mo_dest")
        gid_i = b.sb([128, 16], I32, "mo_gid")
        widx = b.sb([128, NSLOT_T, 4], I32, "mo_widx")
        wgu = [b.sb([128, 8, 1024], BF16, "mo_wgu"), None]
        wd = [b.sb([128, 4, 1024], BF16, "mo_wd"), None]

        def load_expert(i, ex):
            ws = (i * 4 + ex) % 2
            for (wt, nm, key) in ((wgu[ws], "moe_w_gu", "mo_wgu"), (wd[ws], "moe_w_dn", "mo_wd")):
                wsrc = cx.inp[nm]
                b.op("gpsimd", lambda e, wt=wt, wsrc=wsrc: e.indirect_dma_start(
                    out=wt[:].rearrange("p c m -> p (c m)"), out_offset=None, in_=wsrc,
                    in_offset=bass.IndirectOffsetOnAxis(ap=widx[:, i, ex:ex + 1], axis=0),
                    bounds_check=wbound_reg(e, wsrc.shape[0] - 1), oob_is_err=False),
                    reads=["mo_widx"], writes=[(key, ws)], dma=True)

        with b.scope():
            xt_all = b.sb([128, 32, XSW], BF16, "mo_xt")
            xt_f = xt_all.bitcast(F32)
            G1 = b.sb([128, 32, 4], F32, "mo_G1")
            ht = [b.sb([128, 8, NT], F32, "mo_ht") for i in range(2)]
            xnf2 = [b.sb([128, 8, NT], F32, "mo_xnf") for i in range(2)]
            sq = b.sb([128, 8, NT], F32, "mo_sq")
            rs = b.sb([128, NT], F32, "mo_rs")
            wr = b.sb([128, 8, 20], F32, "mo_wr")
            bias = b.sb([128, 20], F32, "mo_bias")
            b.op("sync", lambda e: e.dma_start(out=wr[:, :, 0:4], in_=cx.inp["moe_w_group"][layer].rearrange(
                "(c p) g -> p c g", p=128), allow_slow_non_contiguous=True), writes=[("mo_wr", 0)], dma=True)
            b.op("sync", lambda e: e.dma_start(out=wr[:, :, 4:20], in_=cx.inp["moe_w_expert"][layer].rearrange(
                "(c p) g -> p c g", p=128), allow_slow_non_contiguous=True), writes=[("mo_wr", 1)], dma=True)
            b.op("sync", lambda e: e.dma_start(out=bias[:, 0:4], in_=cx.inp["moe_b_group"][layer:layer + 1, :]
                                               .partition_broadcast(128)), writes=[("mo_bias", 0)], dma=True)
            b.op("sync", lambda e: e.dma_start(out=bias[:, 4:20], in_=cx.inp["moe_b_expert"][layer:layer + 1, :]
                                               .partition_broadcast(128)), writes=[("mo_bias", 1)], dma=True)
            S3 = [128, NSUB, 4]
            S4 = [128, NSUB, 4, 4]
            Lb = b.sb([128, NSUB, 20], F32, "mo_Lb")
            gmax = b.sb([128, NSUB], F32, "mo_gmax")
            gsh = b.sb(S3, F32, "mo_gsh")
            gexp = b.sb(S3, F32, "mo_gexp")
            gsum = b.sb([128, NSUB], F32, "mo_gsum")
            gone = b.sb(S3, F32, "mo_gone")
            gcs = b.sb(S3, F32, "mo_gcs")
            m1 = b.sb(S3, F32, "mo_m1")
            m2 = b.sb(S3, F32, "mo_m2")
            one1 = b.sb(S4, F32, "mo_one1")
            one2 = b.sb(S4, F32, "mo_one2")
            E2 = b.sb(S4, F32, "mo_E2")
            dd = b.sb(S3, F32, "mo_dd")
            w1 = b.sb(S3, F32, "mo_w1")
            w2 = b.sb(S3, F32, "mo_w2")
            g4 = b.sb(S4, F32, "mo_g4")
            gate = b.sb([128, NSUB, 16], F32, "mo_gate")
            for hf in range(2):
                psr, psrk = PS(cx, 1)
                def nrm(t_):
                    s_ = t_ % 2
                    rmsnorm_tile(b, cx, ht[s_][:], ("mo_ht", s_), NT, 3, layer, [(xnf2[s_][:], ("mo_xnf", s_))],
                                 sq[:], "mo_sq", 0, rs[:], "mo_rs")
                if hf == 0:
                    b.dma("sync", ht[0][:], hv_in[:, :, 0:NT], reads=[(hk_in, 0)], writes=[("mo_ht", 0)])
                    b.dma("sync", ht[1][:], hv_in[:, :, NT:2 * NT], reads=[(hk_in, 1)], writes=[("mo_ht", 1)])
                    nrm(0)
                for tt in range(4):
                    t = hf * 4 + tt
                    s = t % 2
                    xnf = xnf2[s]
                    xnfk = ("mo_xnf", s)
                    if t + 1 < 8:
                        nrm(t + 1)
                    if t + 2 < 8:
                        b.dma("sync", ht[s][:], hv_in[:, :, (t + 2) * NT:(t + 3) * NT], reads=[(hk_in, t + 2)],
                              writes=[("mo_ht", s)])
                    for su in range(4):
                        sidx = tt * 4 + su
                        for kc in range(8):
                            mm(b, psr[:, sidx * 20:(sidx + 1) * 20], xnf[:, kc, su * 128:(su + 1) * 128], wr[:, kc, :],
                               kc == 0, kc == 7, reads=[xnfk, ("mo_wr", 0), ("mo_wr", 1)], writes=[psrk])
                    for su in range(4):
                        sg_ = t * 4 + su
                        for h2 in range(2):
                            ps, psk = PS(cx, 2 + h2)
                            for j in range(4):
                                fc = h2 * 4 + j
                                b.op("tensor", lambda e, ps=ps, j=j, fc=fc, su=su, xnf=xnf: e.transpose(
                                    ps[:, j * 128:(j + 1) * 128], xnf[:, fc, su * 128:(su + 1) * 128], cx.ident_f[:]),
                                    reads=[xnfk, "ident_f"], writes=[psk])
                            b.op("scalar", lambda e, ps=ps, sg_=sg_, h2=h2: e.copy(
                                out=xt_all[:, sg_, h2 * 512:(h2 + 1) * 512], in_=ps), reads=[psk], writes=[("mo_xt", sg_)])
                ssl = slice(hf * NSUB, (hf + 1) * NSUB)
                b.op(V, lambda e, psr=psr: e.tensor_tensor(out=Lb[:], in0=psr[:, 0:NSUB * 20].rearrange("p (s j) -> p s j", j=20),
                                                           in1=bc(bias[:], [128, NSUB, 20], 1), op=ALU.add),
                     reads=[psrk, ("mo_bias", 0), ("mo_bias", 1)], writes=["Lb"])
                G = Lb[:, :, 0:4]
                E = Lb[:, :, 4:20].rearrange("p s (g e) -> p s g e", g=4)
                b.op(V, lambda e, G=G: e.tensor_reduce(out=gmax[:], in_=G, axis=AX.X, op=ALU.max), reads=["Lb"], writes=["gmax"])
                b.op(V, lambda e, G=G: e.tensor_tensor(out=gsh[:], in0=G, in1=bc(gmax[:], S3, 2), op=ALU.subtract),
                     reads=["Lb", "gmax"], writes=["gsh"])
                b.op("scalar", lambda e: e.activation(out=gexp[:], in_=gsh[:], func=AF.Exp), reads=["gsh"], writes=["gexp"])
                b.op(V, lambda e: e.tensor_reduce(out=gsum[:], in_=gexp[:], axis=AX.X, op=ALU.add), reads=["gexp"], writes=["gsum"])
                b.op(V, lambda e: e.reciprocal(out=gsum[:], in_=gsum[:]), reads=["gsum"], writes=["gsum"])
                b.op(V, lambda e: e.tensor_single_scalar(out=gone[:], in_=gsh[:], scalar=0.0, op=ALU.is_equal),
                     reads=["gsh"], writes=["gone"])
                b.op(V, lambda e: e.tensor_copy(out=gcs[:, :, 0:1], in_=gone[:, :, 0:1]), reads=["gone"], writes=["gcs"])
                for g_ in range(1, 4):
                    b.op(V, lambda e, g_=g_: e.tensor_tensor(out=gcs[:, :, g_:g_ + 1], in0=gcs[:, :, g_ - 1:g_], in1=gone[:, :, g_:g_ + 1], op=ALU.add),
                         reads=["gone", "gcs"], writes=["gcs"])
                b.op(V, lambda e, ssl=ssl: e.scalar_tensor_tensor(out=G1[:, ssl, :], in0=gcs[:], scalar=1.0, in1=gone[:], op0=ALU.is_equal, op1=ALU.mult),
                     reads=["gone", "gcs"], writes=[("mo_G1", hf)])
                b.op(V, lambda e, ssl=ssl: e.tensor_tensor(out=gone[:], in0=G1[:, ssl, :], in1=bc(gsum[:], S3, 2), op=ALU.mult),
                     reads=[("mo_G1", hf), "gsum", "gone"], writes=["gone"])
                b.op(V, lambda e, E=E: e.tensor_reduce(out=m1[:], in_=E, axis=AX.X, op=ALU.max), reads=["Lb"], writes=["m1"])
                b.op(V, lambda e, E=E: e.tensor_tensor(out=one1[:], in0=E, in1=bc(m1[:], S4, 3), op=ALU.is_equal),
                     reads=["Lb", "m1"], writes=["one1"])
                b.op(V, lambda e: e.scalar_tensor_tensor(out=E2[:].rearrange("p s g e -> p s (g e)"),
                                                         in0=one1[:].rearrange("p s g e -> p s (g e)"), scalar=-1e30,
                                                         in1=Lb[:, :, 4:20], op0=ALU.mult, op1=ALU.add),
                     reads=["one1", "Lb"], writes=["E2"])
                b.op(V, lambda e: e.tensor_reduce(out=m2[:], in_=E2[:], axis=AX.X, op=ALU.max), reads=["E2"], writes=["m2"])
                b.op(V, lambda e: e.tensor_tensor(out=one2[:], in0=E2[:], in1=bc(m2[:], S4, 3), op=ALU.is_equal),
                     reads=["E2", "m2"], writes=["one2"])
                b.op(V, lambda e: e.tensor_tensor(out=dd[:], in0=m2[:], in1=m1[:], op=ALU.subtract), reads=["m1", "m2"], writes=["dd"])
                b.op("scalar", lambda e: e.activation(out=dd[:], in_=dd[:], func=AF.Exp), reads=["dd"], writes=["dd"])
                b.op(V, lambda e: e.tensor_scalar(out=w1[:], in0=dd[:], scalar1=1.0, scalar2=None, op0=ALU.add),
                     reads=["dd"], writes=["w1"])
                b.op(V, lambda e: e.reciprocal(out=w1[:], in_=w1[:]), reads=["w1"], writes=["w1"])
                b.op(V, lambda e: e.tensor_tensor(out=w2[:], in0=dd[:], in1=w1[:], op=ALU.mult), reads=["dd", "w1"], writes=["w2"])
                b.op(V, lambda e: e.tensor_tensor(out=w1[:], in0=w1[:], in1=gone[:], op=ALU.mult), reads=["w1", "gone"], writes=["w1"])
                b.op(V, lambda e: e.tensor_tensor(out=w2[:], in0=w2[:], in1=gone[:], op=ALU.mult), reads=["w2", "gone"], writes=["w2"])
                b.op(V, lambda e: e.tensor_tensor(out=g4[:], in0=one1[:], in1=bc(w1[:], S4, 3), op=ALU.mult),
                     reads=["one1", "w1"], writes=["g4"])
                b.op(V, lambda e: e.tensor_tensor(out=one2[:], in0=one2[:], in1=bc(w2[:], S4, 3), op=ALU.mult),
                     reads=["one2", "w2"], writes=["one2"])
                b.op(V, lambda e: e.tensor_tensor(out=gate[:].rearrange("p s (g e) -> p s g e", g=4), in0=g4[:], in1=one2[:], op=ALU.add),
                     reads=["g4", "one2"], writes=["gate"])
                b.op(V, lambda e, ssl=ssl: e.tensor_tensor(out=xt_f[:, ssl, 512:516], in0=gate[:, :, 0:4], in1=gate[:, :, 4:8], op=ALU.add),
                     reads=["gate"], writes=[("mo_gate4", hf)])
                for g_ in (2, 3):
                    b.op(V, lambda e, ssl=ssl, g_=g_: e.tensor_tensor(out=xt_f[:, ssl, 512:516], in0=xt_f[:, ssl, 512:516], in1=gate[:, :, 4 * g_:4 * g_ + 4], op=ALU.add),
                         reads=["gate", ("mo_gate4", hf)], writes=[("mo_gate4", hf)])
            G1K = [("mo_G1", 0), ("mo_G1", 1)]
            pp, ppk = PS(cx, 4)
            G1f = G1[:].rearrange("p s g -> p (s g)")
            mm(b, pp[:, 0:128], cx.tri_f[:], G1f, True, True, reads=G1K + ["tri_f"], writes=[ppk])
            mm(b, pp[:, 128:256], cx.ones_f[:], G1f, True, True, reads=G1K + ["ones_f"], writes=[ppk])
            rank = b.sb([128, 32, 4], F32, "mo_rank")
            tot = b.sb([128, 32, 4], F32, "mo_tot")
            scA = b.sb([128, 32, 4], F32, "mo_scA")
            scB = b.sb([128, 32, 4], F32, "mo_scB")
            b.op(V, lambda e: e.tensor_copy(out=rank[:].rearrange("p s g -> p (s g)"), in_=pp[:, 0:128]), reads=[ppk], writes=["mo_rank"])
            b.op(V, lambda e: e.tensor_copy(out=tot[:].rearrange("p s g -> p (s g)"), in_=pp[:, 128:256]), reads=[ppk], writes=["mo_tot"])
            cur, curk = tot, "mo_tot"
            pingpong = [(scA, "mo_scA"), (scB, "mo_scB")]
            for i_, d_ in enumerate((1, 2, 4, 8, 16)):
                nxt, nxtk = pingpong[i_ % 2]
                b.op(V, lambda e, cur=cur, nxt=nxt, d_=d_: e.tensor_tensor(out=nxt[:, d_:, :], in0=cur[:, d_:, :], in1=cur[:, 0:32 - d_, :], op=ALU.add),
                     reads=[curk, nxtk], writes=[nxtk])
                b.op(V, lambda e, cur=cur, nxt=nxt, d_=d_: e.tensor_copy(out=nxt[:, 0:d_, :], in_=cur[:, 0:d_, :]),
                     reads=[curk, nxtk], writes=[nxtk])
                cur, curk = nxt, nxtk
            incl, inclk = cur, curk
            ngc = b.sb([128, 4, 8], F32, "mo_ngc")
            cnt = b.sb([128, 4], F32, "mo_cnt")
            base = b.sb([128, 4], F32, "mo_base")
            b.op(V, lambda e: e.tensor_tensor(out=ngc[:], in0=bc(incl[:, 31, :], [128, 4, 8], 2), in1=cx.thr512[:], op=ALU.is_gt),
                 reads=[inclk, "thr512"], writes=["mo_ngc"])
            b.op(V, lambda e: e.tensor_reduce(out=cnt[:], in_=ngc[:], axis=AX.X, op=ALU.add), reads=["mo_ngc"], writes=["mo_cnt"])
            b.op(V, lambda e: e.memset(base[:], 0.0), writes=["mo_base"])
            b.op(V, lambda e: e.tensor_scalar(out=base[:, 1:2], in0=cnt[:, 0:1], scalar1=512.0, scalar2=None, op0=ALU.mult),
                 reads=["mo_cnt", "mo_base"], writes=["mo_base"])
            for g_ in (2, 3):
                b.op(V, lambda e, g_=g_: e.scalar_tensor_tensor(out=base[:, g_:g_ + 1], in0=cnt[:, g_ - 1:g_], scalar=512.0, in1=base[:, g_ - 1:g_],
                                                                op0=ALU.mult, op1=ALU.add),
                     reads=["mo_cnt", "mo_base"], writes=["mo_base"])
            off = scB
            b.op(V, lambda e: e.tensor_tensor(out=off[:], in0=incl[:], in1=tot[:], op=ALU.subtract), reads=[inclk, "mo_tot", "mo_scB"], writes=["mo_scB"])
            b.op(V, lambda e: e.tensor_tensor(out=off[:], in0=off[:], in1=bc(base[:], [128, 32, 4], 1), op=ALU.add),
                 reads=["mo_scB", "mo_base"], writes=["mo_scB"])
            b.op(V, lambda e: e.tensor_tensor(out=off[:], in0=off[:], in1=rank[:], op=ALU.add), reads=["mo_scB", "mo_rank"], writes=["mo_scB"])
            b.op(V, lambda e: e.scalar_tensor_tensor(out=off[:], in0=off[:], scalar=-1.0, in1=G1[:], op0=ALU.add, op1=ALU.mult),
                 reads=["mo_scB"] + G1K, writes=["mo_scB"])
            dest_f = b.sb([128, 32], F32, "mo_destf")
            b.op(V, lambda e: e.tensor_reduce(out=dest_f[:], in_=off[:], axis=AX.X, op=ALU.add), reads=["mo_scB"], writes=["mo_destf"])
            b.op(V, lambda e: e.tensor_copy(out=dest_i[:], in_=dest_f[:]), reads=["mo_destf"], writes=["mo_dest"])
            gcm = b.sb([128, NSLOT_T, 3], F32, "mo_gcm")
            gid_f = b.sb([128, 16], F32, "mo_gidf")
            b.op(V, lambda e: e.memset(gid_f[:], 0.0), writes=["mo_gidf"])
            b.op(V, lambda e: e.tensor_tensor(out=gcm[:], in0=bc(base[:, 1:4], [128, NSLOT_T, 3], 1), in1=cx.tstart[:], op=ALU.is_le),
                 reads=["mo_base", "tstart"], writes=["mo_gcm"])
            b.op(V, lambda e: e.tensor_reduce(out=gid_f[:, 0:NSLOT_T], in_=gcm[:], axis=AX.X, op=ALU.add), reads=["mo_gcm", "mo_gidf"], writes=["mo_gidf"])
            b.op(V, lambda e: e.tensor_copy(out=gid_i[:], in_=gid_f[:]), reads=["mo_gidf"], writes=["mo_gid"])
            wif = b.sb([128, NSLOT_T, 4], F32, "mo_wif")
            b.op(V, lambda e: e.scalar_tensor_tensor(out=wif[:], in0=bc(gid_f[:, 0:NSLOT_T], [128, NSLOT_T, 4], 2), scalar=512.0,
                                                     in1=bc(cx.k32[:, 0:4], [128, NSLOT_T, 4], 1), op0=ALU.mult, op1=ALU.add),
                 reads=["mo_gidf", "k32"], writes=["mo_wif"])
            b.op(V, lambda e: e.tensor_scalar(out=wif[:], in0=wif[:], scalar1=float(layer * 16 * 128), scalar2=None, op0=ALU.add),
                 reads=["mo_wif"], writes=["mo_wif"])
            b.op(V, lambda e: e.tensor_copy(out=widx[:], in_=wif[:]), reads=["mo_wif"], writes=["mo_widx"])
            if getattr(cx, "debug", False):
                dbg_d = scratch(cx, f"dbg_dest{layer}", [128, 32], I32)
                dbg_g = scratch(cx, f"dbg_gid{layer}", [128, 16], I32)
                b.dma("sync", dbg_d, dest_i[:], reads=["mo_dest"], writes=["dbg_d"])
                b.dma("sync", dbg_g, gid_i[:], reads=["mo_gid"], writes=["dbg_g"])
            load_expert(0, 0)
            for s_ in range(32):
                b.op("gpsimd", lambda e, s_=s_: e.indirect_dma_start(
                    out=mo.xs[:, :], out_offset=bass.IndirectOffsetOnAxis(ap=dest_i[:, s_:s_ + 1], axis=0),
                    in_=xt_all[:, s_, :], in_offset=None, bounds_check=bound_reg(e), oob_is_err=False),
                    reads=["mo_dest", ("mo_xt", s_), ("mo_gate4", s_ // NSUB)] + [("mo_xs0", i) for i in range(NSLOT_T)],
                    writes=[("mo_xs", s_)], dma=True)
        XSK = [("mo_xs", s_) for s_ in range(32)]
        with b.scope():
            wgu[1] = b.sb([128, 8, 1024], BF16, "mo_wgu")
            wd[1] = b.sb([128, 4, 1024], BF16, "mo_wd")
            sg = [b.sb([128, NT], F32, "mo_sg") for i in range(2)]
            t1 = [b.sb([128, NT], F32, "mo_t1") for i in range(2)]
            hT = [b.sb([128, 4, NT], BF16, "mo_hT") for i in range(2)]
            xtk = [b.sb([128, 4, XSW], BF16, "mo_xtk") for i in range(2)]
            xtk_f = [x_.bitcast(F32) for x_ in xtk]
            xsT = [b.sb([128, 8, NT], BF16, "mo_xsT") for i in range(2)]
            gT = [b.sb([4, NT], F32, "mo_gT") for i in range(2)]
            ytk = [b.sb([128, 4, 1024], F32, "mo_ytk") for i in range(2)]
            hold = {}

            def load_tile(i):
                s2 = i % 2
                b.dma("sync", xtk[s2][:], mo.xs[i * 512:(i + 1) * 512, :].rearrange("(k p) d -> p k d", p=128),
                      reads=XSK, writes=[("mo_xtk", s2)])

            load_tile(0)
            for i in range(NSLOT_T):
                s2 = i % 2
                if i + 1 < NSLOT_T:
                    load_tile(i + 1)
                for fc in range(8):
                    pb_, pbk = cx.psum_b[5 + fc % 2], ("ps", 5 + fc % 2)
                    for k in range(4):
                        b.op("tensor", lambda e, pb_=pb_, k=k, fc=fc, s2=s2: e.transpose(
                            pb_[:, k * 128:(k + 1) * 128], xtk[s2][:, k, fc * 128:(fc + 1) * 128], cx.ident_b[:]),
                            reads=[("mo_xtk", s2), "ident_b"], writes=[pbk])
                    b.op("scalar", lambda e, pb_=pb_, fc=fc, s2=s2: e.copy(out=xsT[s2][:, fc, :], in_=pb_[:, 0:512]),
                         reads=[pbk], writes=[("mo_xsT", s2, fc)])
                pgt, pgtk = PS(cx, 7)
                for k in range(4):
                    b.op("tensor", lambda e, pgt=pgt, k=k, s2=s2: e.transpose(
                        pgt[0:4, k * 128:(k + 1) * 128], xtk_f[s2][:, k, 512:516], cx.ident_f[:]),
                        reads=[("mo_xtk", s2), "ident_f"], writes=[pgtk])
                b.op("scalar", lambda e, pgt=pgt, s2=s2: e.copy(out=gT[s2][:], in_=pgt[0:4, :]), reads=[pgtk], writes=[("mo_gT", s2)])
                XK = [("mo_xsT", s2, fc) for fc in range(8)]
                for ex in range(4):
                    u = i * 4 + ex
                    ws = u % 2
                    hs = u % 2
                    if ex + 1 < 4:
                        load_expert(i, ex + 1)
                    elif i + 1 < NSLOT_T:
                        load_expert(i + 1, 0)
                    pgb, pgbk = PS(cx, 7)
                    mm(b, pgb, cx.sel_f[0:4, ex, :], gT[s2][:], True, True, reads=[("mo_gT", s2), "sel_f"], writes=[pgbk])
                    for mc in range(4):
                        a = mc % 2
                        pg, pgk = PS(cx, 1 + a)
                        pu, puk = PS(cx, 3 + a)
                        for kc in range(8):
                            mm(b, pg, wgu[ws][:, kc, mc * 128:(mc + 1) * 128], xsT[s2][:, kc, :], kc == 0, kc == 7,
                               reads=XK + [("mo_wgu", ws)], writes=[pgk])
                        for kc in range(8):
                            mm(b, pu, wgu[ws][:, kc, 512 + mc * 128:512 + (mc + 1) * 128], xsT[s2][:, kc, :], kc == 0, kc == 7,
                               reads=XK + [("mo_wgu", ws)], writes=[puk])
                        b.op("scalar", lambda e, a=a, pg=pg: e.activation(out=sg[a][:], in_=pg, func=AF.Silu),
                             reads=[pgk], writes=[("mo_sg", a)])
                        b.op("vector", lambda e, a=a, pgb=pgb: e.tensor_tensor(out=t1[a][:], in0=pgb, in1=sg[a][:], op=ALU.mult),
                             reads=[pgbk, ("mo_sg", a)], writes=[("mo_t1", a)])
                        b.op("vector", lambda e, a=a, hs=hs, mc=mc, pu=pu: e.tensor_tensor(
                            out=hT[hs][:, mc, :], in0=pu, in1=t1[a][:], op=ALU.mult),
                            reads=[puk, ("mo_t1", a)], writes=[("mo_hT", hs, mc)])
                    for k in range(4):
                        for h2 in range(2):
                            j_ = k * 2 + h2
                            py, pyk = PS(cx, (5, 6, 0)[j_ % 3])
                            for kc in range(4):
                                mm(b, py, hT[hs][:, kc, k * 128:(k + 1) * 128], wd[ws][:, kc, h2 * 512:(h2 + 1) * 512], kc == 0, kc == 3,
                                   reads=[("mo_hT", hs, kc), ("mo_wd", ws)], writes=[pyk])
                            ysl = ytk[s2][:, k, h2 * 512:(h2 + 1) * 512]
                            if ex == 0:
                                b.op("scalar", lambda e, py=py, ysl=ysl: e.copy(out=ysl, in_=py),
                                     reads=[pyk], writes=[("mo_ytk", s2, j_)])
                            else:
                                b.op("vector", lambda e, py=py, ysl=ysl: e.tensor_tensor(out=ysl, in0=py, in1=ysl, op=ALU.add),
                                     reads=[pyk, ("mo_ytk", s2, j_)], writes=[("mo_ytk", s2, j_)])
                b.dma("sync", mo.ys[i * 512:(i + 1) * 512, :].rearrange("(k p) d -> p k d", p=128), ytk[s2][:],
                      reads=[("mo_ytk", s2, j_) for j_ in range(8)], writes=[("mo_ys", i)])
        YSK = [("mo_ys", i) for i in range(NSLOT_T)]
        with b.scope():
            ht = [b.sb([128, 8, NT], F32, "mo_hc") for i in range(2)]
            yg = [b.sb([128, 4, 1024], F32, "mo_yg") for i in range(2)]
            def ldc(t):
                s = t % 2
                b.dma("sync", ht[s][:], hv_in[:, :, t * NT:(t + 1) * NT], reads=[(hk_in, t)], writes=[("mo_hc", s)])
                for su in range(4):
                    s_ = t * 4 + su
                    b.op("gpsimd", lambda e, s=s, su=su, s_=s_: e.indirect_dma_start(
                        out=yg[s][:, su, :], out_offset=None, in_=mo.ys[:, :],
                        in_offset=bass.IndirectOffsetOnAxis(ap=dest_i[:, s_:s_ + 1], axis=0),
                        bounds_check=bound_reg(e), oob_is_err=False),
                        reads=["mo_dest"] + YSK, writes=[("mo_yg", s, su)], dma=True)
            ldc(0)
            for t in range(T // NT):
                s = t % 2
                if t + 1 < T // NT:
                    ldc(t + 1)
                for fc in range(8):
                    ps, psk = PS(cx, 1 + fc % 4)
                    for su in range(4):
                        b.op("tensor", lambda e, ps=ps, su=su, fc=fc, s=s: e.transpose(
                            ps[:, su * 128:(su + 1) * 128], yg[s][:, su, fc * 128:(fc + 1) * 128], cx.ident_f[:]),
                            reads=[("mo_yg", s, su), "ident_f"], writes=[psk])
                    b.op("vector", lambda e, ps=ps, fc=fc, s=s: e.tensor_tensor(out=ht[s][:, fc, :], in0=ps, in1=ht[s][:, fc, :], op=ALU.add),
                         reads=[psk, ("mo_hc", s)], writes=[("mo_hc", s)])
                b.dma("sync", hv_out[:, :, t * NT:(t + 1) * NT], ht[s][:], reads=[("mo_hc", s)], writes=[(hk_out, t)])


def host_consts():
    inv = (500000.0 ** (-(np.arange(0, 16, 2, dtype=np.float32)) / 16.0)).astype(np.float32)
    col = np.zeros((128, 1), np.float32)
    for p in range(128):
        d = p % 64
        if d < 16:
            col[p, 0] = inv[d % 8]
    n = np.arange(256)
    j = np.arange(64)
    cover = ((16 * n[:, None] <= 64 * j[None, :] + 63) & (16 * n[:, None] + 31 >= 64 * j[None, :])).astype(np.float32)
    cover[255:] = 0.0
    return {"rope_inv": col, "cover": cover}


WEIGHT_NAMES = [n for n in INPUT_NAMES if n not in ("x", "mem", "positions", "moe_w_gate", "moe_w_up", "moe_w_down")]


def host_moe_weights(inputs):
    wg = np.asarray(inputs["moe_w_gate"], dtype=np.float32)
    wu = np.asarray(inputs["moe_w_up"], dtype=np.float32)
    wd = np.asarray(inputs["moe_w_down"], dtype=np.float32)
    L, E = wg.shape[0], wg.shape[1]
    gu = np.concatenate([wg, wu], axis=-1).reshape(L, E, 8, 128, 1024).transpose(0, 1, 3, 2, 4)
    gu = np.ascontiguousarray(gu).reshape(L * E * 128, 8 * 1024)
    dn = np.ascontiguousarray(wd.reshape(L, E, 4, 128, 1024).transpose(0, 1, 3, 2, 4)).reshape(L * E * 128, 4 * 1024)
    return {"moe_w_gu": gu, "moe_w_dn": dn}


def full_phases(b, cx):
    hbuf = cx.nc.dram_tensor("hbuf", [D, T], F32, kind="Internal").ap()
    phase_rope_tables(b, cx)
    phase_moe_init(b, cx)
    for layer in range(DEPTH):
        h_in, hk_in = (cx.inp["xT"], "x") if layer == 0 else (hbuf, "h")
        if layer % 2 == 0:
            phase_even_mixer(b, cx, layer, h_in, hk_in, hbuf, "h")
        else:
            phase_odd_mixer(b, cx, layer, h_in, hk_in, hbuf, "h")
        phase_xattn(b, cx, layer, hbuf, "h", hbuf, "h")
        phase_moe2(b, cx, layer, hbuf, "h", hbuf, "h")
    phase_final_norm(b, cx, hbuf, "h", cx.out)


def kernel(**inputs):
    x = np.asarray(inputs["x"], dtype=np.float32)
    mem = np.asarray(inputs["mem"], dtype=np.float32)
    pos = np.asarray(inputs["positions"]).astype(np.int32)
    consts = host_consts()
    shapes = {"xT": ((D, T), F32), "memT": ((D, 256), F32), "pos": ((1, T), I32),
              "rope_inv": ((128, 1), F32), "cover": ((256, 64), F32)}
    weights = {}
    for nm in WEIGHT_NAMES:
        w = np.ascontiguousarray(np.asarray(inputs[nm], dtype=np.float32))
        weights[nm] = w
        shapes[nm] = (w.shape, F32)
    mw = host_moe_weights(inputs)
    for nm, w in mw.items():
        weights[nm] = w
        shapes[nm] = (w.shape, F32)
    nc = build_program(shapes, full_phases)
    in_maps = []
    for c in range(NCORES):
        m = {"xT": np.ascontiguousarray(x[c].T), "memT": np.ascontiguousarray(mem[c].T),
             "pos": np.ascontiguousarray(pos[c:c + 1]), "rope_inv": consts["rope_inv"], "cover": consts["cover"]}
        m.update(weights)
        in_maps.append(m)
    res = run_bass_kernel_spmd(nc, in_maps, core_ids=list(range(NCORES)))
    out = np.stack([np.asarray(res.results[c]["out"], dtype=np.float32).T for c in range(NCORES)], axis=0)
    return np.ascontiguousarray(out.astype(np.float32))
```

```python
import contextlib
import math
import numpy as np
import concourse.bass as bass
import concourse.mybir as mybir
from concourse.bass_utils import run_bass_kernel_spmd

F32 = mybir.dt.float32
BF16 = mybir.dt.bfloat16
I32 = mybir.dt.int32
ALU = mybir.AluOpType
AF = mybir.ActivationFunctionType
AX = mybir.AxisListType

D = 1024
T = 4096
NCORES = 8
DEPTH = 4
EPS = 1e-6
ENGS = ("sync", "gpsimd", "scalar", "vector", "tensor")
NS_DMA = 8
NSLOT_T = 11
NSLOT = NSLOT_T * 512
XSW = 1024 + 8


class Op:
    __slots__ = ("eng", "fn", "deps", "marked", "is_dma", "dma_idx", "semval", "gid")


class Builder:
    def __init__(self, nc):
        self.nc = nc
        self.q = {e: [] for e in ENGS}
        self.lastw = {}
        self.readers = {}
        self.ndma = {e: 0 for e in ENGS}
        self.stack = contextlib.ExitStack()
        self.stacks = [self.stack]
        self.uid = 0
        self.gid = 0

    def sb(self, shape, dtype, name=None):
        self.uid += 1
        return self.stacks[-1].enter_context(self.nc.sbuf_tensor(f"{name or 'sb'}_{self.uid}", list(shape), dtype))

    def ps(self, shape, dtype=F32, name=None):
        self.uid += 1
        return self.stacks[-1].enter_context(self.nc.psum_tensor(f"{name or 'ps'}_{self.uid}", list(shape), dtype))

    @contextlib.contextmanager
    def scope(self):
        st = contextlib.ExitStack()
        self.stacks.append(st)
        try:
            yield
        finally:
            self.barrier()
            self.stacks.pop()
            st.close()

    def dram(self, name, shape, dtype, kind="Internal"):
        return self.nc.dram_tensor(name, list(shape), dtype, kind=kind).ap()

    def op(self, eng, fn, reads=(), writes=(), dma=False):
        o = Op()
        o.eng = eng
        o.fn = fn
        o.marked = False
        o.is_dma = dma
        o.dma_idx = -1
        o.semval = 0
        self.gid += 1
        o.gid = self.gid
        psr = [r for r in reads if isinstance(r, tuple) and len(r) == 2 and r[0] == "ps"]
        if psr:
            reads = [r for r in reads if r not in psr]
            writes = list(writes) + psr
        deps = {}
        for r in reads:
            w = self.lastw.get(r)
            if w is not None:
                deps[id(w)] = w
        for k in writes:
            w = self.lastw.get(k)
            if w is not None:
                deps[id(w)] = w
            rd = self.readers.get(k)
            if rd:
                for x in rd[0].values():
                    deps[id(x)] = x
                for x in rd[1]:
                    deps[id(x)] = x
        o.deps = []
        for d in deps.values():
            if d is o:
                continue
            if eng == "tensor" and d.eng == "tensor" and not d.is_dma and not dma:
                continue
            d.marked = True
            o.deps.append(d)
        for r in reads:
            rd = self.readers.get(r)
            if rd is None:
                rd = ({}, [])
                self.readers[r] = rd
            if dma:
                rd[1].append(o)
            else:
                rd[0][eng] = o
        for k in writes:
            self.lastw[k] = o
            self.readers[k] = ({}, [])
        if dma:
            o.dma_idx = self.ndma[eng]
            self.ndma[eng] += 1
        self.q[eng].append(o)
        return o

    def dma(self, eng, out, in_, reads=(), writes=()):
        return self.op(eng, lambda e: e.dma_start(out=out, in_=in_), reads, writes, dma=True)

    def barrier(self):
        key = ("__bar__", self.gid)
        allk = list(self.lastw.keys())
        rk = [k for k, v in self.readers.items() if v[0] or v[1]]
        keys = list(set(allk) | set(rk))
        first = self.op("vector", lambda e: None, reads=(), writes=keys + [key])
        for e in ENGS:
            if e != "vector":
                self.op(e, lambda e_: None, reads=[key], writes=())
        self.lastw = {key: first}
        self.readers = {}

    def emit(self):
        nc = self.nc
        st = self.stack
        esem = {e: st.enter_context(nc.semaphore(f"es_{e}")) for e in ENGS}
        dsem = {e: [st.enter_context(nc.semaphore(f"ds_{e}{i}")) for i in range(NS_DMA)]
                for e in ENGS if self.ndma[e] > 0}
        for e in ENGS:
            c = 0
            for o in self.q[e]:
                if o.is_dma:
                    continue
                if o.marked:
                    c += 1
                    o.semval = c
                else:
                    o.semval = c

        def semref(d):
            if d.is_dma:
                k = d.dma_idx % NS_DMA
                return ("d", d.eng, k), dsem[d.eng][k], 16 * (d.dma_idx // NS_DMA + 1)
            return ("e", d.eng), esem[d.eng], d.semval

        def run(ename, eng):
            waited = {}
            pending_inc = 0
            for o in self.q[ename]:
                for d in o.deps:
                    key, sem, val = semref(d)
                    if waited.get(key, 0) >= val:
                        continue
                    eng.wait_ge(sem, val)
                    waited[key] = val
                if o.is_dma:
                    k = o.dma_idx % NS_DMA
                    prev = 16 * (o.dma_idx // NS_DMA)
                    key = ("d", ename, k)
                    if prev > 0 and waited.get(key, 0) < prev:
                        eng.wait_ge(dsem[ename][k], prev)
                        waited[key] = prev
                    ins = o.fn(eng)
                    ins.then_inc(dsem[ename][k], 16)
                else:
                    ins = o.fn(eng)
                    if o.marked:
                        if ins is None:
                            ins = eng.nop()
                        ins.then_inc(esem[ename], 1)

        with nc.Block() as block:
            @block.sync
            def _(e):
                run("sync", e)

            @block.gpsimd
            def _(e):
                run("gpsimd", e)

            @block.scalar
            def _(e):
                run("scalar", e)

            @block.vector
            def _(e):
                run("vector", e)

            @block.tensor
            def _(e):
                run("tensor", e)
        st.close()


class Ctx:
    pass


def mm(b, out, lhsT, rhs, start, stop, reads, writes):
    return b.op("tensor", lambda e: e.matmul(out, lhsT, rhs, start=start, stop=stop), reads, writes)


def load_w(b, dst, src, key, eng="gpsimd"):
    return b.op(eng, lambda e: e.dma_start(out=dst, in_=src, max_dma_last_dim=4096), writes=[key], dma=True)


def load_w_kc(b, dst, src, key, nk):
    sv = src.rearrange("(c p) m -> p c m", p=128)
    for c in range(nk):
        load_w(b, dst[:, c, :], sv[:, c, :], (key, c))


def wkeys(key, nk):
    return [(key, c) for c in range(nk)]


def setup_consts(b, cx):
    cx.eps_col = b.sb([128, 1], F32, "eps_col")
    b.op("vector", lambda e: e.memset(cx.eps_col[:], EPS), writes=["eps"])
    cx.ones_f = b.sb([128, 128], F32, "ones_f")
    b.op("vector", lambda e: e.memset(cx.ones_f[:], 1.0), writes=["ones_f"])
    cx.ones_b = b.sb([128, 128], BF16, "ones_b")
    b.op("vector", lambda e: e.memset(cx.ones_b[:], 1.0), writes=["ones_b"])
    cx.ident_i = b.sb([128, 128], I32, "ident_i")
    cx.ident_f = b.sb([128, 128], F32, "ident_f")
    cx.ident_b = b.sb([128, 128], BF16, "ident_b")
    b.op("gpsimd", lambda e: e.iota(cx.ident_i[:], pattern=[[1, 128]], base=0, channel_multiplier=-1),
         writes=["ident_i"])
    b.op("vector", lambda e: e.tensor_single_scalar(out=cx.ident_f[:], in_=cx.ident_i[:], scalar=0.0, op=ALU.is_equal),
         reads=["ident_i"], writes=["ident_f"])
    b.op("vector", lambda e: e.tensor_copy(out=cx.ident_b[:], in_=cx.ident_f[:]), reads=["ident_f"], writes=["ident_b"])
    cx.sel_i = b.sb([16, 16, 128], I32, "sel_i")
    cx.sel_f = b.sb([16, 16, 128], F32, "sel_f")
    b.op("gpsimd", lambda e: e.iota(cx.sel_i[:], pattern=[[1, 16], [0, 128]], base=0, channel_multiplier=-1),
         writes=["sel_i"])
    b.op("vector", lambda e: e.tensor_single_scalar(out=cx.sel_f[:], in_=cx.sel_i[:], scalar=0.0, op=ALU.is_equal),
         reads=["sel_i"], writes=["sel_f"])
    cx.gains = b.sb([128, 4 * DEPTH + 1, 8], F32, "gains")
    for i, nm in enumerate(("norm_mix", "norm_xattn", "norm_mem", "norm_ffn")):
        src = cx.inp[nm].rearrange("l (c p) -> p l c", p=128)
        b.op("sync", lambda e, i=i, src=src: e.dma_start(
            out=cx.gains[:, i * DEPTH:(i + 1) * DEPTH, :], in_=src, allow_slow_non_contiguous=True),
            writes=[("gains", i)], dma=True)
    srcf = cx.inp["norm_final"].rearrange("(c p) -> p c", p=128)
    b.op("sync", lambda e: e.dma_start(out=cx.gains[:, 4 * DEPTH, :], in_=srcf, allow_slow_non_contiguous=True),
         writes=[("gains", 4)], dma=True)
    cx.psum = [b.ps([128, 512], F32, f"bank{i}") for i in range(8)]
    cx.psum_b = [p.bitcast(BF16) for p in cx.psum]
    cx.tri_f = b.sb([128, 128], F32, "tri_f")
    b.op("vector", lambda e: e.tensor_single_scalar(out=cx.tri_f[:], in_=cx.ident_i[:], scalar=0.0, op=ALU.is_ge),
         reads=["ident_i"], writes=["tri_f"])
    cx.thr_i = b.sb([128, 4, 8], I32, "thr_i")
    cx.thr512 = b.sb([128, 4, 8], F32, "thr512")
    b.op("gpsimd", lambda e: e.iota(cx.thr_i[:], pattern=[[0, 4], [512, 8]], base=0, channel_multiplier=0), writes=["thr_i"])
    b.op("vector", lambda e: e.tensor_copy(out=cx.thr512[:], in_=cx.thr_i[:]), reads=["thr_i"], writes=["thr512"])
    cx.k32_i = b.sb([128, 32], I32, "k32_i")
    cx.k32 = b.sb([128, 32], F32, "k32")
    b.op("gpsimd", lambda e: e.iota(cx.k32_i[:], pattern=[[128, 32]], base=0, channel_multiplier=1), writes=["k32_i"])
    b.op("vector", lambda e: e.tensor_copy(out=cx.k32[:], in_=cx.k32_i[:]), reads=["k32_i"], writes=["k32"])
    cx.tst_i = b.sb([128, NSLOT_T, 3], I32, "tst_i")
    cx.tstart = b.sb([128, NSLOT_T, 3], F32, "tstart")
    b.op("gpsimd", lambda e: e.iota(cx.tst_i[:], pattern=[[512, NSLOT_T], [0, 3]], base=0, channel_multiplier=0), writes=["tst_i"])
    b.op("vector", lambda e: e.tensor_copy(out=cx.tstart[:], in_=cx.tst_i[:]), reads=["tst_i"], writes=["tstart"])


def PS(cx, i):
    return cx.psum[i][:], ("ps", i)


def gain_col(cx, kind, layer, c):
    idx = 4 * DEPTH if kind == 4 else kind * DEPTH + layer
    return cx.gains[:, idx, c:c + 1]


def rmsnorm_tile(b, cx, ht, htk, N, kind, layer, outs, sq, sqk, bank, rstd, rstdk):
    ps, psk = PS(cx, bank)
    ps = ps[:, 0:N]
    b.op("scalar", lambda e: e.activation(out=sq, in_=ht, func=AF.Square), reads=[htk], writes=[sqk])
    for c in range(8):
        mm(b, ps, cx.ones_f[:], sq[:, c, :], c == 0, c == 7, reads=[sqk, "ones_f"], writes=[psk])
    b.op("scalar", lambda e: e.activation(out=rstd, in_=ps, func=AF.Sqrt, scale=1.0 / D, bias=cx.eps_col[:, 0:1]),
         reads=[psk, "eps"], writes=[rstdk])
    b.op("vector", lambda e: e.reciprocal(out=rstd, in_=rstd), reads=[rstdk], writes=[rstdk])
    for (o, ok) in outs:
        for c in range(8):
            b.op("vector", lambda e, o=o, c=c: e.scalar_tensor_tensor(
                out=o[:, c, :], in0=ht[:, c, :], scalar=gain_col(cx, kind, layer, c), in1=rstd,
                op0=ALU.mult, op1=ALU.mult),
                reads=[htk, rstdk, ("gains", kind)], writes=[ok])


def hview(ap):
    return ap.rearrange("(c p) t -> p c t", p=128)


def phase_final_norm(b, cx, h_in, hk, out_dram):
    NT = 512
    with b.scope():
        ht = [b.sb([128, 8, NT], F32, "fn_ht") for i in range(2)]
        sq = [b.sb([128, 8, NT], F32, "fn_sq") for i in range(2)]
        ot = [b.sb([128, 8, NT], F32, "fn_ot") for i in range(2)]
        rs = [b.sb([128, NT], F32, "fn_rs") for i in range(2)]
        hv = hview(h_in)
        ov = hview(out_dram)
        def ld(t):
            b.dma("sync", ht[t % 2][:], hv[:, :, t * NT:(t + 1) * NT], reads=[(hk, t)], writes=[("fn_ht", t % 2)])
        ld(0)
        for t in range(T // NT):
            s = t % 2
            if t + 1 < T // NT:
                ld(t + 1)
            rmsnorm_tile(b, cx, ht[s][:], ("fn_ht", s), NT, 4, 0, [(ot[s][:], ("fn_ot", s))],
                         sq[s][:], ("fn_sq", s), s, rs[s][:], ("fn_rs", s))
            b.dma("sync", ov[:, :, t * NT:(t + 1) * NT], ot[s][:], reads=[("fn_ot", s)], writes=[("out", t)])


def phase_odd_mixer(b, cx, layer, h_in, hk_in, h_out, hk_out):
    o = layer // 2
    NT = 512
    with b.scope():
        w_in = b.sb([128, 8, 3072], BF16, "od_win")
        w_out = b.sb([128, 8, 1024], BF16, "od_wout")
        cw = b.sb([128, 8, 3], F32, "od_cw")
        load_w_kc(b, w_in, cx.inp["odd_w_in"][o], "od_win", 8)
        load_w_kc(b, w_out, cx.inp["odd_w_out"][o], "od_wout", 8)
        for k in range(3):
            b.op("sync", lambda e, k=k: e.dma_start(out=cw[:, :, k], in_=cx.inp["odd_conv_w"][o, k].rearrange("(c p) -> p c", p=128),
                                                    allow_slow_non_contiguous=True), writes=["od_cw"], dma=True)
        ht = [b.sb([128, 8, NT], F32, "od_ht") for i in range(2)]
        sq = b.sb([128, 8, NT], F32, "od_sq")
        rs = b.sb([128, NT], F32, "od_rs")
        xn = [b.sb([128, 8, NT], BF16, "od_xn") for i in range(2)]
        z = [b.sb([128, 8, NT + 2], F32, "od_z") for i in range(2)]
        csb = [b.sb([128, NT], F32, "od_c") for i in range(2)]
        acc = [b.sb([128, NT], F32, "od_acc") for i in range(2)]
        uT = b.sb([128, 8, NT], BF16, "od_uT")
        b.op("gpsimd", lambda e: e.memset(z[0][:, :, 0:2], 0.0), writes=[("od_zh", 0)])
        hv_in = hview(h_in)
        hv_out = hview(h_out)
        WK = wkeys("od_win", 8)
        WOK = wkeys("od_wout", 8)
        def ld(t):
            b.dma("sync", ht[t % 2][:], hv_in[:, :, t * NT:(t + 1) * NT], reads=[(hk_in, t)], writes=[("od_ht", t % 2)])
        ld(0)
        for t in range(T // NT):
            s = t % 2
            if t + 1 < T // NT:
                ld(t + 1)
            if t == 0:
                rmsnorm_tile(b, cx, ht[0][:], ("od_ht", 0), NT, 0, layer, [(xn[0][:], ("od_xn", 0))],
                             sq[:], "od_sq", 0, rs[:], "od_rs")
            for c in range(8):
                a = c % 2
                banks = [1 + 3 * a, 2 + 3 * a, 3 + 3 * a]
                pss = []
                for j in range(3):
                    ps, psk = PS(cx, banks[j])
                    col = j * 1024 + c * 128
                    for kc in range(8):
                        mm(b, ps, w_in[:, kc, col:col + 128], xn[s][:, kc, :], kc == 0, kc == 7,
                           reads=[("od_xn", s), ("od_win", kc)], writes=[psk])
                    pss.append((ps, psk))
                (pb, pbk), (pc, pck), (pv, pvk) = pss
                b.op("scalar", lambda e, a=a, pc=pc: e.copy(out=csb[a][:], in_=pc), reads=[pck], writes=[("od_c", a)])
                b.op("vector", lambda e, a=a, pv=pv, c=c, s=s: e.tensor_tensor(
                    out=z[s][:, c, 2:2 + NT], in0=pv, in1=csb[a][:], op=ALU.mult),
                    reads=[pvk, ("od_c", a)], writes=[("od_z", s, c)])
                b.op("vector", lambda e, a=a, c=c, s=s: e.tensor_scalar(
                    out=acc[a][:], in0=z[s][:, c, 2:2 + NT], scalar1=cw[:, c, 2:3], scalar2=None, op0=ALU.mult),
                    reads=[("od_z", s, c), "od_cw"], writes=[("od_acc", a)])
                for k in (1, 0):
                    b.op("vector", lambda e, a=a, c=c, s=s, k=k: e.scalar_tensor_tensor(
                        out=acc[a][:], in0=z[s][:, c, k:k + NT], scalar=cw[:, c, k:k + 1], in1=acc[a][:],
                        op0=ALU.mult, op1=ALU.add),
                        reads=[("od_z", s, c), ("od_zh", s), "od_cw", ("od_acc", a)], writes=[("od_acc", a)])
                b.op("vector", lambda e, a=a, c=c, pb=pb: e.tensor_tensor(
                    out=uT[:, c, :], in0=pb, in1=acc[a][:], op=ALU.mult),
                    reads=[pbk, ("od_acc", a)], writes=[("od_uT", c)])
            if t + 1 < T // NT:
                rmsnorm_tile(b, cx, ht[1 - s][:], ("od_ht", 1 - s), NT, 0, layer, [(xn[1 - s][:], ("od_xn", 1 - s))],
                             sq[:], "od_sq", 0, rs[:], "od_rs")
            b.op("gpsimd", lambda e, s=s: e.tensor_copy(out=z[1 - s][:, :, 0:2], in_=z[s][:, :, NT:NT + 2]),
                 reads=[("od_z", s, c) for c in range(8)], writes=[("od_zh", 1 - s)])
            for mc in range(8):
                ps, psk = PS(cx, 1 + (mc % 2) * 3)
                for kc in range(8):
                    mm(b, ps, w_out[:, kc, mc * 128:(mc + 1) * 128], uT[:, kc, :], kc == 0, kc == 7,
                       reads=[("od_uT", kc), ("od_wout", kc)], writes=[psk])
                b.op("vector", lambda e, mc=mc, ps=ps, s=s: e.tensor_tensor(
                    out=ht[s][:, mc, :], in0=ps, in1=ht[s][:, mc, :], op=ALU.add),
                    reads=[psk, ("od_ht", s)], writes=[("od_ht", s)])
            b.dma("sync", hv_out[:, :, t * NT:(t + 1) * NT], ht[s][:], reads=[("od_ht", s)], writes=[(hk_out, t)])


def phase_xattn(b, cx, layer, h_in, hk_in, h_out, hk_out):
    NT = 512
    with b.scope():
        wq = b.sb([128, 8, 1024], BF16, "xa_wq")
        wo = b.sb([128, 8, 1024], BF16, "xa_wo")
        KT = b.sb([128, 8, 256], BF16, "xa_KT")
        V = b.sb([128, 2, 1024], BF16, "xa_V")
        with b.scope():
            wk = b.sb([128, 8, 1024], BF16, "xa_wk")
            wv = b.sb([128, 8, 1024], BF16, "xa_wv")
            memT = b.sb([128, 8, 256], F32, "xa_memT")
            mn = b.sb([128, 8, 256], BF16, "xa_mn")
            sqm = b.sb([128, 8, 256], F32, "xa_sqm")
            rsm = b.sb([128, 256], F32, "xa_rsm")
            for w, nm in ((wk, "xa_wk"), (wv, "xa_wv"), (wq, "xa_wq"), (wo, "xa_wo")):
                load_w_kc(b, w, cx.inp[nm][layer], nm, 8)
            b.dma("sync", memT[:], hview(cx.inp["memT"]), writes=["xa_memT"])
            rmsnorm_tile(b, cx, memT[:], "xa_memT", 256, 2, layer, [(mn[:], "xa_mn")],
                         sqm[:], "xa_sqm", 0, rsm[:], "xa_rsm")
            for mc in range(8):
                ps, psk = PS(cx, 1 + mc % 2)
                ps = ps[:, 0:256]
                for kc in range(8):
                    mm(b, ps, wk[:, kc, mc * 128:(mc + 1) * 128], mn[:, kc, :], kc == 0, kc == 7,
                       reads=["xa_mn", ("xa_wk", kc)], writes=[psk])
                b.op("scalar", lambda e, mc=mc, ps=ps: e.copy(out=KT[:, mc, :], in_=ps), reads=[psk], writes=["xa_KT"])
            for mch in range(2):
                for half in range(2):
                    ps, psk = PS(cx, 1 + half)
                    for kc in range(8):
                        mm(b, ps, mn[:, kc, mch * 128:(mch + 1) * 128], wv[:, kc, half * 512:(half + 1) * 512],
                           kc == 0, kc == 7, reads=["xa_mn", ("xa_wv", kc)], writes=[psk])
                    b.op("scalar", lambda e, mch=mch, half=half, ps=ps: e.copy(
                        out=V[:, mch, half * 512:(half + 1) * 512], in_=ps), reads=[psk], writes=["xa_V"])
        ht = [b.sb([128, 8, NT], F32, "xa_ht") for i in range(3)]
        sq = b.sb([128, 8, NT], F32, "xa_sq")
        rs = b.sb([128, NT], F32, "xa_rs")
        xn = [b.sb([128, 8, NT], BF16, "xa_xn") for i in range(2)]
        qT = b.sb([128, 8, NT], BF16, "xa_qT")
        PT = [b.sb([128, 2, NT], BF16, "xa_PT") for i in range(2)]
        rden = [b.sb([128, NT], F32, "xa_rden") for i in range(2)]
        oT = b.sb([128, 8, NT], BF16, "xa_oT")
        hv_in = hview(h_in)
        hv_out = hview(h_out)
        scale = 1.0 / 16.0
        def ld(t):
            b.dma("sync", ht[t % 3][:], hv_in[:, :, t * NT:(t + 1) * NT], reads=[(hk_in, t)], writes=[("xa_ht", t % 3)])
        def nrm(t):
            s_ = t % 2
            rmsnorm_tile(b, cx, ht[t % 3][:], ("xa_ht", t % 3), NT, 1, layer, [(xn[s_][:], ("xa_xn", s_))],
                         sq[:], "xa_sq", 0, rs[:], "xa_rs")
        ld(0)
        ld(1)
        nrm(0)
        for t in range(T // NT):
            s = t % 2
            s3 = t % 3
            if t + 2 < T // NT:
                ld(t + 2)
            for mc in range(8):
                ps, psk = PS(cx, 1 + mc % 2)
                for kc in range(8):
                    mm(b, ps, wq[:, kc, mc * 128:(mc + 1) * 128], xn[s][:, kc, :], kc == 0, kc == 7,
                       reads=[("xa_xn", s), ("xa_wq", kc)], writes=[psk])
                b.op("scalar", lambda e, mc=mc, ps=ps: e.copy(out=qT[:, mc, :], in_=ps), reads=[psk], writes=[("xa_qT", mc)])
            if t + 1 < T // NT:
                nrm(t + 1)
            for h in range(4):
                a = h % 2
                for mch in range(2):
                    ps, psk = PS(cx, 3 + mch)
                    for dc in range(2):
                        mm(b, ps, KT[:, 2 * h + dc, mch * 128:(mch + 1) * 128], qT[:, 2 * h + dc, :], dc == 0, dc == 1,
                           reads=["xa_KT", ("xa_qT", 2 * h + dc)], writes=[psk])
                    b.op("scalar", lambda e, a=a, mch=mch, ps=ps: e.activation(
                        out=PT[a][:, mch, :], in_=ps, func=AF.Exp, scale=scale), reads=[psk], writes=[("xa_PT", a, mch)])
                ps, psk = PS(cx, 5)
                for mch in range(2):
                    mm(b, ps, cx.ones_b[:], PT[a][:, mch, :], mch == 0, mch == 1,
                       reads=[("xa_PT", a, mch), "ones_b"], writes=[psk])
                b.op("vector", lambda e, a=a, ps=ps: e.reciprocal(out=rden[a][:], in_=ps), reads=[psk], writes=[("xa_rden", a)])
                for dc in range(2):
                    ps, psk = PS(cx, 6 + dc)
                    for mch in range(2):
                        col = h * 256 + dc * 128
                        mm(b, ps, V[:, mch, col:col + 128], PT[a][:, mch, :], mch == 0, mch == 1,
                           reads=[("xa_PT", a, mch), "xa_V"], writes=[psk])
                    b.op("vector", lambda e, a=a, h=h, dc=dc, ps=ps: e.tensor_tensor(
                        out=oT[:, 2 * h + dc, :], in0=ps, in1=rden[a][:], op=ALU.mult),
                        reads=[psk, ("xa_rden", a)], writes=[("xa_oT", 2 * h + dc)])
            for mq in range(0, 8, 4):
                for part in (range(0, 6), range(6, 8)):
                    for mc in range(mq, mq + 4):
                        ps, psk = PS(cx, 1 + mc % 4)
                        for kc in part:
                            mm(b, ps, wo[:, kc, mc * 128:(mc + 1) * 128], oT[:, kc, :], kc == 0, kc == 7,
                               reads=[("xa_oT", kc), ("xa_wo", kc)], writes=[psk])
                for mc in range(mq, mq + 4):
                    ps, psk = PS(cx, 1 + mc % 4)
                    b.op("vector", lambda e, mc=mc, ps=ps, s3=s3: e.tensor_tensor(
                        out=ht[s3][:, mc, :], in0=ps, in1=ht[s3][:, mc, :], op=ALU.add),
                        reads=[psk, ("xa_ht", s3)], writes=[("xa_ht", s3)])
            b.dma("sync", hv_out[:, :, t * NT:(t + 1) * NT], ht[s3][:], reads=[("xa_ht", s3)], writes=[(hk_out, t)])


def bc(ap, shape, axis):
    return ap.unsqueeze(axis).to_broadcast(list(shape))


INPUT_NAMES = ["x", "mem", "positions", "norm_mix", "norm_xattn", "norm_mem", "norm_ffn", "norm_final",
               "even_w_in", "even_w_out", "lru_conv_w", "lru_conv_b", "lru_w_r", "lru_b_r", "lru_w_i", "lru_b_i",
               "lru_lambda", "nsa_cmp_pos_k", "nsa_cmp_w1_k", "nsa_cmp_w2_k", "nsa_cmp_pos_v", "nsa_cmp_w1_v",
               "nsa_cmp_w2_v", "odd_w_in", "odd_conv_w", "odd_w_out", "xa_wq", "xa_wk", "xa_wv", "xa_wo",
               "moe_w_group", "moe_b_group", "moe_w_expert", "moe_b_expert", "moe_w_gate", "moe_w_up", "moe_w_down"]


def build_program(shapes, phases):
    nc = bass.Bass("TRN2", target_bir_lowering=False)
    b = Builder(nc)
    cx = Ctx()
    cx.nc = nc
    cx.inp = {}
    for nm, (shape, dt) in shapes.items():
        cx.inp[nm] = nc.dram_tensor(nm, list(shape), dt, kind="ExternalInput").ap()
    cx.out = nc.dram_tensor("out", [D, T], F32, kind="ExternalOutput").ap()
    setup_consts(b, cx)
    phases(b, cx)
    b.barrier()
    b.emit()
    return nc


NEG = -1e30
FORCE = 1e9
BIGM = 30000.0
GELU_K = 2.0 * math.sqrt(2.0 / math.pi)


_FILL_REGS = {}


def asel(e, **kw):
    f = float(kw["fill"])
    key = (id(e), f)
    if key not in _FILL_REGS:
        _FILL_REGS[key] = e.to_reg(f)
    kw["fill"] = _FILL_REGS[key]
    return e.affine_select(**kw)


def scratch(cx, name, shape, dtype):
    kind = "ExternalOutput" if getattr(cx, "debug", False) else "Internal"
    t = cx.nc.dram_tensor(name, list(shape), dtype, kind=kind).ap()
    return t


def gelu_ops(b, src, srck, out, outk, tA, tAk, tB, tBk):
    b.op("scalar", lambda e: e.activation(out=tA, in_=src, func=AF.Square), reads=[srck], writes=[tAk])
    b.op("vector", lambda e: e.tensor_scalar(out=tA, in0=tA, scalar1=0.044715, scalar2=1.0, op0=ALU.mult, op1=ALU.add),
         reads=[tAk], writes=[tAk])
    b.op("vector", lambda e: e.tensor_tensor(out=tA, in0=src, in1=tA, op=ALU.mult), reads=[srck, tAk], writes=[tAk])
    b.op("scalar", lambda e: e.activation(out=tB, in_=tA, func=AF.Sigmoid, scale=GELU_K), reads=[tAk], writes=[tBk])
    b.op("vector", lambda e: e.tensor_tensor(out=out, in0=src, in1=tB, op=ALU.mult), reads=[srck, tBk], writes=[outk])


def phase_rope_tables(b, cx):
    cx.ropeC = scratch(cx, "ropeC", [128, T], F32)
    cx.ropeS = scratch(cx, "ropeS", [128, T], F32)
    TWO_PI = 2.0 * math.pi
    C1 = 6.28125
    C2 = TWO_PI - C1
    with b.scope():
        pi_ = b.sb([128, T], I32, "rp_pi")
        ang = b.sb([128, T], F32, "rp_ang")
        tmp = b.sb([128, T], F32, "rp_tmp")
        ki = b.sb([128, T], I32, "rp_ki")
        kf = b.sb([128, T], F32, "rp_kf")
        r = b.sb([128, T], F32, "rp_r")
        m = b.sb([128, T], F32, "rp_m")
        inv = b.sb([128, 1], F32, "rp_inv")
        b.dma("sync", inv[:], cx.inp["rope_inv"], writes=["rp_inv"])
        b.op("sync", lambda e: e.dma_start(out=pi_[:], in_=cx.inp["pos"].partition_broadcast(128)), writes=["rp_pi"], dma=True)
        b.op("vector", lambda e: e.tensor_copy(out=ang[:], in_=pi_[:]), reads=["rp_pi"], writes=["rp_ang"])
        b.op("vector", lambda e: e.tensor_scalar(out=ang[:], in0=ang[:], scalar1=inv[:, 0:1], scalar2=None, op0=ALU.mult),
             reads=["rp_ang", "rp_inv"], writes=["rp_ang"])
        for which, off, dst in (("s", 0.0, cx.ropeS), ("c", math.pi / 2.0, cx.ropeC)):
            b.op("vector", lambda e, off=off: e.tensor_scalar(out=r[:], in0=ang[:], scalar1=off, scalar2=None, op0=ALU.add),
                 reads=["rp_ang", "rp_r"], writes=["rp_r"])
            b.op("vector", lambda e: e.tensor_scalar(out=tmp[:], in0=r[:], scalar1=1.0 / TWO_PI, scalar2=None, op0=ALU.mult),
                 reads=["rp_r"], writes=["rp_tmp"])
            b.op("vector", lambda e: e.tensor_copy(out=ki[:], in_=tmp[:]), reads=["rp_tmp"], writes=["rp_ki"])
            b.op("vector", lambda e: e.tensor_copy(out=kf[:], in_=ki[:]), reads=["rp_ki"], writes=["rp_kf"])
            for cc in (C1, C2):
                b.op("vector", lambda e, cc=cc: e.scalar_tensor_tensor(out=r[:], in0=kf[:], scalar=-cc, in1=r[:],
                                                                       op0=ALU.mult, op1=ALU.add),
                     reads=["rp_kf", "rp_r"], writes=["rp_r"])
            b.op("vector", lambda e: e.tensor_single_scalar(out=m[:], in_=r[:], scalar=math.pi, op=ALU.is_gt),
                 reads=["rp_r"], writes=["rp_m"])
            b.op("vector", lambda e: e.scalar_tensor_tensor(out=r[:], in0=m[:], scalar=-TWO_PI, in1=r[:], op0=ALU.mult, op1=ALU.add),
                 reads=["rp_m", "rp_r"], writes=["rp_r"])
            b.op("vector", lambda e: e.tensor_single_scalar(out=m[:], in_=r[:], scalar=-math.pi, op=ALU.is_lt),
                 reads=["rp_r"], writes=["rp_m"])
            b.op("vector", lambda e: e.scalar_tensor_tensor(out=r[:], in0=m[:], scalar=TWO_PI, in1=r[:], op0=ALU.mult, op1=ALU.add),
                 reads=["rp_m", "rp_r"], writes=["rp_r"])
            b.op("vector", lambda e: e.tensor_scalar(out=r[:], in0=r[:], scalar1=math.pi, scalar2=-math.pi, op0=ALU.min, op1=ALU.max),
                 reads=["rp_r"], writes=["rp_r"])
            b.op("scalar", lambda e: e.activation(out=tmp[:], in_=r[:], func=AF.Sin), reads=["rp_r"], writes=["rp_tmp"])
            b.dma("sync", dst, tmp[:], reads=["rp_tmp"], writes=["rope_" + which])


def even_scratch(cx):
    if hasattr(cx, "ev"):
        return cx.ev
    ev = Ctx()
    ev.xl = scratch(cx, "ev_xl", [512, T], F32)
    ev.gg = scratch(cx, "ev_gg", [512, T], F32)
    ev.qn = scratch(cx, "ev_qn", [512, T], BF16)
    ev.qr = scratch(cx, "ev_qr", [512, T], BF16)
    ev.kc = scratch(cx, "ev_kc", [128, T], BF16)
    ev.vc = scratch(cx, "ev_vc", [128, T], BF16)
    ev.ks = scratch(cx, "ev_ks", [128, T], BF16)
    ev.kw = scratch(cx, "ev_kw", [128, T], BF16)
    ev.vs = scratch(cx, "ev_vs", [T, 128], BF16)
    ev.vw = scratch(cx, "ev_vw", [T, 128], BF16)
    ev.gt = scratch(cx, "ev_gt", [T, 24], F32)
    ev.ylru = scratch(cx, "ev_ylru", [512, T], BF16)
    ev.ynsa = scratch(cx, "ev_ynsa", [512, T], BF16)
    cx.ev = ev
    return ev


def phase_even_proj(b, cx, layer, h_in, hk_in, parts=('lru', 'gelu', 'q', 'k', 'kcvc', 'tok')):
    e_ = layer // 2
    ev = even_scratch(cx)
    NT = 512
    NP = 2328
    with b.scope():
        w_in = b.sb([128, 8, NP], BF16, "ev_win")
        w_rot = b.sb([128, 8, 768], BF16, "ev_wrot")
        load_w_kc(b, w_in, cx.inp["even_w_in"][e_], "ev_win", 8)
        b.op("gpsimd", lambda e: e.memset(w_rot[:], 0.0), writes=["ev_wrot"])
        for kc in range(8):
            for (dst0, src0, nh) in ((0, 1024, 8), (512, 1792, 2), (640, 2048, 2)):
                dv = w_rot[:, kc, dst0:dst0 + 64 * nh].rearrange("p (h d) -> p h d", d=64)
                sv = w_in[:, kc, src0:src0 + 64 * nh].rearrange("p (h d) -> p h d", d=64)
                b.op("scalar", lambda e, dv=dv, sv=sv: e.mul(out=dv[:, :, 0:8], in_=sv[:, :, 8:16], mul=-1.0),
                     reads=[("ev_win", kc), "ev_wrot"], writes=["ev_wrot"])
                b.op("scalar", lambda e, dv=dv, sv=sv: e.copy(out=dv[:, :, 8:16], in_=sv[:, :, 0:8]),
                     reads=[("ev_win", kc), "ev_wrot"], writes=["ev_wrot"])
        ht = [b.sb([128, 8, NT], F32, "ev_ht") for i in range(2)]
        sq = b.sb([128, 8, NT], F32, "ev_sq")
        rs = b.sb([128, NT], F32, "ev_rs")
        xn2 = [b.sb([128, 8, NT], BF16, "ev_xn") for i in range(2)]
        Ct = [b.sb([128, NT], F32, "ev_C") for i in range(2)]
        St = [b.sb([128, NT], F32, "ev_S") for i in range(2)]
        stf = [b.sb([128, NT], F32, "ev_stf") for i in range(2)]
        stb = [b.sb([128, NT], BF16, "ev_stb") for i in range(2)]
        tA = [b.sb([128, NT], F32, "ev_tA") for i in range(2)]
        tB = [b.sb([128, NT], F32, "ev_tB") for i in range(2)]
        vst = [b.sb([128, 2, 128], BF16, "ev_vst") for i in range(2)]
        gst = [b.sb([128, 24], F32, "ev_gst") for i in range(2)]
        hv_in = hview(h_in)
        WK = None
        cnt = {"f": 0, "b": 0}

        cur = {}

        def proj(ps, psk, wt, wkey, col, ncols=128):
            for kc in range(8):
                mm(b, ps[0:ncols, :] if ncols != 128 else ps, wt[:, kc, col:col + ncols], cur["xn"][:, kc, :], kc == 0, kc == 7,
                   reads=[cur["k"], wkey if wkey == "ev_wrot" else (wkey, kc)], writes=[psk])

        def nrm(t_):
            s_ = t_ % 2
            rmsnorm_tile(b, cx, ht[s_][:], ("ev_ht", s_), NT, 0, layer, [(xn2[s_][:], ("ev_xn", s_))], sq[:], "ev_sq", 0, rs[:], "ev_rs")

        def ld(t):
            s = t % 2
            tsl = slice(t * NT, (t + 1) * NT)
            b.dma("sync", ht[s][:], hv_in[:, :, tsl], reads=[(hk_in, t)], writes=[("ev_ht", s)])
            b.dma("sync", Ct[s][:], cx.ropeC[:, tsl], reads=["rope_c"], writes=[("ev_C", s)])
            b.dma("sync", St[s][:], cx.ropeS[:, tsl], reads=["rope_s"], writes=[("ev_S", s)])
        ld(0)
        for t in range(T // NT):
            s = t % 2
            tsl = slice(t * NT, (t + 1) * NT)
            if t + 1 < T // NT:
                ld(t + 1)
            if t == 0:
                nrm(0)
            cur["xn"], cur["k"] = xn2[s], ("ev_xn", s)
            xn = xn2[s]
            for c in (range(4) if 'lru' in parts else ()):
                a = cnt["f"] % 2; cnt["f"] += 1
                ps, psk = PS(cx, 1 + a)
                proj(ps, psk, w_in, "ev_win", c * 128)
                b.op("scalar", lambda e, a=a, ps=ps: e.copy(out=stf[a][:], in_=ps), reads=[psk], writes=[("ev_stf", a)])
                b.dma("sync", ev.xl[c * 128:(c + 1) * 128, tsl], stf[a][:], reads=[("ev_stf", a)], writes=[("ev_xl", c, t)])
            for c in (range(4) if 'gelu' in parts else ()):
                a = cnt["f"] % 2; cnt["f"] += 1
                ps, psk = PS(cx, 1 + a)
                proj(ps, psk, w_in, "ev_win", 512 + c * 128)
                gelu_ops(b, ps, psk, stf[a][:], ("ev_stf", a), tA[a][:], ("ev_tA", a), tB[a][:], ("ev_tB", a))
                b.dma("sync", ev.gg[c * 128:(c + 1) * 128, tsl], stf[a][:], reads=[("ev_stf", a)], writes=[("ev_gg", c, t)])
            if t + 1 < T // NT:
                nrm(t + 1)
            def roped(col, rcol, plain_dst, rope_dst, key):
                a = cnt["f"] % 2; cnt["f"] += 1
                ps, psk = PS(cx, 1 + a)
                pr, prk = PS(cx, 3 + a)
                proj(ps, psk, w_in, "ev_win", col)
                proj(pr, prk, w_rot, "ev_wrot", rcol)
                if plain_dst is not None:
                    a2 = cnt["b"] % 2; cnt["b"] += 1
                    b.op("scalar", lambda e, a2=a2, ps=ps: e.copy(out=stb[a2][:], in_=ps), reads=[psk], writes=[("ev_stb", a2)])
                    b.dma("sync", plain_dst, stb[a2][:], reads=[("ev_stb", a2)], writes=[(key, "p", t)])
                b.op("vector", lambda e, a=a, pr=pr, s=s: e.tensor_tensor(out=tA[a][:], in0=pr, in1=St[s][:], op=ALU.mult),
                     reads=[prk, ("ev_S", s)], writes=[("ev_tA", a)])
                b.op("vector", lambda e, a=a, ps=ps, s=s: e.tensor_tensor(out=tB[a][:], in0=ps, in1=Ct[s][:], op=ALU.mult),
                     reads=[psk, ("ev_C", s)], writes=[("ev_tB", a)])
                a2 = cnt["b"] % 2; cnt["b"] += 1
                b.op("vector", lambda e, a=a, a2=a2: e.tensor_tensor(out=stb[a2][:], in0=tA[a][:], in1=tB[a][:], op=ALU.add),
                     reads=[("ev_tA", a), ("ev_tB", a)], writes=[("ev_stb", a2)])
                b.dma("sync", rope_dst, stb[a2][:], reads=[("ev_stb", a2)], writes=[(key, "r", t)])

            for c in (range(4) if 'q' in parts else ()):
                roped(1024 + c * 128, c * 128, ev.qn[c * 128:(c + 1) * 128, tsl], ev.qr[c * 128:(c + 1) * 128, tsl], ("ev_q", c))
            if 'k' in parts:
                roped(1792, 512, None, ev.ks[:, tsl], "ev_ks")
                roped(2048, 640, None, ev.kw[:, tsl], "ev_kw")
            for (col, dst, key) in (((1536, ev.kc, "ev_kc"), (1664, ev.vc, "ev_vc")) if 'kcvc' in parts else ()):
                a = cnt["f"] % 2; cnt["f"] += 1
                ps, psk = PS(cx, 1 + a)
                proj(ps, psk, w_in, "ev_win", col)
                a2 = cnt["b"] % 2; cnt["b"] += 1
                b.op("scalar", lambda e, a2=a2, ps=ps: e.copy(out=stb[a2][:], in_=ps), reads=[psk], writes=[("ev_stb", a2)])
                b.dma("sync", dst[:, tsl], stb[a2][:], reads=[("ev_stb", a2)], writes=[(key, t)])
            for su in (range(4) if 'tok' in parts else ()):
                a = su % 2
                ps, psk = PS(cx, 5 + a)
                tok0 = t * NT + su * 128
                for kc in range(8):
                    mm(b, ps[:, 0:408], xn[:, kc, su * 128:(su + 1) * 128], w_in[:, kc, 1920:2328], kc == 0, kc == 7,
                       reads=[("ev_xn", s), ("ev_win", kc)], writes=[psk])
                b.op("scalar", lambda e, a=a, ps=ps: e.copy(out=vst[a][:, 0, :], in_=ps[:, 0:128]), reads=[psk], writes=[("ev_vst", a, 0)])
                b.op("scalar", lambda e, a=a, ps=ps: e.copy(out=vst[a][:, 1, :], in_=ps[:, 256:384]), reads=[psk], writes=[("ev_vst", a, 1)])
                b.op("scalar", lambda e, a=a, ps=ps: e.activation(out=gst[a][:], in_=ps[:, 384:408], func=AF.Sigmoid),
                     reads=[psk], writes=[("ev_gst", a)])
                b.dma("sync", ev.vs[tok0:tok0 + 128, :], vst[a][:, 0, :], reads=[("ev_vst", a, 0)], writes=[("ev_vs", t, su)])
                b.dma("sync", ev.vw[tok0:tok0 + 128, :], vst[a][:, 1, :], reads=[("ev_vst", a, 1)], writes=[("ev_vw", t, su)])
                b.dma("sync", ev.gt[tok0:tok0 + 128, :], gst[a][:], reads=[("ev_gst", a)], writes=[("ev_gt", t, su)])


def phase_even_lru(b, cx, layer, stop=9):
    e_ = layer // 2
    ev = even_scratch(cx)
    NT = 512
    with b.scope():
        cwv = b.sb([128, 4, 4], F32, "lr_cw")
        cb = b.sb([128, 4], F32, "lr_cb")
        br = b.sb([128, 4], F32, "lr_br")
        bi = b.sb([128, 4], F32, "lr_bi")
        lam = b.sb([128, 4], F32, "lr_lam")
        sp = b.sb([128, 4], F32, "lr_sp")
        n8 = b.sb([128, 4], F32, "lr_n8")
        n16 = b.sb([128, 4], F32, "lr_n16")
        Wr = b.sb([128, 4, 128], BF16, "lr_Wr")
        Wi = b.sb([128, 4, 128], BF16, "lr_Wi")
        for k in range(4):
            b.op("sync", lambda e, k=k: e.dma_start(out=cwv[:, :, k], in_=cx.inp["lru_conv_w"][e_, k].rearrange("(c p) -> p c", p=128),
                                                    allow_slow_non_contiguous=True), writes=["lr_cw"], dma=True)
        for (dst, nm, key) in ((cb, "lru_conv_b", "lr_cb"), (br, "lru_b_r", "lr_br"), (bi, "lru_b_i", "lr_bi"), (lam, "lru_lambda", "lr_lam")):
            b.op("sync", lambda e, dst=dst, nm=nm: e.dma_start(out=dst[:], in_=cx.inp[nm][e_].rearrange("(c p) -> p c", p=128),
                                                               allow_slow_non_contiguous=True), writes=[key], dma=True)
        Wst = b.sb([128, 2, 4, 128], F32, "lr_Wst")
        b.op("vector", lambda e: e.memset(Wst[:], 0.0), writes=["lr_Wst"])
        for wi_, nm in enumerate(("lru_w_r", "lru_w_i")):
            for blk in range(8):
                c, hb = blk // 2, blk % 2
                b.op("sync", lambda e, wi_=wi_, nm=nm, blk=blk, c=c, hb=hb: e.dma_start(
                    out=Wst[hb * 64:(hb + 1) * 64, wi_, c, hb * 64:(hb + 1) * 64], in_=cx.inp[nm][e_, blk]),
                    reads=[], writes=["lr_Wst"], dma=True)
        b.op("vector", lambda e: e.tensor_copy(out=Wr[:], in_=Wst[:, 0, :, :]), reads=["lr_Wst"], writes=["lr_Wr"])
        b.op("vector", lambda e: e.tensor_copy(out=Wi[:], in_=Wst[:, 1, :, :]), reads=["lr_Wst"], writes=["lr_Wi"])
        b.op("scalar", lambda e: e.activation(out=sp[:], in_=lam[:], func=AF.Exp, scale=-1.0), reads=["lr_lam"], writes=["lr_sp"])
        b.op("scalar", lambda e: e.activation(out=sp[:], in_=sp[:], func=AF.Ln, bias=cx.ones_f[:, 0:1]), reads=["lr_sp", "ones_f"], writes=["lr_sp"])
        b.op("vector", lambda e: e.tensor_scalar(out=n8[:], in0=sp[:], scalar1=-8.0, scalar2=None, op0=ALU.mult), reads=["lr_sp"], writes=["lr_n8"])
        b.op("vector", lambda e: e.tensor_scalar(out=n16[:], in0=sp[:], scalar1=-16.0, scalar2=None, op0=ALU.mult), reads=["lr_sp"], writes=["lr_n16"])
        xc = [b.sb([128, NT], F32, "lr_xc") for i in range(2)]
        xcb = [b.sb([128, NT], BF16, "lr_xcb") for i in range(2)]
        rr = [b.sb([128, NT], F32, "lr_r") for i in range(2)]
        ii = [b.sb([128, NT], F32, "lr_i") for i in range(2)]
        aa = [b.sb([128, NT], F32, "lr_a") for i in range(2)]
        a2 = [b.sb([128, NT], F32, "lr_a2") for i in range(2)]
        uu = [b.sb([128, NT], F32, "lr_u") for i in range(2)]
        hh = [b.sb([128, NT], F32, "lr_h") for i in range(2)]
        gg = [b.sb([128, NT], F32, "lr_gg") for i in range(3)]
        xp3 = [b.sb([128, NT + 3], F32, "lr_xp3") for i in range(3)]
        yb = [b.sb([128, NT], BF16, "lr_yb") for i in range(2)]
        nt = T // NT
        pairs = [(c, t) for c in range(4) for t in range(nt)] if stop >= 1 else []
        NPR = len(pairs)

        def ld(i):
            c, t = pairs[i]
            s3 = i % 3
            rows = slice(c * 128, (c + 1) * 128)
            tsl = slice(t * NT, (t + 1) * NT)
            b.dma("sync", xp3[s3][:, 3:3 + NT], ev.xl[rows, tsl], reads=[("ev_xl", c, t)], writes=[("lr_xp", s3)])
            b.dma("sync", gg[s3][:], ev.gg[rows, tsl], reads=[("ev_gg", c, t)], writes=[("lr_gg", s3)])

        def front(i):
            c, t = pairs[i]
            s = i % 2
            s3 = i % 3
            xq = xp3[s3]
            if t == 0:
                b.op("gpsimd", lambda e: e.memset(xq[:, 0:3], 0.0), writes=[("lr_xph", s3)])
            b.op("vector", lambda e: e.tensor_scalar(out=xc[s][:], in0=xq[:, 3:3 + NT], scalar1=cwv[:, c, 3:4],
                                                     scalar2=cb[:, c:c + 1], op0=ALU.mult, op1=ALU.add),
                 reads=[("lr_xp", s3), "lr_cw", "lr_cb"], writes=[("lr_xc", s)])
            for k in range(3):
                b.op("vector", lambda e, k=k: e.scalar_tensor_tensor(
                    out=xc[s][:], in0=xq[:, k:k + NT], scalar=cwv[:, c, k:k + 1], in1=xc[s][:], op0=ALU.mult, op1=ALU.add),
                    reads=[("lr_xp", s3), ("lr_xph", s3), "lr_cw", ("lr_xc", s)], writes=[("lr_xc", s)])
            if t + 1 < nt:
                xnx = xp3[(i + 1) % 3]
                b.op("gpsimd", lambda e: e.tensor_copy(out=xnx[:, 0:3], in_=xq[:, NT:NT + 3]),
                     reads=[("lr_xp", s3)], writes=[("lr_xph", (i + 1) % 3)])
            if stop < 2:
                return
            b.op("scalar", lambda e: e.copy(out=xcb[s][:], in_=xc[s][:]), reads=[("lr_xc", s)], writes=[("lr_xcb", s)])
            pr, prk = PS(cx, 1 + s)
            pi, pik = PS(cx, 3 + s)
            mm(b, pr, Wr[:, c, :], xcb[s][:], True, True, reads=[("lr_xcb", s), "lr_Wr"], writes=[prk])
            mm(b, pi, Wi[:, c, :], xcb[s][:], True, True, reads=[("lr_xcb", s), "lr_Wi"], writes=[pik])
            b.op("scalar", lambda e: e.activation(out=rr[s][:], in_=pr, func=AF.Sigmoid, bias=br[:, c:c + 1]),
                 reads=[prk, "lr_br"], writes=[("lr_r", s)])
            b.op("scalar", lambda e: e.activation(out=ii[s][:], in_=pi, func=AF.Sigmoid, bias=bi[:, c:c + 1]),
                 reads=[pik, "lr_bi"], writes=[("lr_i", s)])
            if stop < 3:
                return
            b.op("scalar", lambda e: e.activation(out=aa[s][:], in_=rr[s][:], func=AF.Exp, scale=n8[:, c:c + 1]),
                 reads=[("lr_r", s), "lr_n8"], writes=[("lr_a", s)])
            b.op("scalar", lambda e: e.activation(out=a2[s][:], in_=rr[s][:], func=AF.Exp, scale=n16[:, c:c + 1]),
                 reads=[("lr_r", s), "lr_n16"], writes=[("lr_a2", s)])
            b.op("scalar", lambda e: e.activation(out=a2[s][:], in_=a2[s][:], func=AF.Sqrt, scale=-1.0, bias=cx.ones_f[:, 0:1]),
                 reads=[("lr_a2", s), "ones_f"], writes=[("lr_a2", s)])

        def back(i):
            c, t = pairs[i]
            s = i % 2
            s3 = i % 3
            if stop < 3:
                return
            b.op("vector", lambda e: e.tensor_tensor(out=uu[s][:], in0=ii[s][:], in1=xc[s][:], op=ALU.mult),
                 reads=[("lr_i", s), ("lr_xc", s)], writes=[("lr_u", s)])
            b.op("vector", lambda e: e.tensor_tensor(out=uu[s][:], in0=uu[s][:], in1=a2[s][:], op=ALU.mult),
                 reads=[("lr_u", s), ("lr_a2", s)], writes=[("lr_u", s)])
            if stop < 4:
                return
            init = 0.0 if t == 0 else hh[1 - s][:, NT - 1:NT]
            b.op("vector", lambda e: e.tensor_tensor_scan(out=hh[s][:], data0=aa[s][:], data1=uu[s][:], initial=init,
                                                          op0=ALU.mult, op1=ALU.add),
                 reads=[("lr_a", s), ("lr_u", s), ("lr_h", 1 - s)], writes=[("lr_h", s)])
            b.op("vector", lambda e: e.tensor_tensor(out=yb[s][:], in0=hh[s][:], in1=gg[s3][:], op=ALU.mult),
                 reads=[("lr_h", s), ("lr_gg", s3)], writes=[("lr_yb", s)])
            b.dma("sync", ev.ylru[c * 128:(c + 1) * 128, t * NT:(t + 1) * NT], yb[s][:], reads=[("lr_yb", s)], writes=[("ev_ylru", c, t)])

        for i in range(min(2, NPR)):
            ld(i)
        if NPR:
            front(0)
        for i in range(NPR):
            if i + 2 < NPR:
                ld(i + 2)
            if i + 1 < NPR:
                front(i + 1)
            back(i)


def phase_even_attn(b, cx, layer, stop=9, nqt=None):
    e_ = layer // 2
    ev = even_scratch(cx)
    NT = 512
    NQT = T // NT
    NKT = T // 128
    NC_ = 255
    scale = 1.0 / 8.0
    with b.scope():
        KS2 = [b.sb([128, T], BF16, "at_KS2") for g in range(2)]
        KW2 = [b.sb([128, T], BF16, "at_KW2") for g in range(2)]
        KC2 = [b.sb([128, 256], BF16, "at_KC2") for g in range(2)]
        VS = [b.sb([128, NKT, 65], BF16, "at_VS") for g in range(2)]
        VW = [b.sb([128, NKT, 65], BF16, "at_VW") for g in range(2)]
        VC = [b.sb([128, 2, 129], BF16, "at_VC") for g in range(2)]
        GT = b.sb([128, T // 128, 24], F32, "at_GT")
        Eall = b.sb([128, NKT, 128], BF16, "at_E")
        for g in range(2):
            for hb in range(2):
                b.dma("sync", KS2[g][hb * 64:(hb + 1) * 64, :], ev.ks[g * 64:(g + 1) * 64, :],
                      reads=[("ev_ks", "r", t) for t in range(NQT)], writes=[("at_KS2", g)])
                b.dma("sync", KW2[g][hb * 64:(hb + 1) * 64, :], ev.kw[g * 64:(g + 1) * 64, :],
                      reads=[("ev_kw", "r", t) for t in range(NQT)], writes=[("at_KW2", g)])
            b.op("vector", lambda e, g=g: e.memset(VS[g][:, :, 64:65], 1.0), writes=[("at_VS", g)])
            b.op("vector", lambda e, g=g: e.memset(VW[g][:, :, 64:65], 1.0), writes=[("at_VW", g)])
            b.op("vector", lambda e, g=g: e.memset(VC[g][:], 0.0), writes=[("at_VC", g)])
            for kq in range(8):
                ksl = slice(kq * 4, (kq + 1) * 4)
                tok = slice(kq * 512, (kq + 1) * 512)
                b.op("sync", lambda e, g=g, ksl=ksl, tok=tok: e.dma_start(
                    out=VS[g][:, ksl, 0:64], in_=ev.vs[tok, g * 64:(g + 1) * 64].rearrange("(k p) d -> p k d", p=128)),
                    reads=[("ev_vs", kq, su) for su in range(4)] + [("at_VS", g)], writes=[("at_VS", g)], dma=True)
                b.op("sync", lambda e, g=g, ksl=ksl, tok=tok: e.dma_start(
                    out=VW[g][:, ksl, 0:64], in_=ev.vw[tok, g * 64:(g + 1) * 64].rearrange("(k p) d -> p k d", p=128)),
                    reads=[("ev_vw", kq, su) for su in range(4)] + [("at_VW", g)], writes=[("at_VW", g)], dma=True)
        for kq in range(8):
            b.op("sync", lambda e, kq=kq: e.dma_start(out=GT[:, kq * 4:(kq + 1) * 4, :],
                                                     in_=ev.gt[kq * 512:(kq + 1) * 512, :].rearrange("(k p) j -> p k j", p=128)),
                 reads=[("ev_gt", kq, su) for su in range(4)], writes=["at_GT"], dma=True)
        b.op("vector", lambda e: e.memset(Eall[:], BIGM), writes=["at_E"])
        for hb in range(2):
            rows = slice(hb * 64, (hb + 1) * 64)
            b.op("gpsimd", lambda e, rows=rows: asel(e, out=Eall[rows], in_=Eall[rows], pattern=[[128, NKT], [1, 128]], compare_op=ALU.is_ge,
                                                     fill=0.0, base=0, channel_multiplier=-64), reads=["at_E"], writes=["at_E"])
            b.op("gpsimd", lambda e, rows=rows: asel(e, out=Eall[rows], in_=Eall[rows], pattern=[[-128, NKT], [-1, 128]], compare_op=ALU.is_ge,
                                                     fill=0.0, base=63, channel_multiplier=64), reads=["at_E"], writes=["at_E"])
        with (b.scope() if stop >= 1 else contextlib.nullcontext()):
          if stop >= 1:
              w1 = b.sb([128, 32, 128], BF16, "cp_w1")
              w2k = b.sb([128, 128], BF16, "cp_w2k")
              w2v = b.sb([128, 64], BF16, "cp_w2v")
              posT = b.sb([128, 32], F32, "cp_posT")
              src = b.sb([128, T], BF16, "cp_src")
              sA = b.sb([128, 256, 16], BF16, "cp_sA")
              sB = b.sb([128, 256, 16], BF16, "cp_sB")
              hg = b.sb([128, 256], BF16, "cp_hg")
              tA = b.sb([128, 256], F32, "cp_tA")
              tB = b.sb([128, 256], F32, "cp_tB")
              cov = b.sb([128, 2, 64], BF16, "cp_cov")
              load_w(b, cov[:], cx.inp["cover"].rearrange("(k p) j -> p k j", p=128), "cp_cov")
              for kv in range(2):
                  sfx = "k" if kv == 0 else "v"
                  srcd = ev.kc if kv == 0 else ev.vc
                  srck = "ev_kc" if kv == 0 else "ev_vc"
                  for hb in range(2):
                      load_w(b, w1[hb * 64:(hb + 1) * 64, :, :], cx.inp["nsa_cmp_w1_" + sfx][e_].rearrange("(l d) j -> d l j", d=64), "cp_w1")
                      b.op("sync", lambda e, hb=hb, sfx=sfx: e.dma_start(out=posT[hb * 64:(hb + 1) * 64, :],
                                                                        in_=cx.inp["nsa_cmp_pos_" + sfx][e_].rearrange("l d -> d l"),
                                                                        allow_slow_non_contiguous=True), writes=["cp_posT"], dma=True)
                  if kv == 0:
                      load_w(b, w2k[:, 0:64], cx.inp["nsa_cmp_w2_k"][e_], "cp_w2k")
                      load_w(b, w2k[:, 64:128], cx.inp["nsa_cmp_w2_k"][e_], "cp_w2k")
                  else:
                      load_w(b, w2v[:], cx.inp["nsa_cmp_w2_v"][e_], "cp_w2v")
                  b.dma("sync", src[:], srcd, reads=[(srck, t) for t in range(NQT)], writes=["cp_src"])
                  sv = src[:].rearrange("p (n l) -> p n l", l=16)
                  b.op("vector", lambda e, sv=sv: e.tensor_tensor(out=sA[:], in0=sv, in1=bc(posT[:, 0:16], [128, 256, 16], 1), op=ALU.add),
                       reads=["cp_src", "cp_posT"], writes=["cp_sA"])
                  b.op("vector", lambda e, sv=sv: e.tensor_tensor(out=sB[:], in0=sv, in1=bc(posT[:, 16:32], [128, 256, 16], 1), op=ALU.add),
                       reads=["cp_src", "cp_posT"], writes=["cp_sB"])
                  for g in range(2):
                      rows = slice(g * 64, (g + 1) * 64)
                      ps, psk = PS(cx, 1 + g)
                      for l in range(32):
                          if l < 16:
                              rhs = sA[rows, 0:NC_, l]
                          else:
                              rhs = sB[rows, 1:NC_ + 1, l - 16]
                          mm(b, ps[:, 0:NC_], w1[rows, l, :], rhs, l == 0, l == 31, reads=["cp_w1", "cp_sA", "cp_sB"], writes=[psk])
                      gelu_ops(b, ps[:, 0:NC_], psk, hg[:, 0:NC_], "cp_hg", tA[:, 0:NC_], "cp_tA", tB[:, 0:NC_], "cp_tB")
                      if kv == 0:
                          p2, p2k = PS(cx, 3 + g)
                          mm(b, p2[:, 0:NC_], w2k[:], hg[:, 0:NC_], True, True, reads=["cp_hg", "cp_w2k"], writes=[p2k])
                          b.op("scalar", lambda e, g=g, p2=p2: e.copy(out=KC2[g][:, 0:NC_], in_=p2[:, 0:NC_]), reads=[p2k], writes=[("at_KC2", g)])
                      else:
                          for nt_ in range(2):
                              nn = 128 if nt_ == 0 else NC_ - 128
                              p2, p2k = PS(cx, 3 + nt_)
                              mm(b, p2[0:nn, 0:64], hg[:, nt_ * 128:nt_ * 128 + nn], w2v[:], True, True, reads=["cp_hg", "cp_w2v"], writes=[p2k])
                              b.op("scalar", lambda e, g=g, nt_=nt_, nn=nn, p2=p2: e.copy(out=VC[g][0:nn, nt_, 0:64], in_=p2[0:nn, 0:64]),
                                   reads=[p2k, ("at_VC", g)], writes=[("at_VC", g)])
                              b.op("vector", lambda e, g=g, nt_=nt_, nn=nn: e.tensor_copy(out=VC[g][0:nn, nt_, 64:128], in_=cov[0:nn, nt_, :]),
                                   reads=["cp_cov", ("at_VC", g)], writes=[("at_VC", g)])
                              b.op("vector", lambda e, g=g, nt_=nt_, nn=nn: e.memset(VC[g][0:nn, nt_, 128:129], 1.0),
                                   reads=[("at_VC", g)], writes=[("at_VC", g)])
        qn = [b.sb([128, 4, NT], BF16, "at_qn") for i in range(2)]
        qr = [b.sb([128, 4, NT], BF16, "at_qr") for i in range(2)]
        qrs = [b.sb([128, 4, NT], BF16, "at_qrs") for i in range(2)]
        PT = [b.sb([128, NKT, NT], BF16, "at_PT") for i in range(2)]
        yacc = b.sb([128, 4, 512], F32, "at_yacc")
        imp = b.sb([128, 4, 64], F32, "at_imp")
        rden = [b.sb([128, 1], F32, "at_rden") for i in range(4)]
        sc1 = b.sb([128, 4, 64], F32, "at_sc1")
        sc2 = b.sb([128, 4, 64], F32, "at_sc2")
        sc3 = b.sb([128, 4, 64], F32, "at_sc3")
        m8a = b.sb([128, 4, 8], F32, "at_m8a")
        m8b = b.sb([128, 4, 8], F32, "at_m8b")
        selm = b.sb([128, 4, 128], F32, "at_selm")
        selT = b.sb([128, NT], BF16, "at_selT")
        yT = [b.sb([128, 4, NT], BF16, "at_yT") for i in range(2)]
        pt_i = 0
        rd_i = 0
        pv_i = 0
        for qt in (range(NQT if nqt is None else nqt) if stop >= 2 else ()):
            s = qt % 2
            q0 = qt * NT
            qsl = slice(q0, q0 + NT)
            def ldq(qt_):
                s_ = qt_ % 2
                qsl_ = slice(qt_ * NT, (qt_ + 1) * NT)
                b.dma("sync", qn[s_][:], ev.qn.rearrange("(c p) t -> p c t", p=128)[:, :, qsl_],
                      reads=[(("ev_q", c), "p", qt_) for c in range(4)], writes=[("at_qn", s_)])
                b.dma("sync", qr[s_][:], ev.qr.rearrange("(c p) t -> p c t", p=128)[:, :, qsl_],
                      reads=[(("ev_q", c), "r", qt_) for c in range(4)], writes=[("at_qr", s_)])
                qsw = ev.qr.rearrange("(c h p) t -> h p c t", h=2, p=64)
                for hb_ in range(2):
                    b.dma("sync", qrs[s_][hb_ * 64:(hb_ + 1) * 64, :, :], qsw[1 - hb_][:, :, qsl_],
                          reads=[(("ev_q", c), "r", qt_) for c in range(4)], writes=[("at_qrs", s_, hb_)])
            nq_ = NQT if nqt is None else nqt
            if qt == 0:
                ldq(0)
            if qt + 1 < nq_:
                ldq(qt + 1)
            for g in range(2):
                for hh in range(4):
                    h = 4 * g + hh
                    c_, hb = h // 2, h % 2
                    prow = slice(hb * 64, (hb + 1) * 64)
                    P = PT[pt_i % 2]; pk = ("at_PT", pt_i % 2); pt_i += 1
                    for nt_ in range(2):
                        nn = 128 if nt_ == 0 else NC_ - 128
                        ps, psk = PS(cx, 1 + nt_)
                        mm(b, ps[0:nn, :], KC2[g][prow, nt_ * 128:nt_ * 128 + nn], qn[s][prow, c_, :], True, True,
                           reads=[("at_KC2", g), ("at_qn", s)], writes=[psk])
                        b.op("scalar", lambda e, P=P, nt_=nt_, nn=nn, ps=ps: e.activation(out=P[0:nn, nt_, :], in_=ps[0:nn, :], func=AF.Exp, scale=scale),
                             reads=[psk], writes=[(pk, nt_)])
                        b.op("gpsimd", lambda e, P=P, nt_=nt_, nn=nn, q0=q0: asel(e,
                            out=P[0:nn, nt_, :], in_=P[0:nn, nt_, :], pattern=[[1, NT]], compare_op=ALU.is_ge, fill=0.0,
                            base=q0 - 31 - 16 * 128 * nt_, channel_multiplier=-16), reads=[(pk, nt_)], writes=[(pk, nt_)])
                    for qs in range(4):
                        po, pok = PS(cx, 3 + pv_i % 2); pv_i += 1
                        for nt_ in range(2):
                            nn = 128 if nt_ == 0 else NC_ - 128
                            mm(b, po[:, 0:129], P[0:nn, nt_, qs * 128:(qs + 1) * 128], VC[g][0:nn, nt_, :], nt_ == 0, nt_ == 1,
                               reads=[(pk, 0), (pk, 1), ("at_VC", g)], writes=[pok])
                        rd = rden[rd_i % 4]; rdk = ("at_rden", rd_i % 4); rd_i += 1
                        b.op("vector", lambda e, rd=rd, po=po: e.tensor_scalar(out=rd[:], in0=po[:, 128:129], scalar1=1e-30, scalar2=None, op0=ALU.max),
                             reads=[pok], writes=[rdk])
                        b.op("vector", lambda e, rd=rd: e.reciprocal(out=rd[:], in_=rd[:]), reads=[rdk], writes=[rdk])
                        gi = qt * 4 + qs
                        b.op("vector", lambda e, rd=rd, po=po, qs=qs, h=h, gi=gi: e.tensor_scalar(
                            out=yacc[:, qs, h * 64:(h + 1) * 64], in0=po[:, 0:64], scalar1=rd[:, 0:1], scalar2=GT[:, gi, 3 * h:3 * h + 1],
                            op0=ALU.mult, op1=ALU.mult), reads=[pok, rdk, "at_GT"], writes=[("at_yacc", qs, h)])
                        if hh == 0:
                            b.op("vector", lambda e, rd=rd, po=po, qs=qs: e.tensor_scalar(
                                out=imp[:, qs, :], in0=po[:, 64:128], scalar1=rd[:, 0:1], scalar2=None, op0=ALU.mult),
                                reads=[pok, rdk], writes=[("at_imp", qs)])
                        else:
                            b.op("vector", lambda e, rd=rd, po=po, qs=qs: e.scalar_tensor_tensor(
                                out=imp[:, qs, :], in0=po[:, 64:128], scalar=rd[:, 0:1], in1=imp[:, qs, :], op0=ALU.mult, op1=ALU.add),
                                reads=[pok, rdk, ("at_imp", qs)], writes=[("at_imp", qs)])
                for qs in (range(4) if stop >= 3 else ()):
                    qq0 = q0 + qs * 128
                    b.op("gpsimd", lambda e, qs=qs, qq0=qq0: asel(e,
                        out=sc1[:, qs, :], in_=imp[:, qs, :], pattern=[[-64, 64]], compare_op=ALU.is_ge, fill=FORCE,
                        base=qq0 - 128, channel_multiplier=1), reads=[("at_imp", qs)], writes=[("at_sc1", qs)])
                    b.op("gpsimd", lambda e, qs=qs, qq0=qq0: asel(e,
                        out=sc2[:, qs, :], in_=sc1[:, qs, :], pattern=[[-64, 64]], compare_op=ALU.is_ge, fill=NEG,
                        base=qq0, channel_multiplier=1), reads=[("at_sc1", qs)], writes=[("at_sc2", qs)])
                    b.op("vector", lambda e, qs=qs: e.memset(sc2[:, qs, 0:1], FORCE), reads=[("at_sc2", qs)], writes=[("at_sc2", qs)])
                    b.op("vector", lambda e, qs=qs: e.max(out=m8a[:, qs, :], in_=sc2[:, qs, :]), reads=[("at_sc2", qs)], writes=[("at_m8a", qs)])
                    b.op("vector", lambda e, qs=qs: e.match_replace(out=sc3[:, qs, :], in_to_replace=m8a[:, qs, :], in_values=sc2[:, qs, :],
                                                                    imm_value=-3.0e38),
                         reads=[("at_sc2", qs), ("at_m8a", qs)], writes=[("at_sc3", qs)])
                    b.op("vector", lambda e, qs=qs: e.max(out=m8b[:, qs, :], in_=sc3[:, qs, :]), reads=[("at_sc3", qs)], writes=[("at_m8b", qs)])
                    for hb in range(2):
                        b.op("vector", lambda e, qs=qs, hb=hb: e.tensor_scalar(out=selm[:, qs, hb * 64:(hb + 1) * 64], in0=sc2[:, qs, :],
                                                                           scalar1=m8b[:, qs, 7:8], scalar2=-1.0, op0=ALU.is_ge, op1=ALU.add),
                             reads=[("at_sc2", qs), ("at_m8b", qs)], writes=[("at_selm", qs)])

                def emit_selT():
                    pst, pstk = PS(cx, 5)
                    for qs in range(4):
                        b.op("tensor", lambda e, qs=qs, pst=pst: e.transpose(pst[:, qs * 128:(qs + 1) * 128], selm[:, qs, :], cx.ident_f[:]),
                             reads=[("at_selm", qs), "ident_f"], writes=[pstk])
                    b.op("scalar", lambda e, pst=pst: e.copy(out=selT[:], in_=pst), reads=[pstk], writes=["at_selT"])

                def emit_qk(hh, br_):
                    nonlocal pt_i
                    h = 4 * g + hh
                    c_, hb = h // 2, h % 2
                    prow = slice(hb * 64, (hb + 1) * 64)
                    P = PT[pt_i % 2]; pk = ("at_PT", pt_i % 2); pt_i += 1
                    if br_ == 1:
                        kts = list(range(0, 4 * qt + 4))
                        Ksrc, Kkey = KS2[g], ("at_KS2", g)
                    else:
                        kts = list(range(max(0, 4 * qt - 4), 4 * qt + 4))
                        Ksrc, Kkey = KW2[g], ("at_KW2", g)
                    for kt in kts:
                        k0 = kt * 128
                        ps, psk = PS(cx, (1, 2, 6, 7)[kt % 4])
                        if kt % 2 == 0:
                            pr, qsrc, qkey = prow, qr[s], ("at_qr", s)
                        else:
                            pr, qsrc, qkey = slice((1 - hb) * 64, (2 - hb) * 64), qrs[s], ("at_qrs", s, 1 - hb)
                        mm(b, ps, Ksrc[pr, k0:k0 + 128], qsrc[pr, c_, :], True, br_ == 2,
                           reads=[Kkey, qkey], writes=[psk])
                        if br_ == 1:
                            mm(b, ps, Eall[pr, kt, :], selT[pr, :], False, True, reads=["at_E", "at_selT"], writes=[psk])
                        b.op("scalar", lambda e, P=P, kt=kt, ps=ps: e.activation(out=P[:, kt, :], in_=ps, func=AF.Exp, scale=scale),
                             reads=[psk], writes=[(pk, kt)])
                        if kt >= 4 * qt:
                            b.op("gpsimd", lambda e, P=P, kt=kt, k0=k0, q0=q0: asel(e,
                                out=P[:, kt, :], in_=P[:, kt, :], pattern=[[1, NT]], compare_op=ALU.is_ge, fill=0.0,
                                base=q0 - k0, channel_multiplier=-1), reads=[(pk, kt)], writes=[(pk, kt)])
                        elif br_ == 2:
                            b.op("gpsimd", lambda e, P=P, kt=kt, k0=k0, q0=q0: asel(e,
                                out=P[:, kt, :], in_=P[:, kt, :], pattern=[[-1, NT]], compare_op=ALU.is_ge, fill=0.0,
                                base=k0 - q0 + 511, channel_multiplier=1), reads=[(pk, kt)], writes=[(pk, kt)])
                    return (hh, br_, P, pk)

                def emit_pv(hh, br_, P, pk):
                    nonlocal pv_i, rd_i
                    h = 4 * g + hh
                    if br_ == 1:
                        Vsrc, Vkey = VS[g], ("at_VS", g)
                    else:
                        Vsrc, Vkey = VW[g], ("at_VW", g)
                    for qs in range(4):
                        hi = 4 * qt + qs
                        lo = 0 if br_ == 1 else max(0, hi - 4)
                        klist = list(range(lo, hi + 1))
                        po, pok = PS(cx, 3 + pv_i % 2); pv_i += 1
                        for i_, kt in enumerate(klist):
                            mm(b, po[:, 0:65], P[:, kt, qs * 128:(qs + 1) * 128], Vsrc[:, kt, :], i_ == 0, i_ == len(klist) - 1,
                               reads=[(pk, kt), Vkey], writes=[pok])
                        rd = rden[rd_i % 4]; rdk = ("at_rden", rd_i % 4); rd_i += 1
                        b.op("vector", lambda e, rd=rd, po=po: e.reciprocal(out=rd[:], in_=po[:, 64:65]), reads=[pok], writes=[rdk])
                        gi = qt * 4 + qs
                        b.op("vector", lambda e, rd=rd, po=po, qs=qs, h=h, gi=gi, br_=br_: e.tensor_scalar(
                            out=sc1[:, qs, :], in0=po[:, 0:64], scalar1=rd[:, 0:1], scalar2=GT[:, gi, 3 * h + br_:3 * h + br_ + 1],
                            op0=ALU.mult, op1=ALU.mult), reads=[pok, rdk, "at_GT"], writes=[("at_sc1", qs)])
                        b.op("vector", lambda e, qs=qs, h=h: e.tensor_tensor(
                            out=yacc[:, qs, h * 64:(h + 1) * 64], in0=yacc[:, qs, h * 64:(h + 1) * 64], in1=sc1[:, qs, :], op=ALU.add),
                            reads=[("at_sc1", qs), ("at_yacc", qs, h)], writes=[("at_yacc", qs, h)])

                if stop >= 4:
                    units = [(hh, 2) for hh in range(4)] + [(hh, 1) for hh in range(4)]
                    pend = None
                    for (hh, br_) in units:
                        if br_ == 1 and hh == 0:
                            emit_selT()
                        cur = emit_qk(hh, br_)
                        if pend is not None:
                            emit_pv(*pend)
                        pend = cur
                    emit_pv(*pend)
                elif stop >= 3:
                    emit_selT()
            if stop < 5:
                continue
            for fc in range(4):
                pt, ptk = PS(cx, 6 + fc % 2)
                for qs in range(4):
                    b.op("tensor", lambda e, fc=fc, qs=qs, pt=pt: e.transpose(pt[:, qs * 128:(qs + 1) * 128], yacc[:, qs, fc * 128:(fc + 1) * 128], cx.ident_f[:]),
                         reads=[("at_yacc", qs, 2 * fc), ("at_yacc", qs, 2 * fc + 1), "ident_f"], writes=[ptk])
                b.op("scalar", lambda e, fc=fc, pt=pt, s=s: e.copy(out=yT[s][:, fc, :], in_=pt), reads=[ptk], writes=[("at_yT", s, fc)])
            b.dma("sync", ev.ynsa.rearrange("(c p) t -> p c t", p=128)[:, :, qsl], yT[s][:],
                  reads=[("at_yT", s, fc) for fc in range(4)], writes=[("ev_ynsa", qt)])


def phase_even_out(b, cx, layer, h_in, hk_in, h_out, hk_out):
    e_ = layer // 2
    ev = even_scratch(cx)
    NT = 512
    with b.scope():
        w_out = b.sb([128, 8, 1024], BF16, "eo_w")
        load_w_kc(b, w_out, cx.inp["even_w_out"][e_], "eo_w", 8)
        ht = [b.sb([128, 8, NT], F32, "eo_ht") for i in range(2)]
        yy = [b.sb([128, 8, NT], BF16, "eo_y") for i in range(2)]
        hv_in = hview(h_in)
        hv_out = hview(h_out)
        def ld(t):
            s = t % 2
            tsl = slice(t * NT, (t + 1) * NT)
            b.dma("sync", ht[s][:], hv_in[:, :, tsl], reads=[(hk_in, t)], writes=[("eo_ht", s)])
            b.dma("sync", yy[s][:, 0:4, :], ev.ylru.rearrange("(c p) t -> p c t", p=128)[:, :, tsl],
                  reads=[("ev_ylru", c, t) for c in range(4)], writes=[("eo_y", s, 0)])
            b.dma("sync", yy[s][:, 4:8, :], ev.ynsa.rearrange("(c p) t -> p c t", p=128)[:, :, tsl],
                  reads=[("ev_ynsa", t)], writes=[("eo_y", s, 1)])
        ld(0)
        for t in range(T // NT):
            s = t % 2
            tsl = slice(t * NT, (t + 1) * NT)
            if t + 1 < T // NT:
                ld(t + 1)
            for mc in range(8):
                ps, psk = PS(cx, 1 + mc % 2)
                for kc in range(8):
                    mm(b, ps, w_out[:, kc, mc * 128:(mc + 1) * 128], yy[s][:, kc, :], kc == 0, kc == 7,
                       reads=[("eo_y", s, 0), ("eo_y", s, 1), ("eo_w", kc)], writes=[psk])
                b.op("vector", lambda e, mc=mc, ps=ps, s=s: e.tensor_tensor(out=ht[s][:, mc, :], in0=ps, in1=ht[s][:, mc, :], op=ALU.add),
                     reads=[psk, ("eo_ht", s)], writes=[("eo_ht", s)])
            b.dma("sync", hv_out[:, :, tsl], ht[s][:], reads=[("eo_ht", s)], writes=[(hk_out, t)])


def phase_even_mixer(b, cx, layer, h_in, hk_in, h_out, hk_out):
    phase_even_proj(b, cx, layer, h_in, hk_in)
    phase_even_lru(b, cx, layer)
    phase_even_attn(b, cx, layer)
    phase_even_out(b, cx, layer, h_in, hk_in, h_out, hk_out)


_BOUND_REGS = {}


def bound_reg(e):
    if id(e) not in _BOUND_REGS:
        _BOUND_REGS[id(e)] = e.to_reg(NSLOT - 1)
    return _BOUND_REGS[id(e)]


def wbound_reg(e, val):
    key = (id(e), "w", int(val))
    if key not in _BOUND_REGS:
        _BOUND_REGS[key] = e.to_reg(int(val))
    return _BOUND_REGS[key]


def moe_scratch(cx):
    if hasattr(cx, "mo"):
        return cx.mo
    mo = Ctx()
    mo.xs = scratch(cx, "mo_xs", [NSLOT, XSW], BF16)
    mo.ys = scratch(cx, "mo_ys", [NSLOT, 1024], F32)
    cx.mo = mo
    return mo


def phase_moe_init(b, cx):
    mo = moe_scratch(cx)
    with b.scope():
        zb = b.sb([128, 4, XSW], BF16, "mo_zb")
        b.op("vector", lambda e: e.memset(zb[:], 0.0), writes=["mo_zb"])
        for i in range(NSLOT_T):
            b.dma("sync", mo.xs[i * 512:(i + 1) * 512, :].rearrange("(k p) d -> p k d", p=128), zb[:],
                  reads=["mo_zb"], writes=[("mo_xs0", i)])


def phase_moe2(b, cx, layer, h_in, hk_in, h_out, hk_out):
    mo = moe_scratch(cx)
    NT = 512
    NSUB = 16
    V = "vector"
    hv_in = hview(h_in)
    hv_out = hview(h_out)
    with b.scope():
        dest_i = b.sb([128, 32], I32, "mo_dest")
        gid_i = b.sb([128, 16], I32, "mo_gid")
        widx = b.sb([128, NSLOT_T, 4], I32, "mo_widx")
        wgu = [b.sb([128, 8, 1024], BF16, "mo_wgu"), None]
        wd = [b.sb([128, 4, 1024], BF16, "mo_wd"), None]

        def load_expert(i, ex):
            ws = (i * 4 + ex) % 2
            for (wt, nm, key) in ((wgu[ws], "moe_w_gu", "mo_wgu"), (wd[ws], "moe_w_dn", "mo_wd")):
                wsrc = cx.inp[nm]
                b.op("gpsimd", lambda e, wt=wt, wsrc=wsrc: e.indirect_dma_start(
                    out=wt[:].rearrange("p c m -> p (c m)"), out_offset=None, in_=wsrc,
                    in_offset=bass.IndirectOffsetOnAxis(ap=widx[:, i, ex:ex + 1], axis=0),
                    bounds_check=wbound_reg(e, wsrc.shape[0] - 1), oob_is_err=False),
                    reads=["mo_widx"], writes=[(key, ws)], dma=True)

        with b.scope():
            xt_all = b.sb([128, 32, XSW], BF16, "mo_xt")
            xt_f = xt_all.bitcast(F32)
            G1 = b.sb([128, 32, 4], F32, "mo_G1")
            ht = [b.sb([128, 8, NT], F32, "mo_ht") for i in range(2)]
            xnf2 = [b.sb([128, 8, NT], F32, "mo_xnf") for i in range(2)]
            sq = b.sb([128, 8, NT], F32, "mo_sq")
            rs = b.sb([128, NT], F32, "mo_rs")
            wr = b.sb([128, 8, 20], F32, "mo_wr")
            bias = b.sb([128, 20], F32, "mo_bias")
            b.op("sync", lambda e: e.dma_start(out=wr[:, :, 0:4], in_=cx.inp["moe_w_group"][layer].rearrange(
                "(c p) g -> p c g", p=128), allow_slow_non_contiguous=True), writes=[("mo_wr", 0)], dma=True)
            b.op("sync", lambda e: e.dma_start(out=wr[:, :, 4:20], in_=cx.inp["moe_w_expert"][layer].rearrange(
                "(c p) g -> p c g", p=128), allow_slow_non_contiguous=True), writes=[("mo_wr", 1)], dma=True)
            b.op("sync", lambda e: e.dma_start(out=bias[:, 0:4], in_=cx.inp["moe_b_group"][layer:layer + 1, :]
                                               .partition_broadcast(128)), writes=[("mo_bias", 0)], dma=True)
            b.op("sync", lambda e: e.dma_start(out=bias[:, 4:20], in_=cx.inp["moe_b_expert"][layer:layer + 1, :]
                                               .partition_broadcast(128)), writes=[("mo_bias", 1)], dma=True)
            S3 = [128, NSUB, 4]
            S4 = [128, NSUB, 4, 4]
            Lb = b.sb([128, NSUB, 20], F32, "mo_Lb")
            gmax = b.sb([128, NSUB], F32, "mo_gmax")
            gsh = b.sb(S3, F32, "mo_gsh")
            gexp = b.sb(S3, F32, "mo_gexp")
            gsum = b.sb([128, NSUB], F32, "mo_gsum")
            gone = b.sb(S3, F32, "mo_gone")
            gcs = b.sb(S3, F32, "mo_gcs")
            m1 = b.sb(S3, F32, "mo_m1")
            m2 = b.sb(S3, F32, "mo_m2")
            one1 = b.sb(S4, F32, "mo_one1")
            one2 = b.sb(S4, F32, "mo_one2")
            E2 = b.sb(S4, F32, "mo_E2")
            dd = b.sb(S3, F32, "mo_dd")
            w1 = b.sb(S3, F32, "mo_w1")
            w2 = b.sb(S3, F32, "mo_w2")
            g4 = b.sb(S4, F32, "mo_g4")
            gate = b.sb([128, NSUB, 16], F32, "mo_gate")
            for hf in range(2):
                psr, psrk = PS(cx, 1)
                def nrm(t_):
                    s_ = t_ % 2
                    rmsnorm_tile(b, cx, ht[s_][:], ("mo_ht", s_), NT, 3, layer, [(xnf2[s_][:], ("mo_xnf", s_))],
                                 sq[:], "mo_sq", 0, rs[:], "mo_rs")
                if hf == 0:
                    b.dma("sync", ht[0][:], hv_in[:, :, 0:NT], reads=[(hk_in, 0)], writes=[("mo_ht", 0)])
                    b.dma("sync", ht[1][:], hv_in[:, :, NT:2 * NT], reads=[(hk_in, 1)], writes=[("mo_ht", 1)])
                    nrm(0)
                for tt in range(4):
                    t = hf * 4 + tt
                    s = t % 2
                    xnf = xnf2[s]
                    xnfk = ("mo_xnf", s)
                    if t + 1 < 8:
                        nrm(t + 1)
                    if t + 2 < 8:
                        b.dma("sync", ht[s][:], hv_in[:, :, (t + 2) * NT:(t + 3) * NT], reads=[(hk_in, t + 2)],
                              writes=[("mo_ht", s)])
                    for su in range(4):
                        sidx = tt * 4 + su
                        for kc in range(8):
                            mm(b, psr[:, sidx * 20:(sidx + 1) * 20], xnf[:, kc, su * 128:(su + 1) * 128], wr[:, kc, :],
                               kc == 0, kc == 7, reads=[xnfk, ("mo_wr", 0), ("mo_wr", 1)], writes=[psrk])
                    for su in range(4):
                        sg_ = t * 4 + su
                        for h2 in range(2):
                            ps, psk = PS(cx, 2 + h2)
                            for j in range(4):
                                fc = h2 * 4 + j
                                b.op("tensor", lambda e, ps=ps, j=j, fc=fc, su=su, xnf=xnf: e.transpose(
                                    ps[:, j * 128:(j + 1) * 128], xnf[:, fc, su * 128:(su + 1) * 128], cx.ident_f[:]),
                                    reads=[xnfk, "ident_f"], writes=[psk])
                            b.op("scalar", lambda e, ps=ps, sg_=sg_, h2=h2: e.copy(
                                out=xt_all[:, sg_, h2 * 512:(h2 + 1) * 512], in_=ps), reads=[psk], writes=[("mo_xt", sg_)])
                ssl = slice(hf * NSUB, (hf + 1) * NSUB)
                b.op(V, lambda e, psr=psr: e.tensor_tensor(out=Lb[:], in0=psr[:, 0:NSUB * 20].rearrange("p (s j) -> p s j", j=20),
                                                           in1=bc(bias[:], [128, NSUB, 20], 1), op=ALU.add),
                     reads=[psrk, ("mo_bias", 0), ("mo_bias", 1)], writes=["Lb"])
                G = Lb[:, :, 0:4]
                E = Lb[:, :, 4:20].rearrange("p s (g e) -> p s g e", g=4)
                b.op(V, lambda e, G=G: e.tensor_reduce(out=gmax[:], in_=G, axis=AX.X, op=ALU.max), reads=["Lb"], writes=["gmax"])
                b.op(V, lambda e, G=G: e.tensor_tensor(out=gsh[:], in0=G, in1=bc(gmax[:], S3, 2), op=ALU.subtract),
                     reads=["Lb", "gmax"], writes=["gsh"])
                b.op("scalar", lambda e: e.activation(out=gexp[:], in_=gsh[:], func=AF.Exp), reads=["gsh"], writes=["gexp"])
                b.op(V, lambda e: e.tensor_reduce(out=gsum[:], in_=gexp[:], axis=AX.X, op=ALU.add), reads=["gexp"], writes=["gsum"])
                b.op(V, lambda e: e.reciprocal(out=gsum[:], in_=gsum[:]), reads=["gsum"], writes=["gsum"])
                b.op(V, lambda e: e.tensor_single_scalar(out=gone[:], in_=gsh[:], scalar=0.0, op=ALU.is_equal),
                     reads=["gsh"], writes=["gone"])
                b.op(V, lambda e: e.tensor_copy(out=gcs[:, :, 0:1], in_=gone[:, :, 0:1]), reads=["gone"], writes=["gcs"])
                for g_ in range(1, 4):
                    b.op(V, lambda e, g_=g_: e.tensor_tensor(out=gcs[:, :, g_:g_ + 1], in0=gcs[:, :, g_ - 1:g_], in1=gone[:, :, g_:g_ + 1], op=ALU.add),
                         reads=["gone", "gcs"], writes=["gcs"])
                b.op(V, lambda e, ssl=ssl: e.scalar_tensor_tensor(out=G1[:, ssl, :], in0=gcs[:], scalar=1.0, in1=gone[:], op0=ALU.is_equal, op1=ALU.mult),
                     reads=["gone", "gcs"], writes=[("mo_G1", hf)])
                b.op(V, lambda e, ssl=ssl: e.tensor_tensor(out=gone[:], in0=G1[:, ssl, :], in1=bc(gsum[:], S3, 2), op=ALU.mult),
                     reads=[("mo_G1", hf), "gsum", "gone"], writes=["gone"])
                b.op(V, lambda e, E=E: e.tensor_reduce(out=m1[:], in_=E, axis=AX.X, op=ALU.max), reads=["Lb"], writes=["m1"])
                b.op(V, lambda e, E=E: e.tensor_tensor(out=one1[:], in0=E, in1=bc(m1[:], S4, 3), op=ALU.is_equal),
                     reads=["Lb", "m1"], writes=["one1"])
                b.op(V, lambda e: e.scalar_tensor_tensor(out=E2[:].rearrange("p s g e -> p s (g e)"),
                                                         in0=one1[:].rearrange("p s g e -> p s (g e)"), scalar=-1e30,
                                                         in1=Lb[:, :, 4:20], op0=ALU.mult, op1=ALU.add),
                     reads=["one1", "Lb"], writes=["E2"])
                b.op(V, lambda e: e.tensor_reduce(out=m2[:], in_=E2[:], axis=AX.X, op=ALU.max), reads=["E2"], writes=["m2"])
                b.op(V, lambda e: e.tensor_tensor(out=one2[:], in0=E2[:], in1=bc(m2[:], S4, 3), op=ALU.is_equal),
                     reads=["E2", "m2"], writes=["one2"])
                b.op(V, lambda e: e.tensor_tensor(out=dd[:], in0=m2[:], in1=m1[:], op=ALU.subtract), reads=["m1", "m2"], writes=["dd"])
                b.op("scalar", lambda e: e.activation(out=dd[:], in_=dd[:], func=AF.Exp), reads=["dd"], writes=["dd"])
                b.op(V, lambda e: e.tensor_scalar(out=w1[:], in0=dd[:], scalar1=1.0, scalar2=None, op0=ALU.add),
                     reads=["dd"], writes=["w1"])
                b.op(V, lambda e: e.reciprocal(out=w1[:], in_=w1[:]), reads=["w1"], writes=["w1"])
                b.op(V, lambda e: e.tensor_tensor(out=w2[:], in0=dd[:], in1=w1[:], op=ALU.mult), reads=["dd", "w1"], writes=["w2"])
                b.op(V, lambda e: e.tensor_tensor(out=w1[:], in0=w1[:], in1=gone[:], op=ALU.mult), reads=["w1", "gone"], writes=["w1"])
                b.op(V, lambda e: e.tensor_tensor(out=w2[:], in0=w2[:], in1=gone[:], op=ALU.mult), reads=["w2", "gone"], writes=["w2"])
                b.op(V, lambda e: e.tensor_tensor(out=g4[:], in0=one1[:], in1=bc(w1[:], S4, 3), op=ALU.mult),
                     reads=["one1", "w1"], writes=["g4"])
                b.op(V, lambda e: e.tensor_tensor(out=one2[:], in0=one2[:], in1=bc(w2[:], S4, 3), op=ALU.mult),
                     reads=["one2", "w2"], writes=["one2"])
                b.op(V, lambda e: e.tensor_tensor(out=gate[:].rearrange("p s (g e) -> p s g e", g=4), in0=g4[:], in1=one2[:], op=ALU.add),
                     reads=["g4", "one2"], writes=["gate"])
                b.op(V, lambda e, ssl=ssl: e.tensor_tensor(out=xt_f[:, ssl, 512:516], in0=gate[:, :, 0:4], in1=gate[:, :, 4:8], op=ALU.add),
                     reads=["gate"], writes=[("mo_gate4", hf)])
                for g_ in (2, 3):
                    b.op(V, lambda e, ssl=ssl, g_=g_: e.tensor_tensor(out=xt_f[:, ssl, 512:516], in0=xt_f[:, ssl, 512:516], in1=gate[:, :, 4 * g_:4 * g_ + 4], op=ALU.add),
                         reads=["gate", ("mo_gate4", hf)], writes=[("mo_gate4", hf)])
            G1K = [("mo_G1", 0), ("mo_G1", 1)]
            pp, ppk = PS(cx, 4)
            G1f = G1[:].rearrange("p s g -> p (s g)")
            mm(b, pp[:, 0:128], cx.tri_f[:], G1f, True, True, reads=G1K + ["tri_f"], writes=[ppk])
            mm(b, pp[:, 128:256], cx.ones_f[:], G1f, True, True, reads=G1K + ["ones_f"], writes=[ppk])
            rank = b.sb([128, 32, 4], F32, "mo_rank")
            tot = b.sb([128, 32, 4], F32, "mo_tot")
            scA = b.sb([128, 32, 4], F32, "mo_scA")
            scB = b.sb([128, 32, 4], F32, "mo_scB")
            b.op(V, lambda e: e.tensor_copy(out=rank[:].rearrange("p s g -> p (s g)"), in_=pp[:, 0:128]), reads=[ppk], writes=["mo_rank"])
            b.op(V, lambda e: e.tensor_copy(out=tot[:].rearrange("p s g -> p (s g)"), in_=pp[:, 128:256]), reads=[ppk], writes=["mo_tot"])
            cur, curk = tot, "mo_tot"
            pingpong = [(scA, "mo_scA"), (scB, "mo_scB")]
            for i_, d_ in enumerate((1, 2, 4, 8, 16)):
                nxt, nxtk = pingpong[i_ % 2]
                b.op(V, lambda e, cur=cur, nxt=nxt, d_=d_: e.tensor_tensor(out=nxt[:, d_:, :], in0=cur[:, d_:, :], in1=cur[:, 0:32 - d_, :], op=ALU.add),
                     reads=[curk, nxtk], writes=[nxtk])
                b.op(V, lambda e, cur=cur, nxt=nxt, d_=d_: e.tensor_copy(out=nxt[:, 0:d_, :], in_=cur[:, 0:d_, :]),
                     reads=[curk, nxtk], writes=[nxtk])
                cur, curk = nxt, nxtk
            incl, inclk = cur, curk
            ngc = b.sb([128, 4, 8], F32, "mo_ngc")
            cnt = b.sb([128, 4], F32, "mo_cnt")
            base = b.sb([128, 4], F32, "mo_base")
            b.op(V, lambda e: e.tensor_tensor(out=ngc[:], in0=bc(incl[:, 31, :], [128, 4, 8], 2), in1=cx.thr512[:], op=ALU.is_gt),
                 reads=[inclk, "thr512"], writes=["mo_ngc"])
            b.op(V, lambda e: e.tensor_reduce(out=cnt[:], in_=ngc[:], axis=AX.X, op=ALU.add), reads=["mo_ngc"], writes=["mo_cnt"])
            b.op(V, lambda e: e.memset(base[:], 0.0), writes=["mo_base"])
            b.op(V, lambda e: e.tensor_scalar(out=base[:, 1:2], in0=cnt[:, 0:1], scalar1=512.0, scalar2=None, op0=ALU.mult),
                 reads=["mo_cnt", "mo_base"], writes=["mo_base"])
            for g_ in (2, 3):
                b.op(V, lambda e, g_=g_: e.scalar_tensor_tensor(out=base[:, g_:g_ + 1], in0=cnt[:, g_ - 1:g_], scalar=512.0, in1=base[:, g_ - 1:g_],
                                                                op0=ALU.mult, op1=ALU.add),
                     reads=["mo_cnt", "mo_base"], writes=["mo_base"])
            off = scB
            b.op(V, lambda e: e.tensor_tensor(out=off[:], in0=incl[:], in1=tot[:], op=ALU.subtract), reads=[inclk, "mo_tot", "mo_scB"], writes=["mo_scB"])
            b.op(V, lambda e: e.tensor_tensor(out=off[:], in0=off[:], in1=bc(base[:], [128, 32, 4], 1), op=ALU.add),
                 reads=["mo_scB", "mo_base"], writes=["mo_scB"])
            b.op(V, lambda e: e.tensor_tensor(out=off[:], in0=off[:], in1=rank[:], op=ALU.add), reads=["mo_scB", "mo_rank"], writes=["mo_scB"])
            b.op(V, lambda e: e.scalar_tensor_tensor(out=off[:], in0=off[:], scalar=-1.0, in1=G1[:], op0=ALU.add, op1=ALU.mult),
                 reads=["mo_scB"] + G1K, writes=["mo_scB"])
            dest_f = b.sb([128, 32], F32, "mo_destf")
            b.op(V, lambda e: e.tensor_reduce(out=dest_f[:], in_=off[:], axis=AX.X, op=ALU.add), reads=["mo_scB"], writes=["mo_destf"])
            b.op(V, lambda e: e.tensor_copy(out=dest_i[:], in_=dest_f[:]), reads=["mo_destf"], writes=["mo_dest"])
            gcm = b.sb([128, NSLOT_T, 3], F32, "mo_gcm")
            gid_f = b.sb([128, 16], F32, "mo_gidf")
            b.op(V, lambda e: e.memset(gid_f[:], 0.0), writes=["mo_gidf"])
            b.op(V, lambda e: e.tensor_tensor(out=gcm[:], in0=bc(base[:, 1:4], [128, NSLOT_T, 3], 1), in1=cx.tstart[:], op=ALU.is_le),
                 reads=["mo_base", "tstart"], writes=["mo_gcm"])
            b.op(V, lambda e: e.tensor_reduce(out=gid_f[:, 0:NSLOT_T], in_=gcm[:], axis=AX.X, op=ALU.add), reads=["mo_gcm", "mo_gidf"], writes=["mo_gidf"])
            b.op(V, lambda e: e.tensor_copy(out=gid_i[:], in_=gid_f[:]), reads=["mo_gidf"], writes=["mo_gid"])
            wif = b.sb([128, NSLOT_T, 4], F32, "mo_wif")
            b.op(V, lambda e: e.scalar_tensor_tensor(out=wif[:], in0=bc(gid_f[:, 0:NSLOT_T], [128, NSLOT_T, 4], 2), scalar=512.0,
                                                     in1=bc(cx.k32[:, 0:4], [128, NSLOT_T, 4], 1), op0=ALU.mult, op1=ALU.add),
                 reads=["mo_gidf", "k32"], writes=["mo_wif"])
            b.op(V, lambda e: e.tensor_scalar(out=wif[:], in0=wif[:], scalar1=float(layer * 16 * 128), scalar2=None, op0=ALU.add),
                 reads=["mo_wif"], writes=["mo_wif"])
            b.op(V, lambda e: e.tensor_copy(out=widx[:], in_=wif[:]), reads=["mo_wif"], writes=["mo_widx"])
            if getattr(cx, "debug", False):
                dbg_d = scratch(cx, f"dbg_dest{layer}", [128, 32], I32)
                dbg_g = scratch(cx, f"dbg_gid{layer}", [128, 16], I32)
                b.dma("sync", dbg_d, dest_i[:], reads=["mo_dest"], writes=["dbg_d"])
                b.dma("sync", dbg_g, gid_i[:], reads=["mo_gid"], writes=["dbg_g"])
            load_expert(0, 0)
            for s_ in range(32):
                b.op("gpsimd", lambda e, s_=s_: e.indirect_dma_start(
                    out=mo.xs[:, :], out_offset=bass.IndirectOffsetOnAxis(ap=dest_i[:, s_:s_ + 1], axis=0),
                    in_=xt_all[:, s_, :], in_offset=None, bounds_check=bound_reg(e), oob_is_err=False),
                    reads=["mo_dest", ("mo_xt", s_), ("mo_gate4", s_ // NSUB)] + [("mo_xs0", i) for i in range(NSLOT_T)],
                    writes=[("mo_xs", s_)], dma=True)
        XSK = [("mo_xs", s_) for s_ in range(32)]
        with b.scope():
            wgu[1] = b.sb([128, 8, 1024], BF16, "mo_wgu")
            wd[1] = b.sb([128, 4, 1024], BF16, "mo_wd")
            sg = [b.sb([128, NT], F32, "mo_sg") for i in range(2)]
            t1 = [b.sb([128, NT], F32, "mo_t1") for i in range(2)]
            hT = [b.sb([128, 4, NT], BF16, "mo_hT") for i in range(2)]
            xtk = [b.sb([128, 4, XSW], BF16, "mo_xtk") for i in range(2)]
            xtk_f = [x_.bitcast(F32) for x_ in xtk]
            xsT = [b.sb([128, 8, NT], BF16, "mo_xsT") for i in range(2)]
            gT = [b.sb([4, NT], F32, "mo_gT") for i in range(2)]
            ytk = [b.sb([128, 4, 1024], F32, "mo_ytk") for i in range(2)]
            hold = {}

            def load_tile(i):
                s2 = i % 2
                b.dma("sync", xtk[s2][:], mo.xs[i * 512:(i + 1) * 512, :].rearrange("(k p) d -> p k d", p=128),
                      reads=XSK, writes=[("mo_xtk", s2)])

            load_tile(0)
            for i in range(NSLOT_T):
                s2 = i % 2
                if i + 1 < NSLOT_T:
                    load_tile(i + 1)
                for fc in range(8):
                    pb_, pbk = cx.psum_b[5 + fc % 2], ("ps", 5 + fc % 2)
                    for k in range(4):
                        b.op("tensor", lambda e, pb_=pb_, k=k, fc=fc, s2=s2: e.transpose(
                            pb_[:, k * 128:(k + 1) * 128], xtk[s2][:, k, fc * 128:(fc + 1) * 128], cx.ident_b[:]),
                            reads=[("mo_xtk", s2), "ident_b"], writes=[pbk])
                    b.op("scalar", lambda e, pb_=pb_, fc=fc, s2=s2: e.copy(out=xsT[s2][:, fc, :], in_=pb_[:, 0:512]),
                         reads=[pbk], writes=[("mo_xsT", s2, fc)])
                pgt, pgtk = PS(cx, 7)
                for k in range(4):
                    b.op("tensor", lambda e, pgt=pgt, k=k, s2=s2: e.transpose(
                        pgt[0:4, k * 128:(k + 1) * 128], xtk_f[s2][:, k, 512:516], cx.ident_f[:]),
                        reads=[("mo_xtk", s2), "ident_f"], writes=[pgtk])
                b.op("scalar", lambda e, pgt=pgt, s2=s2: e.copy(out=gT[s2][:], in_=pgt[0:4, :]), reads=[pgtk], writes=[("mo_gT", s2)])
                XK = [("mo_xsT", s2, fc) for fc in range(8)]
                for ex in range(4):
                    u = i * 4 + ex
                    ws = u % 2
                    hs = u % 2
                    if ex + 1 < 4:
                        load_expert(i, ex + 1)
                    elif i + 1 < NSLOT_T:
                        load_expert(i + 1, 0)
                    pgb, pgbk = PS(cx, 7)
                    mm(b, pgb, cx.sel_f[0:4, ex, :], gT[s2][:], True, True, reads=[("mo_gT", s2), "sel_f"], writes=[pgbk])
                    for mc in range(4):
                        a = mc % 2
                        pg, pgk = PS(cx, 1 + a)
                        pu, puk = PS(cx, 3 + a)
                        for kc in range(8):
                            mm(b, pg, wgu[ws][:, kc, mc * 128:(mc + 1) * 128], xsT[s2][:, kc, :], kc == 0, kc == 7,
                               reads=XK + [("mo_wgu", ws)], writes=[pgk])
                        for kc in range(8):
                            mm(b, pu, wgu[ws][:, kc, 512 + mc * 128:512 + (mc + 1) * 128], xsT[s2][:, kc, :], kc == 0, kc == 7,
                               reads=XK + [("mo_wgu", ws)], writes=[puk])
                        b.op("scalar", lambda e, a=a, pg=pg: e.activation(out=sg[a][:], in_=pg, func=AF.Silu),
                             reads=[pgk], writes=[("mo_sg", a)])
                        b.op("vector", lambda e, a=a, pgb=pgb: e.tensor_tensor(out=t1[a][:], in0=pgb, in1=sg[a][:], op=ALU.mult),
                             reads=[pgbk, ("mo_sg", a)], writes=[("mo_t1", a)])
                        b.op("vector", lambda e, a=a, hs=hs, mc=mc, pu=pu: e.tensor_tensor(
                            out=hT[hs][:, mc, :], in0=pu, in1=t1[a][:], op=ALU.mult),
                            reads=[puk, ("mo_t1", a)], writes=[("mo_hT", hs, mc)])
                    for k in range(4):
                        for h2 in range(2):
                            j_ = k * 2 + h2
                            py, pyk = PS(cx, (5, 6, 0)[j_ % 3])
                            for kc in range(4):
                                mm(b, py, hT[hs][:, kc, k * 128:(k + 1) * 128], wd[ws][:, kc, h2 * 512:(h2 + 1) * 512], kc == 0, kc == 3,
                                   reads=[("mo_hT", hs, kc), ("mo_wd", ws)], writes=[pyk])
                            ysl = ytk[s2][:, k, h2 * 512:(h2 + 1) * 512]
                            if ex == 0:
                                b.op("scalar", lambda e, py=py, ysl=ysl: e.copy(out=ysl, in_=py),
                                     reads=[pyk], writes=[("mo_ytk", s2, j_)])
                            else:
                                b.op("vector", lambda e, py=py, ysl=ysl: e.tensor_tensor(out=ysl, in0=py, in1=ysl, op=ALU.add),
                                     reads=[pyk, ("mo_ytk", s2, j_)], writes=[("mo_ytk", s2, j_)])
                b.dma("sync", mo.ys[i * 512:(i + 1) * 512, :].rearrange("(k p) d -> p k d", p=128), ytk[s2][:],
                      reads=[("mo_ytk", s2, j_) for j_ in range(8)], writes=[("mo_ys", i)])
        YSK = [("mo_ys", i) for i in range(NSLOT_T)]
        with b.scope():
            ht = [b.sb([128, 8, NT], F32, "mo_hc") for i in range(2)]
            yg = [b.sb([128, 4, 1024], F32, "mo_yg") for i in range(2)]
            def ldc(t):
                s = t % 2
                b.dma("sync", ht[s][:], hv_in[:, :, t * NT:(t + 1) * NT], reads=[(hk_in, t)], writes=[("mo_hc", s)])
                for su in range(4):
                    s_ = t * 4 + su
                    b.op("gpsimd", lambda e, s=s, su=su, s_=s_: e.indirect_dma_start(
                        out=yg[s][:, su, :], out_offset=None, in_=mo.ys[:, :],
                        in_offset=bass.IndirectOffsetOnAxis(ap=dest_i[:, s_:s_ + 1], axis=0),
                        bounds_check=bound_reg(e), oob_is_err=False),
                        reads=["mo_dest"] + YSK, writes=[("mo_yg", s, su)], dma=True)
            ldc(0)
            for t in range(T // NT):
                s = t % 2
                if t + 1 < T // NT:
                    ldc(t + 1)
                for fc in range(8):
                    ps, psk = PS(cx, 1 + fc % 4)
                    for su in range(4):
                        b.op("tensor", lambda e, ps=ps, su=su, fc=fc, s=s: e.transpose(
                            ps[:, su * 128:(su + 1) * 128], yg[s][:, su, fc * 128:(fc + 1) * 128], cx.ident_f[:]),
                            reads=[("mo_yg", s, su), "ident_f"], writes=[psk])
                    b.op("vector", lambda e, ps=ps, fc=fc, s=s: e.tensor_tensor(out=ht[s][:, fc, :], in0=ps, in1=ht[s][:, fc, :], op=ALU.add),
                         reads=[psk, ("mo_hc", s)], writes=[("mo_hc", s)])
                b.dma("sync", hv_out[:, :, t * NT:(t + 1) * NT], ht[s][:], reads=[("mo_hc", s)], writes=[(hk_out, t)])


def host_consts():
    inv = (500000.0 ** (-(np.arange(0, 16, 2, dtype=np.float32)) / 16.0)).astype(np.float32)
    col = np.zeros((128, 1), np.float32)
    for p in range(128):
        d = p % 64
        if d < 16:
            col[p, 0] = inv[d % 8]
    n = np.arange(256)
    j = np.arange(64)
    cover = ((16 * n[:, None] <= 64 * j[None, :] + 63) & (16 * n[:, None] + 31 >= 64 * j[None, :])).astype(np.float32)
    cover[255:] = 0.0
    return {"rope_inv": col, "cover": cover}


WEIGHT_NAMES = [n for n in INPUT_NAMES if n not in ("x", "mem", "positions", "moe_w_gate", "moe_w_up", "moe_w_down")]


def host_moe_weights(inputs):
    wg = np.asarray(inputs["moe_w_gate"], dtype=np.float32)
    wu = np.asarray(inputs["moe_w_up"], dtype=np.float32)
    wd = np.asarray(inputs["moe_w_down"], dtype=np.float32)
    L, E = wg.shape[0], wg.shape[1]
    gu = np.concatenate([wg, wu], axis=-1).reshape(L, E, 8, 128, 1024).transpose(0, 1, 3, 2, 4)
    gu = np.ascontiguousarray(gu).reshape(L * E * 128, 8 * 1024)
    dn = np.ascontiguousarray(wd.reshape(L, E, 4, 128, 1024).transpose(0, 1, 3, 2, 4)).reshape(L * E * 128, 4 * 1024)
    return {"moe_w_gu": gu, "moe_w_dn": dn}


def full_phases(b, cx):
    hbuf = cx.nc.dram_tensor("hbuf", [D, T], F32, kind="Internal").ap()
    phase_rope_tables(b, cx)
    phase_moe_init(b, cx)
    for layer in range(DEPTH):
        h_in, hk_in = (cx.inp["xT"], "x") if layer == 0 else (hbuf, "h")
        if layer % 2 == 0:
            phase_even_mixer(b, cx, layer, h_in, hk_in, hbuf, "h")
        else:
            phase_odd_mixer(b, cx, layer, h_in, hk_in, hbuf, "h")
        phase_xattn(b, cx, layer, hbuf, "h", hbuf, "h")
        phase_moe2(b, cx, layer, hbuf, "h", hbuf, "h")
    phase_final_norm(b, cx, hbuf, "h", cx.out)


def kernel(**inputs):
    x = np.asarray(inputs["x"], dtype=np.float32)
    mem = np.asarray(inputs["mem"], dtype=np.float32)
    pos = np.asarray(inputs["positions"]).astype(np.int32)
    consts = host_consts()
    shapes = {"xT": ((D, T), F32), "memT": ((D, 256), F32), "pos": ((1, T), I32),
              "rope_inv": ((128, 1), F32), "cover": ((256, 64), F32)}
    weights = {}
    for nm in WEIGHT_NAMES:
        w = np.ascontiguousarray(np.asarray(inputs[nm], dtype=np.float32))
        weights[nm] = w
        shapes[nm] = (w.shape, F32)
    mw = host_moe_weights(inputs)
    for nm, w in mw.items():
        weights[nm] = w
        shapes[nm] = (w.shape, F32)
    nc = build_program(shapes, full_phases)
    in_maps = []
    for c in range(NCORES):
        m = {"xT": np.ascontiguousarray(x[c].T), "memT": np.ascontiguousarray(mem[c].T),
             "pos": np.ascontiguousarray(pos[c:c + 1]), "rope_inv": consts["rope_inv"], "cover": consts["cover"]}
        m.update(weights)
        in_maps.append(m)
    res = run_bass_kernel_spmd(nc, in_maps, core_ids=list(range(NCORES)))
    out = np.stack([np.asarray(res.results[c]["out"], dtype=np.float32).T for c in range(NCORES)], axis=0)
    return np.ascontiguousarray(out.astype(np.float32))
```
